# Optimizing a Trainium2 kernel written in Bass

```python
import math
import jax, jax.numpy as jnp
from jax import lax
import numpy as np

D_MODEL = 1024
BATCH = 16
SEQ = 2048
DEPTH = 2

LRU_WIDTH = 512
LRU_HEADS = 8
LRU_HEAD_DIM = LRU_WIDTH // LRU_HEADS
LRU_CONV = 4
LRU_C = 8.0
MOBA_HEADS = 8
MOBA_HEAD_DIM = 64
MOBA_WIDTH = MOBA_HEADS * MOBA_HEAD_DIM
MOBA_BLOCK = 256
MOBA_TOPK = 3
MOBA_Q_CHUNK = 16
MEM_LEN = 256
XATTN_HEADS = 4
XATTN_HEAD_DIM = 128
XATTN_WIDTH = XATTN_HEADS * XATTN_HEAD_DIM
N_BRANCH = 3
IN_COLS = 2 * LRU_WIDTH + 3 * MOBA_WIDTH + XATTN_WIDTH + N_BRANCH * D_MODEL
D_FF = 3 * D_MODEL
FFN_CONV = 3
NORM_EPS = 1e-6

kernel_name = "hybrid_rglru_moba_xattn_convffn"


def rms_norm(x, g):
    x32 = x.astype(jnp.float32)
    y = x32 * lax.rsqrt(jnp.mean(x32 * x32, axis=-1, keepdims=True) + NORM_EPS)
    return (y * g.astype(jnp.float32)).astype(x.dtype)


def causal_dwconv(x, w, b):
    k, c = w.shape
    y = lax.conv_general_dilated(x, w[:, None, :].astype(x.dtype), (1,), [(k - 1, 0)],
                                 dimension_numbers=("NWC", "WIO", "NWC"),
                                 feature_group_count=c)
    return y + b.astype(x.dtype)


def rg_lru(x, w_a, b_a, w_x, b_x, lam):
    bsz, s, w = x.shape
    xh = x.reshape(bsz, s, LRU_HEADS, LRU_HEAD_DIM)
    r = jax.nn.sigmoid((jnp.einsum("bshi,hij->bshj", xh, w_a) + b_a).reshape(bsz, s, w).astype(jnp.float32))
    i = jax.nn.sigmoid((jnp.einsum("bshi,hij->bshj", xh, w_x) + b_x).reshape(bsz, s, w).astype(jnp.float32))
    log_a = -LRU_C * r * jax.nn.softplus(-lam.astype(jnp.float32))
    a = jnp.exp(log_a)
    mult = jnp.sqrt(-jnp.expm1(2.0 * log_a))
    u = mult * i * x.astype(jnp.float32)

    def combine(lhs, rhs):
        a1, b1 = lhs
        a2, b2 = rhs
        return a1 * a2, a2 * b1 + b2

    _, h = lax.associative_scan(combine, (a, u), axis=1)
    return h.astype(x.dtype)


def alibi_slopes(n_heads):
    return jnp.exp2(-8.0 * jnp.arange(1, n_heads + 1, dtype=jnp.float32) / n_heads)


def moba_attention(q, k, v):
    bsz, nh, s, dh = q.shape
    nb = -(-s // MOBA_BLOCK)
    sp = nb * MOBA_BLOCK
    pad = ((0, 0), (0, 0), (0, sp - s), (0, 0))
    q, k, v = jnp.pad(q, pad), jnp.pad(k, pad), jnp.pad(v, pad)
    kb = k.reshape(bsz, nh, nb, MOBA_BLOCK, dh)
    vb = v.reshape(bsz, nh, nb, MOBA_BLOCK, dh)
    kmean = jnp.mean(kb.astype(jnp.float32), axis=3)
    n_sel = min(MOBA_TOPK, nb - 1)
    slopes = alibi_slopes(nh)[None, :, None]
    scale = dh ** -0.5
    nq = sp // MOBA_Q_CHUNK
    qc = q.reshape(bsz, nh, nq, MOBA_Q_CHUNK, dh).transpose(2, 0, 1, 3, 4)
    bi = jnp.arange(bsz)[:, None, None, None]
    hi = jnp.arange(nh)[None, :, None, None]
    blk_pos = jnp.arange(MOBA_BLOCK)

    def attend_chunk(args):
        qi, c = args
        t = c * MOBA_Q_CHUNK + jnp.arange(MOBA_Q_CHUNK)
        own = (c * MOBA_Q_CHUNK) // MOBA_BLOCK
        k_own = lax.dynamic_index_in_dim(kb, own, axis=2, keepdims=False)
        v_own = lax.dynamic_index_in_dim(vb, own, axis=2, keepdims=False)
        dist_own = (t[:, None] - (own * MOBA_BLOCK + blk_pos)[None, :]).astype(jnp.float32)
        s_own = (jnp.einsum("bhqd,bhkd->bhqk", qi, k_own, preferred_element_type=jnp.float32) * scale
                 - slopes[..., None] * dist_own)
        s_own = jnp.where(dist_own >= 0, s_own, -jnp.inf)
        if n_sel == 0:
            p = jax.nn.softmax(s_own, axis=-1).astype(v.dtype)
            return jnp.einsum("bhqk,bhkd->bhqd", p, v_own)
        gate = jnp.einsum("bhqd,bhnd->bhqn", qi.astype(jnp.float32), kmean)
        gate = jnp.where(jnp.arange(nb) < own, gate, -jnp.inf)
        _, sel = lax.top_k(gate, n_sel)
        valid = sel < own
        ks = kb[bi, hi, sel]
        vs = vb[bi, hi, sel]
        kpos_sel = sel[..., None] * MOBA_BLOCK + blk_pos
        dist_sel = (t[None, None, :, None, None] - kpos_sel).astype(jnp.float32)
        s_sel = (jnp.einsum("bhqd,bhqnkd->bhqnk", qi, ks, preferred_element_type=jnp.float32) * scale
                 - slopes[..., None, None] * dist_sel)
        s_sel = jnp.where(valid[..., None], s_sel, -jnp.inf)
        nsk = n_sel * MOBA_BLOCK
        scores = jnp.concatenate([s_sel.reshape(bsz, nh, MOBA_Q_CHUNK, nsk), s_own], axis=-1)
        p = jax.nn.softmax(scores, axis=-1).astype(v.dtype)
        p_sel = p[..., :nsk].reshape(bsz, nh, MOBA_Q_CHUNK, n_sel, MOBA_BLOCK)
        p_own = p[..., nsk:]
        return (jnp.einsum("bhqnk,bhqnkd->bhqd", p_sel, vs)
                + jnp.einsum("bhqk,bhkd->bhqd", p_own, v_own))

    out = lax.map(attend_chunk, (qc, jnp.arange(nq)))
    out = out.transpose(1, 2, 0, 3, 4).reshape(bsz, nh, sp, dh)
    return out[:, :, :s]


def memory_cross_attention(q, mem_k, mem_v):
    scores = jnp.einsum("bshd,bmhd->bhsm", q, mem_k, preferred_element_type=jnp.float32) * (XATTN_HEAD_DIM ** -0.5)
    p = jax.nn.softmax(scores, axis=-1).astype(mem_v.dtype)
    o = jnp.einsum("bhsm,bmhd->bshd", p, mem_v)
    return o.reshape(q.shape[0], q.shape[1], XATTN_WIDTH)


def setup_inputs(seed: int = 0) -> dict:
    key = jax.random.key(seed)
    ks = jax.random.split(key, 32)
    f32 = jnp.float32

    def nrm(k, shape, scale):
        return jax.random.normal(k, shape, f32) * scale

    def gain(k, shape):
        return 1.0 + 0.02 * jax.random.normal(k, shape, f32)

    u = jax.random.uniform(ks[8], (DEPTH, LRU_WIDTH), f32, 0.9, 0.999)
    sa = u ** (1.0 / LRU_C)
    lru_lambda = jnp.log(sa) - jnp.log1p(-sa)
    return {
        "x": nrm(ks[0], (BATCH, SEQ, D_MODEL), 1.0),
        "mem": nrm(ks[1], (BATCH, MEM_LEN, D_MODEL), 1.0),
        "mix_norm_gain": gain(ks[2], (DEPTH, D_MODEL)),
        "w_in": nrm(ks[3], (DEPTH, D_MODEL, IN_COLS), D_MODEL ** -0.5),
        "lru_conv_w": nrm(ks[4], (DEPTH, LRU_CONV, LRU_WIDTH), LRU_CONV ** -0.5),
        "lru_conv_b": nrm(ks[5], (DEPTH, LRU_WIDTH), 0.02),
        "lru_w_a": nrm(ks[6], (DEPTH, LRU_HEADS, LRU_HEAD_DIM, LRU_HEAD_DIM), LRU_HEAD_DIM ** -0.5),
        "lru_b_a": nrm(ks[7], (DEPTH, LRU_HEADS, LRU_HEAD_DIM), 0.02),
        "lru_w_x": nrm(ks[9], (DEPTH, LRU_HEADS, LRU_HEAD_DIM, LRU_HEAD_DIM), LRU_HEAD_DIM ** -0.5),
        "lru_b_x": nrm(ks[10], (DEPTH, LRU_HEADS, LRU_HEAD_DIM), 0.02),
        "lru_lambda": lru_lambda,
        "mem_norm_gain": gain(ks[11], (DEPTH, D_MODEL)),
        "w_mem_kv": nrm(ks[12], (DEPTH, D_MODEL, 2 * XATTN_WIDTH), D_MODEL ** -0.5),
        "w_branch": nrm(ks[13], (DEPTH, N_BRANCH, LRU_WIDTH, D_MODEL), LRU_WIDTH ** -0.5),
        "w_out": nrm(ks[14], (DEPTH, D_MODEL, D_MODEL), D_MODEL ** -0.5),
        "ffn_norm_gain": gain(ks[15], (DEPTH, D_MODEL)),
        "w_ffn_gate": nrm(ks[16], (DEPTH, D_MODEL, D_FF), D_MODEL ** -0.5),
        "w_ffn_up": nrm(ks[17], (DEPTH, D_MODEL, D_FF), D_MODEL ** -0.5),
        "ffn_conv_w": nrm(ks[18], (DEPTH, FFN_CONV, D_FF), FFN_CONV ** -0.5),
        "ffn_conv_b": nrm(ks[19], (DEPTH, D_FF), 0.02),
        "w_ffn_down": nrm(ks[20], (DEPTH, D_FF, D_MODEL), D_FF ** -0.5),
        "final_norm_gain": gain(ks[21], (D_MODEL,)),
    }


def reference(x, mem, mix_norm_gain, w_in, lru_conv_w, lru_conv_b, lru_w_a, lru_b_a, lru_w_x, lru_b_x,
              lru_lambda, mem_norm_gain, w_mem_kv, w_branch, w_out, ffn_norm_gain, w_ffn_gate, w_ffn_up,
              ffn_conv_w, ffn_conv_b, w_ffn_down, final_norm_gain):
    bsz, s, d = x.shape
    m = mem.shape[1]
    splits = list(np.cumsum([LRU_WIDTH, LRU_WIDTH, MOBA_WIDTH, MOBA_WIDTH, MOBA_WIDTH, XATTN_WIDTH]))
    for l in range(DEPTH):
        h = rms_norm(x, mix_norm_gain[l])
        proj = h @ w_in[l]
        xa, ga, qm, km, vm, qx, g_logits = jnp.split(proj, splits, axis=-1)
        xa = causal_dwconv(xa, lru_conv_w[l], lru_conv_b[l])
        y_a = rg_lru(xa, lru_w_a[l], lru_b_a[l], lru_w_x[l], lru_b_x[l], lru_lambda[l]) * jax.nn.gelu(ga)
        to_heads = lambda t_: t_.reshape(bsz, s, MOBA_HEADS, MOBA_HEAD_DIM).transpose(0, 2, 1, 3)
        y_b = moba_attention(to_heads(qm), to_heads(km), to_heads(vm))
        y_b = y_b.transpose(0, 2, 1, 3).reshape(bsz, s, MOBA_WIDTH)
        mkv = rms_norm(mem, mem_norm_gain[l]) @ w_mem_kv[l]
        mk, mv = jnp.split(mkv.reshape(bsz, m, 2, XATTN_HEADS, XATTN_HEAD_DIM), 2, axis=2)
        y_c = memory_cross_attention(qx.reshape(bsz, s, XATTN_HEADS, XATTN_HEAD_DIM), mk[:, :, 0], mv[:, :, 0])
        ys = jnp.stack([y_a, y_b, y_c], axis=2)
        branch_out = jnp.einsum("bsnw,nwd->bsnd", ys, w_branch[l])
        gates = jax.nn.sigmoid(g_logits.astype(jnp.float32)).astype(x.dtype).reshape(bsz, s, N_BRANCH, d)
        merged = jnp.sum(gates * branch_out, axis=2)
        x = x + merged @ w_out[l]
        h = rms_norm(x, ffn_norm_gain[l])
        g = causal_dwconv(h @ w_ffn_gate[l], ffn_conv_w[l], ffn_conv_b[l])
        x = x + (jax.nn.gelu(g) * (h @ w_ffn_up[l])) @ w_ffn_down[l]
    return rms_norm(x, final_norm_gain)
```

```python
import math
from contextlib import ExitStack
import numpy as np
import concourse.bass as bass
import concourse.mybir as mybir
from concourse.bass_utils import run_bass_kernel_spmd

F32 = mybir.dt.float32
BF16 = mybir.dt.bfloat16
U8 = mybir.dt.uint8
AF = mybir.ActivationFunctionType
ALU = mybir.AluOpType
AX = mybir.AxisListType

NCORES = 8
SEQ_PER_CORE = 2
S = 2048
D = 1024
DEPTH = 2
NT = 4
TT = 512
GELU = AF.Gelu_apprx_tanh

ENGS = ["pe", "act", "dve", "pool", "sp"]
NDMA = 24
GRAN = 256


class Dep:
    __slots__ = ("w", "rs")

    def __init__(self):
        self.w = {}
        self.rs = {}


class _Rec:
    def __getattr__(self, name):
        def f(*a, **k):
            self.call = (name, a, k)
            return self
        return f


class Prog:
    def __init__(self, nc, stack):
        self.nc = nc
        self.streams = {e: [] for e in ENGS}
        self.cnt = {e: 0 for e in ENGS}
        self.seen = {e: {} for e in ENGS}
        self.sem = {}
        for e in ENGS:
            self.sem[e] = stack.enter_context(nc.semaphore("s_" + e))
        self.dsem = []
        self.dtot = []
        for i in range(NDMA):
            self.dsem.append(stack.enter_context(nc.semaphore("d_%d" % i)))
            self.dtot.append(0)
            self.sem[("dma", i)] = self.dsem[i]
        self.dnext = 0
        self.ninst = 0

    def _need(self, eng, reads, writes):
        need = {}
        for d in reads:
            for k, v in d.w.items():
                if need.get(k, 0) < v:
                    need[k] = v
        for d in writes:
            for k, v in d.w.items():
                if need.get(k, 0) < v:
                    need[k] = v
            for k, v in d.rs.items():
                if need.get(k, 0) < v:
                    need[k] = v
        out = []
        seen = self.seen[eng]
        raw_self = 0
        if eng in ("act", "dve", "pool"):
            for d in reads:
                v = d.w.get(eng, 0)
                if v > raw_self:
                    raw_self = v
        for k, v in need.items():
            if k == eng:
                if raw_self == 0:
                    continue
                v = raw_self
            if seen.get(k, 0) >= v:
                continue
            seen[k] = v
            out.append((self.sem[k], v))
        return out

    @staticmethod
    def _mark(key, val, reads, writes):
        for d in reads:
            if d.rs.get(key, 0) < val:
                d.rs[key] = val
        for d in writes:
            if d.w.get(key, 0) < val:
                d.w[key] = val

    def op(self, eng, fn, reads=(), writes=(), inc=True):
        waits = self._need(eng, reads, writes)
        if inc:
            self.cnt[eng] += 1
            val = self.cnt[eng]
        else:
            val = self.cnt[eng] + 1
        self._mark(eng, val, reads, writes)
        sem = self.sem[eng]
        self.ninst += 1
        rec = _Rec()
        fn(rec)
        cname, cargs, ckw = rec.call

        def thunk(h):
            for s, v in waits[:-1]:
                h.wait_ge(s, v)
            inst = getattr(h, cname)(*cargs, **ckw)
            if waits:
                inst._wait_ge(*waits[-1])
            if inc:
                inst.then_inc(sem, 1)
        self.streams[eng].append(thunk)

    def dma(self, eng, out, in_, reads=(), writes=(), **kw):
        k = self.dnext
        self.dnext = (self.dnext + 1) % NDMA
        key = ("dma", k)
        waits = self._need(eng, reads, writes)
        prev = self.dtot[k]
        if prev > 0 and self.seen[eng].get(key, 0) < prev:
            self.seen[eng][key] = prev
            waits.append((self.dsem[k], prev))
        self.dtot[k] += 16
        val = self.dtot[k]
        self._mark(key, val, reads, writes)
        sem = self.dsem[k]
        self.ninst += 1

        def thunk(h):
            for s, v in waits:
                h.wait_ge(s, v)
            h.dma_start(out=out, in_=in_, **kw).then_inc(sem, 16)
        self.streams[eng].append(thunk)

    def finish(self):
        waits = [(self.dsem[k], self.dtot[k]) for k in range(NDMA) if self.dtot[k] > 0]
        others = [(self.sem[e], self.cnt[e]) for e in ENGS if e != "sp" and self.cnt[e] > 0]

        def fthunk(h):
            for s, v in waits + others:
                h.wait_ge(s, v)
        self.streams["sp"].append(fthunk)
        nc = self.nc
        st = self.streams
        with nc.Block() as block:
            @block.tensor
            def _(h):
                for t in st["pe"]:
                    t(h)

            @block.scalar
            def _(h):
                for t in st["act"]:
                    t(h)

            @block.vector
            def _(h):
                for t in st["dve"]:
                    t(h)

            @block.gpsimd
            def _(h):
                for t in st["pool"]:
                    t(h)

            @block.sync
            def _(h):
                for t in st["sp"]:
                    t(h)


class SBT:
    def __init__(self, arena, gran, off, shape, dt):
        self.esz = 4 if dt == F32 else 2
        self.off = off
        self.shape = shape
        self.dt = dt
        n = 1
        for s_ in shape:
            n *= s_
        self.nbytes = n * self.esz
        v = arena[:, off:off + self.nbytes].bitcast(dt)
        if len(shape) == 2:
            v = v.rearrange("p (a b) -> p a b", a=shape[0])
        elif len(shape) == 3:
            v = v.rearrange("p (a b c) -> p a b c", a=shape[0], b=shape[1])
        self.v = v
        self.gran = gran

    def dall(self):
        return self.gran[self.off // GRAN:(self.off + self.nbytes + GRAN - 1) // GRAN]

    def d(self, lo, hi):
        a = self.off + lo * self.esz
        b = self.off + hi * self.esz
        return self.gran[a // GRAN:(b + GRAN - 1) // GRAN]

    def d2(self, i0, lo, hi):
        n1 = self.shape[-1] if len(self.shape) == 2 else self.shape[1] * self.shape[2]
        return self.d(i0 * n1 + lo, i0 * n1 + hi)

    def d2s(self, i0s, lo, hi):
        out = []
        for i0 in i0s:
            out += self.d2(i0, lo, hi)
        return out

    def d3(self, i0, i1, lo, hi):
        n2 = self.shape[2]
        base = (i0 * self.shape[1] + i1) * n2
        return self.d(base + lo, base + hi)


class Arena:
    def __init__(self, nc, nbytes):
        self.t = nc.alloc_sbuf_tensor("arena", [128, nbytes], U8)
        self.nbytes = nbytes
        self.gran = [Dep() for _ in range((nbytes + GRAN - 1) // GRAN + 1)]
        self.top = 0

    def alloc(self, shape, dt):
        off = (self.top + GRAN - 1) // GRAN * GRAN
        n = 4 if dt == F32 else 2
        for s_ in shape:
            n *= s_
        assert off + n <= self.nbytes, ("SBUF arena overflow", off, n, self.nbytes)
        b = SBT(self.t, self.gran, off, list(shape), dt)
        self.top = off + b.nbytes
        self.peak = max(getattr(self, "peak", 0), self.top)
        return b

    def mark(self):
        return self.top

    def release(self, m):
        self.top = m


PV_MIXG, PV_FFNG, PV_MEMG, PV_FING = 0, 8, 16, 24
PV_LCW, PV_LCB, PV_LBA, PV_LBX, PV_LAM = 32, 48, 52, 56, 60
PV_FCW, PV_FCB = 64, 136
PV_N = 160


def _chunked(v):
    return np.ascontiguousarray(v.reshape(-1, 128).T)


def build_program(n_layers=DEPTH, n_seq=SEQ_PER_CORE, debug=None):
    nc = bass.Bass("TRN2", target_bir_lowering=False)
    dram = {}

    def din(name, shape):
        dram[name] = nc.dram_tensor(name, list(shape), F32, kind="ExternalInput").ap()
        return dram[name]

    x_d = din("x", [SEQ_PER_CORE, S, D])
    mem_d = din("mem", [SEQ_PER_CORE, 256, D])
    w_in_d = din("w_in", [DEPTH, D, 6144])
    w_mkv_d = din("w_mem_kv", [DEPTH, D, 1024])
    w_br_d = din("w_branch", [DEPTH, 3, 512, D])
    w_out_d = din("w_out", [DEPTH, D, D])
    w_fg_d = din("w_ffn_gate", [DEPTH, D, 3072])
    w_fu_d = din("w_ffn_up", [DEPTH, D, 3072])
    w_fd_d = din("w_ffn_down", [DEPTH, 3072, D])
    pvec_d = din("pvec", [DEPTH, 128, PV_N])
    wabd_d = din("wabd", [DEPTH, 2, 128, 4, 128])
    cident_d = din("c_ident", [128, 128])
    ctri_d = din("c_tri", [128, 128])
    ckrow_d = din("c_krow", [8, 4, S])
    cqrow_d = din("c_qrow", [4, S])
    coneh_d = din("c_onehot", [8, S])
    cpast_d = din("c_past", [128, 4, 16])
    out_d = nc.dram_tensor("out", [SEQ_PER_CORE, S, D], F32, kind="ExternalOutput").ap()
    dbg_d = {}
    if debug:
        for name, shape in debug.items():
            dbg_d[name] = nc.dram_tensor("dbg_" + name, list(shape), F32, kind="ExternalOutput").ap()

    stack = ExitStack()
    P = Prog(nc, stack)
    total = nc.sbuf_top - nc.sbuf_base - 64
    AR = Arena(nc, total // GRAN * GRAN - GRAN)

    psb = [nc.alloc_psum_tensor("ps%d" % i, [128, 512], F32) for i in range(8)]
    psd = [Dep() for _ in range(8)]

    class Ring:
        def __init__(self, banks):
            self.banks = banks
            self.i = 0

        def next(self):
            b = self.banks[self.i % len(self.banks)]
            self.i += 1
            return psb[b], [psd[b]]

    xT = AR.alloc([8, S], F32)
    hT = AR.alloc([8, S], BF16)
    pv = AR.alloc([DEPTH, PV_N], F32)
    identf = AR.alloc([128], F32)
    identb = AR.alloc([128], BF16)
    onesf = AR.alloc([128], F32)
    onesb = AR.alloc([128], BF16)
    trib = AR.alloc([128], BF16)
    pastm = AR.alloc([4, 16], F32)
    cst = AR.alloc([8], F32)
    lruc = AR.alloc([DEPTH, 8], F32)
    wabd = AR.alloc([2, 4 * 128], BF16)
    gtail = AR.alloc([24, 2], F32)
    RING_SLOTS = 8
    SLOT = 4096
    ring_off = (AR.top + GRAN - 1) // GRAN * GRAN
    AR.top = ring_off + RING_SLOTS * SLOT
    ring_state = {"i": 0}

    def ring_alloc(shape, dt=BF16):
        n = 1
        for s_ in shape:
            n *= s_
        nb = n * 2
        ns = (nb + SLOT - 1) // SLOT
        i = ring_state["i"]
        if i + ns > RING_SLOTS:
            i = 0
        ring_state["i"] = i + ns
        return SBT(AR.t, AR.gran, ring_off + i * SLOT, list(shape), dt)

    phase_base = AR.mark()

    def dbg(name, sbt_ap, deps, dram_ap=None):
        if debug and name in dbg_d:
            P.dma("sp", dbg_d[name] if dram_ap is None else dram_ap, sbt_ap, reads=deps)

    def mm_group(ps_ap, ps_deps, pairs, rdeps):
        n = len(pairs)
        for i, (l_ap, r_ap) in enumerate(pairs):
            P.op("pe", (lambda h, l_ap=l_ap, r_ap=r_ap, i=i: h.matmul(ps_ap, l_ap, r_ap, start=(i == 0), stop=(i == n - 1))),
                 reads=rdeps if i == 0 else (), writes=ps_deps, inc=(i == n - 1))

    def wload(dst, src_ap):
        P.dma("pool", dst.v, src_ap, writes=dst.dall())

    P.dma("sp", pv.v, pvec_d.rearrange("l p n -> p l n"), writes=pv.dall())
    P.dma("sp", identf.v, cident_d, writes=identf.dall())
    P.dma("pool", identb.v, cident_d, writes=identb.dall())
    P.dma("pool", trib.v, ctri_d, writes=trib.dall())
    P.dma("sp", pastm.v, cpast_d, writes=pastm.dall())
    P.op("dve", lambda h: h.memset(onesf.v, 1.0), writes=onesf.dall())
    P.op("dve", lambda h: h.memset(onesb.v, 1.0), writes=onesb.dall())
    P.op("dve", lambda h: h.memset(cst.v[:, 0:1], 1e-6), writes=cst.dall())
    P.op("dve", lambda h: h.memset(cst.v[:, 1:2], 1.0), writes=cst.dall())
    P.op("dve", lambda h: h.memset(gtail.v, 0.0), writes=gtail.dall())

    for l in range(n_layers):
        m0 = AR.mark()
        t_ = AR.alloc([4], F32); e_ = AR.alloc([4], F32); z_ = AR.alloc([4], F32)
        z2 = AR.alloc([4], F32); pl = AR.alloc([4], F32); ab = AR.alloc([4], F32)
        lam = pv.v[:, l, PV_LAM:PV_LAM + 4]
        dd = t_.dall() + e_.dall() + z_.dall() + z2.dall() + pl.dall() + ab.dall() + pv.dall() + lruc.dall()
        P.op("dve", lambda h, lam=lam: h.tensor_scalar(t_.v, lam, -1.0, None, op0=ALU.mult), dd, dd)
        P.op("dve", lambda h, lam=lam: h.tensor_tensor(ab.v, t_.v, lam, op=ALU.max), dd, dd)
        P.op("act", lambda h: h.activation(out=e_.v, in_=ab.v, func=AF.Exp, scale=-1.0), dd, dd)
        P.op("dve", lambda h: h.tensor_scalar(z_.v, e_.v, 2.0, None, op0=ALU.add), dd, dd)
        P.op("dve", lambda h: h.reciprocal(z_.v, z_.v), dd, dd)
        P.op("dve", lambda h: h.tensor_tensor(z_.v, z_.v, e_.v, op=ALU.mult), dd, dd)
        P.op("dve", lambda h: h.tensor_tensor(z2.v, z_.v, z_.v, op=ALU.mult), dd, dd)
        P.op("dve", lambda h: h.tensor_scalar(pl.v, z2.v, 1.0 / 13, 1.0 / 11, op0=ALU.mult, op1=ALU.add), dd, dd)
        for cf in (1.0 / 9, 1.0 / 7, 1.0 / 5, 1.0 / 3, 1.0):
            P.op("dve", lambda h: h.tensor_tensor(pl.v, pl.v, z2.v, op=ALU.mult), dd, dd)
            P.op("dve", lambda h, cf=cf: h.tensor_scalar(pl.v, pl.v, cf, None, op0=ALU.add), dd, dd)
        P.op("dve", lambda h: h.tensor_tensor(pl.v, pl.v, z_.v, op=ALU.mult), dd, dd)
        P.op("dve", lambda h: h.tensor_scalar(pl.v, pl.v, 2.0, None, op0=ALU.mult), dd, dd)
        P.op("dve", lambda h: h.tensor_scalar(t_.v, t_.v, 0.0, None, op0=ALU.max), dd, dd)
        P.op("dve", lambda h: h.tensor_tensor(pl.v, pl.v, t_.v, op=ALU.add), dd, dd)
        P.op("dve", lambda h, l=l: h.tensor_scalar(lruc.v[:, l, 0:4], pl.v, -8.0, None, op0=ALU.mult), dd, dd)
        P.op("dve", lambda h, l=l: h.tensor_scalar(lruc.v[:, l, 4:8], pl.v, -16.0, None, op0=ALU.mult), dd, dd)
        AR.release(m0)

    P.marks = []

    def mark(name):
        P.marks.append((name, len(P.streams['pe']), len(P.streams['act']), len(P.streams['dve'])))

    R_MM = Ring([0, 1, 2, 3])
    R_AUX = Ring([4, 5])
    R_ACC = Ring([6, 7])

    def rmsnorm_to_hT(gain_col, l):
        m0 = AR.mark()
        sq = [AR.alloc([TT], F32) for _ in range(2)]
        rs = [AR.alloc([TT], F32) for _ in range(2)]
        for j in range(NT):
            t0 = j * TT
            ps, pd = R_MM.next()
            for c in range(8):
                s_ = sq[c % 2]
                P.op("act", lambda h, s_=s_, c=c: h.activation(out=s_.v, in_=xT.v[:, c, t0:t0 + TT], func=AF.Square),
                     reads=xT.d2(c, t0, t0 + TT), writes=s_.dall())
                P.op("pe", lambda h, s_=s_, c=c, ps=ps: h.matmul(ps[:, :], onesf.v, s_.v, start=(c == 0), stop=(c == 7)),
                     reads=s_.dall() + onesf.dall(), writes=pd)
            r_ = rs[j % 2]
            P.op("act", lambda h, r_=r_, ps=ps: h.activation(out=r_.v, in_=ps[:, :], func=AF.Sqrt, scale=1.0 / D, bias=cst.v[:, 0:1]),
                 reads=pd + cst.dall(), writes=r_.dall())
            P.op("dve", lambda h, r_=r_: h.reciprocal(r_.v, r_.v), reads=r_.dall(), writes=r_.dall())
            for c in range(8):
                P.op("dve", lambda h, r_=r_, c=c: h.scalar_tensor_tensor(
                    out=hT.v[:, c, t0:t0 + TT], in0=xT.v[:, c, t0:t0 + TT], scalar=pv.v[:, l, gain_col + c:gain_col + c + 1],
                    in1=r_.v, op0=ALU.mult, op1=ALU.mult),
                    reads=xT.d2(c, t0, t0 + TT) + r_.dall() + pv.dall(), writes=hT.d2(c, t0, t0 + TT))
        AR.release(m0)

    def hT_rhs(k, t0, n):
        return hT.v[:, k, t0:t0 + n]

    def hT_deps(t0, n):
        return hT.d2s(range(8), t0, t0 + n)

    def load_x(b):
        m0 = AR.mark()
        stg = [AR.alloc([D], F32) for _ in range(2)]
        for tt in range(S // 128):
            s_ = stg[tt % 2]
            P.dma("sp", s_.v, x_d[b, tt * 128:(tt + 1) * 128, :], writes=s_.dall())
            for g in range(2):
                ps, pd = R_MM.next()
                for cc in range(4):
                    c = g * 4 + cc
                    P.op("pe", lambda h, s_=s_, c=c, cc=cc, ps=ps: h.transpose(ps[:, cc * 128:(cc + 1) * 128], s_.v[:, c * 128:(c + 1) * 128], identf.v),
                         reads=s_.dall() + identf.dall(), writes=pd, inc=(cc == 3))
                eng = "act" if g == 0 else "dve"
                outap = xT.v[:, g * 4:(g + 1) * 4, tt * 128:(tt + 1) * 128]
                inap = ps[:, :].rearrange("p (a b) -> p a b", a=4)
                wd = xT.d2s(range(g * 4, g * 4 + 4), tt * 128, (tt + 1) * 128)
                if eng == "act":
                    P.op("act", lambda h, outap=outap, inap=inap: h.copy(out=outap, in_=inap), reads=pd, writes=wd)
                else:
                    P.op("dve", lambda h, outap=outap, inap=inap: h.tensor_copy(out=outap, in_=inap), reads=pd, writes=wd)
        AR.release(m0)

    def final_store(b):
        m0 = AR.mark()
        sq = [AR.alloc([TT], F32) for _ in range(2)]
        rs = [AR.alloc([TT], F32) for _ in range(2)]
        nrm = [AR.alloc([8, TT], F32) for _ in range(2)]
        stg = [AR.alloc([D], F32) for _ in range(2)]
        si = 0
        for j in range(NT):
            t0 = j * TT
            ps, pd = R_MM.next()
            for c in range(8):
                s_ = sq[c % 2]
                P.op("act", lambda h, s_=s_, c=c: h.activation(out=s_.v, in_=xT.v[:, c, t0:t0 + TT], func=AF.Square),
                     reads=xT.d2(c, t0, t0 + TT), writes=s_.dall())
                P.op("pe", lambda h, s_=s_, c=c, ps=ps: h.matmul(ps[:, :], onesf.v, s_.v, start=(c == 0), stop=(c == 7)),
                     reads=s_.dall() + onesf.dall(), writes=pd)
            r_ = rs[j % 2]
            P.op("act", lambda h, r_=r_, ps=ps: h.activation(out=r_.v, in_=ps[:, :], func=AF.Sqrt, scale=1.0 / D, bias=cst.v[:, 0:1]),
                 reads=pd + cst.dall(), writes=r_.dall())
            P.op("dve", lambda h, r_=r_: h.reciprocal(r_.v, r_.v), reads=r_.dall(), writes=r_.dall())
            n_ = nrm[j % 2]
            for c in range(8):
                P.op("dve", lambda h, r_=r_, c=c, n_=n_: h.scalar_tensor_tensor(
                    out=n_.v[:, c, :], in0=xT.v[:, c, t0:t0 + TT], scalar=pv.v[:, 0, PV_FING + c:PV_FING + c + 1],
                    in1=r_.v, op0=ALU.mult, op1=ALU.mult),
                    reads=xT.d2(c, t0, t0 + TT) + r_.dall() + pv.dall(), writes=n_.d2(c, 0, TT))
            for sub in range(4):
                s_ = stg[si % 2]
                si += 1
                for g in range(2):
                    ps2, pd2 = R_MM.next()
                    for cc in range(4):
                        c = g * 4 + cc
                        P.op("pe", lambda h, n_=n_, c=c, cc=cc, ps2=ps2, sub=sub: h.transpose(
                            ps2[:, cc * 128:(cc + 1) * 128], n_.v[:, c, sub * 128:(sub + 1) * 128], identf.v),
                            reads=n_.d2(c, 0, TT) + identf.dall(), writes=pd2, inc=(cc == 3))
                    if g == 0:
                        P.op("act", lambda h, s_=s_, ps2=ps2: h.copy(out=s_.v[:, 0:512], in_=ps2[:, :]), reads=pd2, writes=s_.dall())
                    else:
                        P.op("dve", lambda h, s_=s_, ps2=ps2: h.tensor_copy(out=s_.v[:, 512:1024], in_=ps2[:, :]), reads=pd2, writes=s_.dall())
                P.dma("sp", out_d[b, t0 + sub * 128:t0 + (sub + 1) * 128, :], s_.v, reads=s_.dall())
        AR.release(m0)

    def lru_phase(l, yA):
        m0 = AR.mark()
        xat = [AR.alloc([4, 3 + TT], F32) for _ in range(2)]
        P.op("dve", lambda h: h.memset(xat[0].v[:, :, 0:3], 0.0), writes=xat[0].dall())
        P.dma("pool", wabd.v, wabd_d[l].rearrange("g p c o -> p g (c o)"), writes=wabd.dall())
        wga = ring_alloc([8, 512])
        wload(wga, w_in_d[l, :, 512:1024].rearrange("(k p) n -> p k n", p=128))
        wxa = ring_alloc([8, 512])
        wload(wxa, w_in_d[l, :, 0:512].rearrange("(k p) n -> p k n", p=128))
        for j in range(NT):
            t0 = j * TT
            for c in range(4):
                ps, pd = R_MM.next()
                mm_group(ps[:, :], pd, [(wga.v[:, k, c * 128:(c + 1) * 128], hT_rhs(k, t0, TT)) for k in range(8)],
                         wga.dall() + hT_deps(t0, TT))
                P.op("act", lambda h, ps=ps, c=c, t0=t0: h.activation(out=yA.v[:, c, t0:t0 + TT], in_=ps[:, :], func=GELU),
                     reads=pd, writes=yA.d2(c, t0, t0 + TT))
        NS = 2
        tmp = {}
        for nm in ("xc", "r", "i", "a", "m"):
            tmp[nm] = [AR.alloc([TT], F32) for _ in range(NS)]
        tmp["xb"] = [AR.alloc([TT], BF16) for _ in range(NS)]
        carry = AR.alloc([4], F32)
        r_gate = Ring([4, 5, 6, 7])
        iters = [(j, c) for j in range(NT) for c in range(4)]
        ctx = {}

        def head(idx):
            j, c = iters[idx]
            t0 = j * TT
            xa = xat[j % 2]
            xap = xat[(j - 1) % 2]
            q = idx % NS
            xc, xb = tmp["xc"][q], tmp["xb"][q]
            if j > 0:
                P.op("act", lambda h: h.copy(out=xa.v[:, c, 0:3], in_=xap.v[:, c, TT:TT + 3]),
                     reads=xap.d2(c, TT, TT + 3), writes=xa.d2(c, 0, 3))
            ps, pd = R_MM.next()
            mm_group(ps[:, :], pd, [(wxa.v[:, k, c * 128:(c + 1) * 128], hT_rhs(k, t0, TT)) for k in range(8)],
                     wxa.dall() + hT_deps(t0, TT))
            P.op("act", lambda h: h.copy(out=xa.v[:, c, 3:3 + TT], in_=ps[:, :]), reads=pd, writes=xa.d2(c, 3, 3 + TT))
            cw = lambda k: pv.v[:, l, PV_LCW + k * 4 + c:PV_LCW + k * 4 + c + 1]
            cb = pv.v[:, l, PV_LCB + c:PV_LCB + c + 1]
            xin = lambda k: xa.v[:, c, k:k + TT]
            xdeps = xa.d2(c, 0, TT + 3) + pv.dall()
            P.op("dve", lambda h: h.tensor_scalar(xc.v, xin(3), cw(3), cb, op0=ALU.mult, op1=ALU.add), reads=xdeps, writes=xc.dall())
            for k in range(3):
                P.op("dve", lambda h, k=k: h.scalar_tensor_tensor(out=xc.v, in0=xin(k), scalar=cw(k), in1=xc.v, op0=ALU.mult, op1=ALU.add),
                     reads=xdeps + xc.dall(), writes=xc.dall())
            P.op("dve", lambda h: h.tensor_copy(out=xb.v, in_=xc.v), reads=xc.dall(), writes=xb.dall())
            psa, pda = r_gate.next()
            P.op("pe", lambda h: h.matmul(psa[:, :], wabd.v[:, 0, c * 128:(c + 1) * 128], xb.v, start=True, stop=True),
                 reads=xb.dall() + wabd.dall(), writes=pda)
            psx, pdx = r_gate.next()
            P.op("pe", lambda h: h.matmul(psx[:, :], wabd.v[:, 1, c * 128:(c + 1) * 128], xb.v, start=True, stop=True),
                 reads=xb.dall() + wabd.dall(), writes=pdx)
            ctx[idx] = (psa, pda, psx, pdx)

        def tail(idx):
            j, c = iters[idx]
            t0 = j * TT
            q = idx % NS
            xc, r_, i_, a_, m_ = (tmp[n][q] for n in ("xc", "r", "i", "a", "m"))
            psa, pda, psx, pdx = ctx.pop(idx)
            P.op("act", lambda h: h.activation(out=r_.v, in_=psa[:, :], func=AF.Sigmoid, bias=pv.v[:, l, PV_LBA + c:PV_LBA + c + 1]),
                 reads=pda + pv.dall(), writes=r_.dall())
            P.op("act", lambda h: h.activation(out=i_.v, in_=psx[:, :], func=AF.Sigmoid, bias=pv.v[:, l, PV_LBX + c:PV_LBX + c + 1]),
                 reads=pdx + pv.dall(), writes=i_.dall())
            P.op("act", lambda h: h.activation(out=a_.v, in_=r_.v, func=AF.Exp, scale=lruc.v[:, l, c:c + 1]),
                 reads=r_.dall() + lruc.dall(), writes=a_.dall())
            P.op("act", lambda h: h.activation(out=m_.v, in_=r_.v, func=AF.Exp, scale=lruc.v[:, l, 4 + c:5 + c]),
                 reads=r_.dall() + lruc.dall(), writes=m_.dall())
            P.op("act", lambda h: h.activation(out=m_.v, in_=m_.v, func=AF.Ln, scale=-1.0, bias=cst.v[:, 1:2]),
                 reads=m_.dall() + cst.dall(), writes=m_.dall())
            P.op("act", lambda h: h.activation(out=m_.v, in_=m_.v, func=AF.Exp, scale=0.5), reads=m_.dall(), writes=m_.dall())
            P.op("dve", lambda h: h.tensor_tensor(i_.v, i_.v, xc.v, op=ALU.mult), reads=i_.dall() + xc.dall(), writes=i_.dall())
            P.op("dve", lambda h: h.tensor_tensor(i_.v, i_.v, m_.v, op=ALU.mult), reads=i_.dall() + m_.dall(), writes=i_.dall())
            hcur = r_
            if j == 0:
                P.op("dve", lambda h: h.tensor_tensor_scan(hcur.v, a_.v, i_.v, 0.0, ALU.mult, ALU.add),
                     reads=a_.dall() + i_.dall(), writes=hcur.dall())
            else:
                P.op("dve", lambda h: h.tensor_tensor_scan(hcur.v, a_.v, i_.v, carry.v[:, c:c + 1], ALU.mult, ALU.add),
                     reads=a_.dall() + i_.dall() + carry.dall(), writes=hcur.dall())
            P.op("dve", lambda h: h.tensor_copy(out=carry.v[:, c:c + 1], in_=hcur.v[:, TT - 1:TT]), reads=hcur.dall(), writes=carry.dall())
            P.op("dve", lambda h: h.tensor_tensor(yA.v[:, c, t0:t0 + TT], yA.v[:, c, t0:t0 + TT], hcur.v, op=ALU.mult),
                 reads=hcur.dall() + yA.d2(c, t0, t0 + TT), writes=yA.d2(c, t0, t0 + TT))

        for idx in range(len(iters)):
            if idx == 0:
                head(0)
            if idx + 1 < len(iters):
                head(idx + 1)
            tail(idx)
        AR.release(m0)

    def xattn_phase(l, b, yC):
        m0 = AR.mark()
        memK = AR.alloc([4, 256], BF16)
        memV = AR.alloc([2, 512], BF16)
        memh = AR.alloc([8, 256], BF16)
        m1 = AR.mark()
        mstg = [AR.alloc([D], F32)] * 2
        memT = AR.alloc([8, 256], F32)
        sq = [AR.alloc([256], F32) for _ in range(2)]
        rs = AR.alloc([256], F32)
        wk = ring_alloc([8, 512])
        wload(wk, w_mkv_d[l, :, 0:512].rearrange("(k p) n -> p k n", p=128))
        wv = ring_alloc([8, 512])
        wload(wv, w_mkv_d[l, :, 512:1024].rearrange("(k p) n -> p k n", p=128))
        wq = ring_alloc([8, 512])
        wload(wq, w_in_d[l, :, 2560:3072].rearrange("(k p) n -> p k n", p=128))
        for mt in range(2):
            s_ = mstg[mt]
            P.dma("sp", s_.v, mem_d[b, mt * 128:(mt + 1) * 128, :], writes=s_.dall())
            for g in range(2):
                ps, pd = R_MM.next()
                for cc in range(4):
                    c = g * 4 + cc
                    P.op("pe", lambda h, s_=s_, c=c, cc=cc, ps=ps: h.transpose(ps[:, cc * 128:(cc + 1) * 128], s_.v[:, c * 128:(c + 1) * 128], identf.v),
                         reads=s_.dall() + identf.dall(), writes=pd, inc=(cc == 3))
                outap = memT.v[:, g * 4:(g + 1) * 4, mt * 128:(mt + 1) * 128]
                inap = ps[:, :].rearrange("p (a b) -> p a b", a=4)
                P.op("act", lambda h, outap=outap, inap=inap: h.copy(out=outap, in_=inap), reads=pd, writes=memT.dall())
        ps, pd = R_MM.next()
        for c in range(8):
            s_ = sq[c % 2]
            P.op("act", lambda h, s_=s_, c=c: h.activation(out=s_.v, in_=memT.v[:, c, :], func=AF.Square),
                 reads=memT.dall(), writes=s_.dall())
            P.op("pe", lambda h, s_=s_, c=c, ps=ps: h.matmul(ps[:, 0:256], onesf.v, s_.v, start=(c == 0), stop=(c == 7)),
                 reads=s_.dall() + onesf.dall(), writes=pd)
        P.op("act", lambda h, ps=ps: h.activation(out=rs.v, in_=ps[:, 0:256], func=AF.Sqrt, scale=1.0 / D, bias=cst.v[:, 0:1]),
             reads=pd + cst.dall(), writes=rs.dall())
        P.op("dve", lambda h: h.reciprocal(rs.v, rs.v), reads=rs.dall(), writes=rs.dall())
        for c in range(8):
            P.op("dve", lambda h, c=c: h.scalar_tensor_tensor(
                out=memh.v[:, c, :], in0=memT.v[:, c, :], scalar=pv.v[:, l, PV_MEMG + c:PV_MEMG + c + 1],
                in1=rs.v, op0=ALU.mult, op1=ALU.mult),
                reads=memT.dall() + rs.dall() + pv.dall(), writes=memh.dall())
        for hd in range(4):
            ps, pd = R_MM.next()
            mm_group(ps[:, 0:256], pd, [(wk.v[:, k, hd * 128:(hd + 1) * 128], memh.v[:, k, :]) for k in range(8)],
                     wk.dall() + memh.dall())
            P.op("act", lambda h, ps=ps, hd=hd: h.copy(out=memK.v[:, hd, :], in_=ps[:, 0:256]), reads=pd, writes=memK.dall())
        for mt in range(2):
            ps, pd = R_MM.next()
            mm_group(ps[:, :], pd, [(memh.v[:, k, mt * 128:(mt + 1) * 128], wv.v[:, k, :]) for k in range(8)],
                     wv.dall() + memh.dall())
            P.op("act", lambda h, ps=ps, mt=mt: h.copy(out=memV.v[:, mt, :], in_=ps[:, :]), reads=pd, writes=memV.dall())
        AR.release(m1)
        qx = [AR.alloc([TT], BF16) for _ in range(2)]
        pt = [AR.alloc([2, TT], BF16) for _ in range(2)]
        rd = [AR.alloc([TT], F32) for _ in range(2)]
        sc = 128 ** -0.5
        r_q = Ring([0, 1])
        r_s = Ring([2, 3, 4, 5])
        iters = [(j, hd) for j in range(NT) for hd in range(4)]
        ctx = {}

        def head(idx):
            j, hd = iters[idx]
            t0 = j * TT
            q_ = qx[idx % 2]
            ps, pd = r_q.next()
            mm_group(ps[:, :], pd, [(wq.v[:, k, hd * 128:(hd + 1) * 128], hT_rhs(k, t0, TT)) for k in range(8)],
                     wq.dall() + hT_deps(t0, TT))
            P.op("act", lambda h: h.copy(out=q_.v, in_=ps[:, :]), reads=pd, writes=q_.dall())
            sb = []
            for mt in range(2):
                sps, spd = r_s.next()
                P.op("pe", lambda h, sps=sps, mt=mt: h.matmul(sps[:, :], memK.v[:, hd, mt * 128:(mt + 1) * 128], q_.v, start=True, stop=True),
                     reads=q_.dall() + memK.dall(), writes=spd)
                sb.append((sps, spd))
            ctx[idx] = sb

        def tail(idx):
            j, hd = iters[idx]
            t0 = j * TT
            p_ = pt[idx % 2]; r_ = rd[idx % 2]
            sb = ctx.pop(idx)
            for mt in range(2):
                sps, spd = sb[mt]
                P.op("act", lambda h, sps=sps, mt=mt: h.activation(out=p_.v[:, mt, :], in_=sps[:, :], func=AF.Exp, scale=sc),
                     reads=spd, writes=p_.dall())
            ops, opd = psb[6], [psd[6]]
            mm_group(ops[:, :], opd, [(memV.v[:, mt, hd * 128:(hd + 1) * 128], p_.v[:, mt, :]) for mt in range(2)],
                     memV.dall() + p_.dall())
            dps, dpd = psb[7], [psd[7]]
            mm_group(dps[:, :], dpd, [(onesb.v, p_.v[:, mt, :]) for mt in range(2)], onesb.dall() + p_.dall())
            P.op("act", lambda h: h.activation(out=r_.v, in_=dps[:, :], func=AF.Ln), reads=dpd, writes=r_.dall())
            P.op("act", lambda h: h.activation(out=r_.v, in_=r_.v, func=AF.Exp, scale=-1.0), reads=r_.dall(), writes=r_.dall())
            P.op("dve", lambda h: h.tensor_tensor(yC.v[:, hd, t0:t0 + TT], ops[:, :], r_.v, op=ALU.mult),
                 reads=opd + r_.dall(), writes=yC.d2(hd, t0, t0 + TT))

        for idx in range(len(iters)):
            if idx == 0:
                head(0)
            if idx + 1 < len(iters):
                head(idx + 1)
            tail(idx)
        AR.release(m0)

    def moba_phase(l, yB):
        m0 = AR.mark()
        NSET = 2
        KA = [[AR.alloc([S], BF16) for _ in range(2)] for _ in range(NSET)]
        QA = [[AR.alloc([S], BF16) for _ in range(2)] for _ in range(NSET)]
        VT = AR.alloc([16, 2, 128], BF16)
        NPT = 4
        PT = [AR.alloc([2, 256], BF16) for _ in range(NPT)]
        kmT = [[AR.alloc([8], BF16) for _ in range(2)] for _ in range(NSET)]
        ksum = AR.alloc([8], F32)
        gm = AR.alloc([16], F32)
        top8 = AR.alloc([16], F32)
        stage = [[AR.alloc([2, 128], BF16) for _ in range(2)] for _ in range(4)]
        dsh = [AR.alloc([256], F32) for _ in range(2)]
        r_proj = Ring([0, 1])
        r_sc = Ring([3, 4, 5])
        r_acc = Ring([6, 7])
        gate_bank, gate_dep = psb[2], [psd[2]]
        for s_ in range(NSET):
            for hh in range(2):
                P.op("dve", lambda h, s_=s_, hh=hh: h.memset(QA[s_][hh].v[64:72, 0:1024], 0.0), writes=QA[s_][hh].d(0, 1024))
                P.dma("pool", QA[s_][hh].v[72:76, :], cqrow_d, writes=QA[s_][hh].dall())
                P.dma("pool", KA[s_][hh].v[64:72, :], coneh_d, writes=KA[s_][hh].dall())
        P.op("dve", lambda h: h.memset(VT.v, 1.0), writes=VT.dall())
        for q4 in range(4):
            for sub in range(2):
                t_ = stage[q4][sub]
                P.op("dve", lambda h, t_=t_: h.memset(t_.v, 0.0), writes=t_.dall())
        state = {"pti": 0, "acc": 0, "wv": {}}

        def project(pair):
            s_ = pair % NSET
            ka, qa, km = KA[s_], QA[s_], kmT[s_]
            wq = ring_alloc([8, 128]); wload(wq, w_in_d[l, :, 1024 + pair * 128:1024 + (pair + 1) * 128].rearrange("(k p) n -> p k n", p=128))
            wk = ring_alloc([8, 128]); wload(wk, w_in_d[l, :, 1536 + pair * 128:1536 + (pair + 1) * 128].rearrange("(k p) n -> p k n", p=128))
            for hh in range(2):
                P.dma("pool", ka[hh].v[72:76, :], ckrow_d[pair * 2 + hh], writes=ka[hh].dall())
            for j in range(NT):
                t0 = j * TT
                ps, pd = r_proj.next()
                mm_group(ps[:, :], pd, [(wk.v[:, k, :], hT_rhs(k, t0, TT)) for k in range(8)], wk.dall() + hT_deps(t0, TT))
                P.op("dve", lambda h, ps=ps: h.tensor_copy(out=ka[0].v[0:64, t0:t0 + TT], in_=ps[0:64, :]), reads=pd, writes=ka[0].d(t0, t0 + TT))
                P.op("act", lambda h, ps=ps: h.copy(out=ka[1].v[0:64, t0:t0 + TT], in_=ps[64:128, :]), reads=pd, writes=ka[1].d(t0, t0 + TT))
                ps, pd = r_proj.next()
                mm_group(ps[:, :], pd, [(wq.v[:, k, :], hT_rhs(k, t0, TT)) for k in range(8)], wq.dall() + hT_deps(t0, TT))
                P.op("dve", lambda h, ps=ps: h.tensor_scalar(qa[0].v[0:64, t0:t0 + TT], ps[0:64, :], 0.125, None, op0=ALU.mult), reads=pd, writes=qa[0].d(t0, t0 + TT))
                P.op("act", lambda h, ps=ps: h.mul(out=qa[1].v[0:64, t0:t0 + TT], in_=ps[64:128, :], mul=0.125), reads=pd, writes=qa[1].d(t0, t0 + TT))
            for hh in range(2):
                P.op("dve", lambda h, hh=hh: h.tensor_reduce(out=ksum.v[0:64, :], in_=ka[hh].v[0:64, :].rearrange("p (a b) -> p a b", a=8),
                                                              axis=AX.X, op=ALU.add),
                     reads=ka[hh].dall(), writes=ksum.dall())
                P.op("act", lambda h, hh=hh: h.mul(out=km[hh].v[0:64, :], in_=ksum.v[0:64, :], mul=1.0 / 256), reads=ksum.dall(), writes=km[hh].dall())
            for qb in range(4, 8):
                for sub in range(2):
                    qs = qb * 256 + sub * 128
                    gcol = ((qb - 4) * 2 + sub) * 16
                    for hh in range(2):
                        P.op("pe", lambda h, hh=hh, qs=qs, gcol=gcol: h.matmul(gate_bank[:, gcol + hh * 8:gcol + (hh + 1) * 8], qa[hh].v[0:64, qs:qs + 128],
                                                                             km[hh].v[0:64, :], start=True, stop=True),
                             reads=qa[hh].d(qs, qs + 128) + km[hh].dall(), writes=gate_dep)
                    P.op("dve", lambda h, qb=qb, gcol=gcol: h.tensor_tensor(gm.v, gate_bank[:, gcol:gcol + 16], pastm.v[:, qb - 4, :], op=ALU.add),
                         reads=gate_dep + pastm.dall(), writes=gm.dall())
                    st_ = stage[qb - 4][sub]
                    for hh in range(2):
                        P.op("dve", lambda h, hh=hh: h.max(out=top8.v[:, hh * 8:(hh + 1) * 8], in_=gm.v[:, hh * 8:(hh + 1) * 8]),
                             reads=gm.dall(), writes=top8.dall())
                        P.op("dve", lambda h, hh=hh, st_=st_, qb=qb: h.tensor_scalar(
                            st_.v[:, hh, 64:64 + qb], gm.v[:, hh * 8:hh * 8 + qb], top8.v[:, hh * 8 + 2:hh * 8 + 3], -30000.0,
                            op0=ALU.is_lt, op1=ALU.mult),
                            reads=gm.dall() + top8.dall(), writes=st_.dall())

        def project_v(pair):
            vt = VT
            wv = ring_alloc([8, 128]); wload(wv, w_in_d[l, :, 2048 + pair * 128:2048 + (pair + 1) * 128].rearrange("(k p) n -> p k n", p=128))
            for j in range(NT):
                ps, pd = r_proj.next()
                for sub in range(4):
                    tk = j * 4 + sub
                    mm_group(ps[:, sub * 128:(sub + 1) * 128], pd, [(hT_rhs(k, tk * 128, 128), wv.v[:, k, :]) for k in range(8)],
                             wv.dall() + hT_deps(tk * 128, 128))
                pv4 = ps[:, :].rearrange("p (a b) -> p a b", a=4)
                P.op("dve", lambda h, pv4=pv4, j=j: h.tensor_copy(out=vt.v[:, j * 4:(j + 1) * 4, 0, 0:64], in_=pv4[:, :, 0:64]),
                     reads=pd, writes=vt.d2s(range(j * 4, j * 4 + 4), 0, 256))
                P.op("dve", lambda h, pv4=pv4, j=j: h.tensor_copy(out=vt.v[:, j * 4:(j + 1) * 4, 1, 64:128], in_=pv4[:, :, 64:128]),
                     reads=pd, writes=vt.d2s(range(j * 4, j * 4 + 4), 0, 256))

        def mask_rows(pair):
            s_ = pair % NSET
            qa = QA[s_]
            for qb in range(4, 8):
                for sub in range(2):
                    qs = qb * 256 + sub * 128
                    st_ = stage[qb - 4][sub]
                    tps, tpd = r_sc.next()
                    tpb = tps[:, :].bitcast(BF16)
                    for hh in range(2):
                        P.op("pe", lambda h, tpb=tpb, st_=st_, hh=hh: h.transpose(tpb[:, hh * 128:(hh + 1) * 128], st_.v[:, hh, :], identb.v),
                             reads=st_.dall() + identb.dall(), writes=tpd, inc=(hh == 1))
                    for hh in range(2):
                        P.op("act", lambda h, tpb=tpb, hh=hh, qs=qs: h.copy(out=qa[hh].v[64:72, qs:qs + 128], in_=tpb[64:72, hh * 128:(hh + 1) * 128]),
                             reads=tpd, writes=qa[hh].d(qs, qs + 128))

        def attention(pair, qbs):
            s_ = pair % NSET
            ka, qa, vt = KA[s_], QA[s_], VT
            units = []
            for qb in qbs:
                q0 = qb * 256
                for hh in range(2):
                    ai = state["acc"]; state["acc"] += 1
                    ops, opd = r_acc.next()
                    ds_ = dsh[ai % 2]
                    nun = qb + 1
                    for ui in range(nun):
                        units.append(dict(qb=qb, q0=q0, hh=hh, ui=ui, nun=nun, ops=ops, opd=opd, ds=ds_))
            for u in units:
                u["p"] = PT[state["pti"] % NPT]; state["pti"] += 1
                u["sps"], u["spd"] = None, None

            def emit_qk(u):
                sps, spd = r_sc.next()
                u["sps"], u["spd"] = sps, spd
                hh, q0, qb = u["hh"], u["q0"], u["qb"]
                if u["ui"] == 0:
                    k0 = 2 * qb
                    P.op("pe", lambda h: h.matmul(sps[:, 0:256], ka[hh].v[0:76, k0 * 128:(k0 + 1) * 128], qa[hh].v[0:76, q0:q0 + 256], start=True, stop=False),
                         reads=ka[hh].d(k0 * 128, (k0 + 1) * 128) + qa[hh].d(q0, q0 + 256) + trib.dall() + identb.dall(), writes=spd, inc=False)
                    P.op("pe", lambda h: h.matmul(sps[:, 0:128], identb.v, trib.v, start=False, stop=True), writes=spd, inc=False)
                    P.op("pe", lambda h: h.matmul(sps[:, 384:512], ka[hh].v[0:76, (k0 + 1) * 128:(k0 + 2) * 128], qa[hh].v[0:76, q0 + 128:q0 + 256], start=True, stop=False),
                         reads=ka[hh].d((k0 + 1) * 128, (k0 + 2) * 128) + qa[hh].d(q0, q0 + 256), writes=spd, inc=False)
                    P.op("pe", lambda h: h.matmul(sps[:, 384:512], identb.v, trib.v, start=False, stop=True), writes=spd)
                else:
                    kp = u["ui"] - 1
                    for i2 in range(2):
                        kt = 2 * kp + i2
                        P.op("pe", lambda h, kt=kt, i2=i2: h.matmul(sps[:, i2 * 256:(i2 + 1) * 256], ka[hh].v[0:76, kt * 128:(kt + 1) * 128],
                                                                  qa[hh].v[0:76, q0:q0 + 256], start=True, stop=True),
                             reads=ka[hh].d(kt * 128, (kt + 1) * 128) + qa[hh].d(q0, q0 + 256), writes=spd, inc=(i2 == 1))

            def emit_rest(u):
                sps, spd, p_ = u["sps"], u["spd"], u["p"]
                hh, q0, qb, ops, opd = u["hh"], u["q0"], u["qb"], u["ops"], u["opd"]
                last = (u["ui"] == u["nun"] - 1)
                if u["ui"] == 0:
                    k0 = 2 * qb
                    P.op("act", lambda h: h.activation(out=p_.v[:, 0, :], in_=sps[:, 0:256], func=AF.Exp), reads=spd, writes=p_.dall())
                    P.op("act", lambda h: h.activation(out=p_.v[:, 1, 128:256], in_=sps[:, 384:512], func=AF.Exp), reads=spd, writes=p_.dall())
                    P.op("pe", lambda h: h.matmul(ops[:, 0:256], vt.v[:, k0, hh, :], p_.v[:, 0, :], start=True, stop=False),
                         reads=vt.d2(k0, 0, 256) + p_.dall(), writes=opd, inc=False)
                    P.op("pe", lambda h: h.matmul(ops[:, 128:256], vt.v[:, k0 + 1, hh, :], p_.v[:, 1, 128:256], start=False, stop=last),
                         reads=vt.d2(k0 + 1, 0, 256) + p_.dall(), writes=opd, inc=True)
                else:
                    kp = u["ui"] - 1
                    P.op("act", lambda h: h.activation(out=p_.v.rearrange("p a b -> p (a b)"), in_=sps[:, :], func=AF.Exp),
                         reads=spd, writes=p_.dall())
                    for i2 in range(2):
                        kt = 2 * kp + i2
                        P.op("pe", lambda h, kt=kt, i2=i2: h.matmul(ops[:, 0:256], vt.v[:, kt, hh, :], p_.v[:, i2, :], start=False, stop=(last and i2 == 1)),
                             reads=vt.d2(kt, 0, 256) + p_.dall(), writes=opd, inc=(i2 == 1))
                if last:
                    ds_ = u["ds"]
                    olo, dlo = (0, 64) if hh == 0 else (64, 0)
                    P.op("act", lambda h: h.activation(out=ds_.v[olo:olo + 64, :], in_=ops[dlo:dlo + 64, 0:256], func=AF.Ln), reads=opd, writes=ds_.dall())
                    P.op("act", lambda h: h.activation(out=ds_.v[olo:olo + 64, :], in_=ds_.v[olo:olo + 64, :], func=AF.Exp, scale=-1.0), reads=ds_.dall(), writes=ds_.dall())
                    P.op("dve", lambda h: h.tensor_tensor(yB.v[olo:olo + 64, pair, q0:q0 + 256], ops[olo:olo + 64, 0:256], ds_.v[olo:olo + 64, :], op=ALU.mult),
                         reads=opd + ds_.dall(), writes=yB.d2(pair, q0, q0 + 256))

            for i, u in enumerate(units):
                if i == 0:
                    emit_qk(u)
                if i + 1 < len(units):
                    emit_qk(units[i + 1])
                emit_rest(u)

        project(0)
        for pair in range(4):
            project_v(pair)
            attention(pair, range(0, 4))
            mask_rows(pair)
            if pair + 1 < 4:
                project(pair + 1)
            attention(pair, range(4, 8))
        AR.release(m0)

    def merge_phase(l, yA, yB, yC):
        m0 = AR.mark()
        mg = AR.alloc([8, 1024], BF16)
        NGT = 2 if (AR.nbytes - AR.top) >= 2 * 3 * 2048 + 1024 else 1
        gt = [[AR.alloc([TT], F32) for _ in range(3)] for _ in range(NGT)]
        gi = 0
        ys = [yA, yB, yC]
        gview = w_in_d[l, :, 3072:6144].rearrange("(k p) (n d) -> p k n d", p=128, n=3)
        bview = w_br_d[l].rearrange("n (k p) d -> p k n d", p=128)
        for hf in range(2):
            for dc in range(8):
                G = ring_alloc([8, 3, 128])
                B = ring_alloc([4, 3, 128])
                for n in range(3):
                    P.dma("pool", G.v[:, :, n, :], gview[:, :, n, dc * 128:(dc + 1) * 128], writes=G.dall())
                    P.dma("pool", B.v[:, :, n, :], bview[:, :, n, dc * 128:(dc + 1) * 128], writes=B.dall())
                for jj in range(2):
                    t0 = hf * 1024 + jj * TT
                    gts = gt[gi % NGT]; gi += 1
                    for n in range(3):
                        ps, pd = R_MM.next()
                        mm_group(ps[:, :], pd, [(G.v[:, k, n, :], hT_rhs(k, t0, TT)) for k in range(8)],
                                 G.dall() + hT_deps(t0, TT))
                        P.op("act", lambda h, ps=ps, n=n, gts=gts: h.activation(out=gts[n].v, in_=ps[:, :], func=AF.Sigmoid), reads=pd, writes=gts[n].dall())
                        ps2, pd2 = R_MM.next()
                        mm_group(ps2[:, :], pd2, [(B.v[:, k, n, :], ys[n].v[:, k, t0:t0 + TT]) for k in range(4)],
                                 B.dall() + ys[n].d2s(range(4), t0, t0 + TT))
                        P.op("dve", lambda h, ps2=ps2, n=n, gts=gts: h.tensor_tensor(gts[n].v, ps2[:, :], gts[n].v, op=ALU.mult),
                             reads=pd2 + gts[n].dall(), writes=gts[n].dall())
                    P.op("dve", lambda h, gts=gts: h.tensor_tensor(gts[0].v, gts[0].v, gts[1].v, op=ALU.add),
                         reads=gts[0].dall() + gts[1].dall(), writes=gts[0].dall())
                    P.op("dve", lambda h, dc=dc, jj=jj, gts=gts: h.tensor_tensor(mg.v[:, dc, jj * TT:(jj + 1) * TT], gts[0].v, gts[2].v, op=ALU.add),
                         reads=gts[0].dall() + gts[2].dall(), writes=mg.d2(dc, jj * TT, (jj + 1) * TT))
            for pp in range(2):
                Wo = ring_alloc([8, 512])
                wload(Wo, w_out_d[l, :, pp * 512:(pp + 1) * 512].rearrange("(k p) n -> p k n", p=128))
                for jj in range(2):
                    t0 = hf * 1024 + jj * TT
                    for dci in range(4):
                        dc = pp * 4 + dci
                        ps, pd = R_MM.next()
                        mm_group(ps[:, :], pd, [(Wo.v[:, k, dci * 128:(dci + 1) * 128], mg.v[:, k, jj * TT:(jj + 1) * TT]) for k in range(8)],
                                 Wo.dall() + mg.d2s(range(8), jj * TT, (jj + 1) * TT))
                        P.op("dve", lambda h, ps=ps, dc=dc, t0=t0: h.tensor_tensor(xT.v[:, dc, t0:t0 + TT], ps[:, :], xT.v[:, dc, t0:t0 + TT], op=ALU.add),
                             reads=pd + xT.d2(dc, t0, t0 + TT), writes=xT.d2(dc, t0, t0 + TT))
        AR.release(m0)

    def ffn_phase(l):
        m0 = AR.mark()
        act = AR.alloc([24, 1024], BF16)
        gbuf = [AR.alloc([2 + 1024], F32) for _ in range(2)]
        acc = [AR.alloc([TT], F32) for _ in range(2)]
        gel = [AR.alloc([TT], F32) for _ in range(2)]
        it = 0
        for hf in range(2):
            for ffg in range(12):
                Wg = ring_alloc([8, 256]); wload(Wg, w_fg_d[l, :, ffg * 256:(ffg + 1) * 256].rearrange("(k p) n -> p k n", p=128))
                Wu = ring_alloc([8, 256]); wload(Wu, w_fu_d[l, :, ffg * 256:(ffg + 1) * 256].rearrange("(k p) n -> p k n", p=128))
                for fci in range(2):
                    fc = ffg * 2 + fci
                    gb = gbuf[fc % 2]
                    if hf == 0:
                        P.op("dve", lambda h, gb=gb: h.memset(gb.v[:, 0:2], 0.0), writes=gb.d(0, 2))
                    else:
                        P.op("dve", lambda h, gb=gb, fc=fc: h.tensor_copy(out=gb.v[:, 0:2], in_=gtail.v[:, fc, :]), reads=gtail.dall(), writes=gb.d(0, 2))
                    for jj in range(2):
                        t0 = hf * 1024 + jj * TT
                        g0 = 2 + jj * TT
                        ps, pd = R_MM.next()
                        mm_group(ps[:, :], pd, [(Wg.v[:, k, fci * 128:(fci + 1) * 128], hT_rhs(k, t0, TT)) for k in range(8)],
                                 Wg.dall() + hT_deps(t0, TT))
                        ups, upd = R_MM.next()
                        mm_group(ups[:, :], upd, [(Wu.v[:, k, fci * 128:(fci + 1) * 128], hT_rhs(k, t0, TT)) for k in range(8)],
                                 Wu.dall() + hT_deps(t0, TT))
                        P.op("act", lambda h, ps=ps, gb=gb, g0=g0: h.copy(out=gb.v[:, g0:g0 + TT], in_=ps[:, :]), reads=pd, writes=gb.d(g0, g0 + TT))
                        a_ = acc[it % 2]; ge = gel[it % 2]; it += 1
                        cw = lambda k, fc=fc: pv.v[:, l, PV_FCW + k * 24 + fc:PV_FCW + k * 24 + fc + 1]
                        cb = pv.v[:, l, PV_FCB + fc:PV_FCB + fc + 1]
                        gd = gb.d(g0 - 2, g0 + TT) + pv.dall()
                        P.op("dve", lambda h, a_=a_, gb=gb, g0=g0, cw=cw, cb=cb: h.tensor_scalar(a_.v, gb.v[:, g0:g0 + TT], cw(2), cb, op0=ALU.mult, op1=ALU.add),
                             reads=gd, writes=a_.dall())
                        P.op("dve", lambda h, a_=a_, gb=gb, g0=g0, cw=cw: h.scalar_tensor_tensor(out=a_.v, in0=gb.v[:, g0 - 1:g0 - 1 + TT], scalar=cw(1), in1=a_.v,
                                                                                               op0=ALU.mult, op1=ALU.add),
                             reads=gd + a_.dall(), writes=a_.dall())
                        P.op("dve", lambda h, a_=a_, gb=gb, g0=g0, cw=cw: h.scalar_tensor_tensor(out=a_.v, in0=gb.v[:, g0 - 2:g0 - 2 + TT], scalar=cw(0), in1=a_.v,
                                                                                               op0=ALU.mult, op1=ALU.add),
                             reads=gd + a_.dall(), writes=a_.dall())
                        P.op("act", lambda h, a_=a_, ge=ge: h.activation(out=ge.v, in_=a_.v, func=GELU), reads=a_.dall(), writes=ge.dall())
                        P.op("dve", lambda h, ups=ups, ge=ge, fc=fc, jj=jj: h.tensor_tensor(act.v[:, fc, jj * TT:(jj + 1) * TT], ups[:, :], ge.v, op=ALU.mult),
                             reads=upd + ge.dall(), writes=act.d2(fc, jj * TT, (jj + 1) * TT))
                    if hf == 0:
                        P.op("dve", lambda h, gb=gb, fc=fc: h.tensor_copy(out=gtail.v[:, fc, :], in_=gb.v[:, 1024:1026]), reads=gb.d(1024, 1026), writes=gtail.dall())
            for dcg in range(4):
                Wd = ring_alloc([24, 256]); wload(Wd, w_fd_d[l, :, dcg * 256:(dcg + 1) * 256].rearrange("(k p) n -> p k n", p=128))
                for jj in range(2):
                    t0 = hf * 1024 + jj * TT
                    for dci in range(2):
                        dc = dcg * 2 + dci
                        ps, pd = R_MM.next()
                        mm_group(ps[:, :], pd, [(Wd.v[:, k, dci * 128:(dci + 1) * 128], act.v[:, k, jj * TT:(jj + 1) * TT]) for k in range(24)],
                                 Wd.dall() + act.d2s(range(24), jj * TT, (jj + 1) * TT))
                        P.op("dve", lambda h, ps=ps, dc=dc, t0=t0: h.tensor_tensor(xT.v[:, dc, t0:t0 + TT], ps[:, :], xT.v[:, dc, t0:t0 + TT], op=ALU.add),
                             reads=pd + xT.d2(dc, t0, t0 + TT), writes=xT.d2(dc, t0, t0 + TT))
        AR.release(m0)

    for b in range(n_seq):
        mark('load'); load_x(b)
        for l in range(n_layers):
            mark('norm1'); rmsnorm_to_hT(PV_MIXG, l)
            mY = AR.mark()
            yB = AR.alloc([4, S], BF16)
            mark('moba'); moba_phase(l, yB)
            yA = AR.alloc([4, S], BF16)
            mark('lru'); lru_phase(l, yA)
            yC = AR.alloc([4, S], BF16)
            mark('xattn'); xattn_phase(l, b, yC)
            if debug and b == 0:
                for nm, y_ in (("yA%d" % l, yA), ("yB%d" % l, yB), ("yC%d" % l, yC)):
                    if nm in dbg_d:
                        P.dma("pool", dbg_d[nm].rearrange("(c p) t -> p c t", p=128), y_.v, reads=y_.dall())
            mark('merge'); merge_phase(l, yA, yB, yC)
            AR.release(mY)
            if debug and b == 0 and ("xmid%d" % l) in dbg_d:
                P.dma("sp", dbg_d["xmid%d" % l].rearrange("(c p) t -> p c t", p=128), xT.v, reads=xT.dall())
            mark('norm2'); rmsnorm_to_hT(PV_FFNG, l)
            mark('ffn'); ffn_phase(l)
            if debug and b == 0 and ("xout%d" % l) in dbg_d:
                P.dma("sp", dbg_d["xout%d" % l].rearrange("(c p) t -> p c t", p=128), xT.v, reads=xT.dall())
        mark('final'); final_store(b)
    mark('end')
    P.finish()
    stack.close()
    return nc, P


def host_consts():
    t = np.arange(S)
    c = {}
    c["c_ident"] = np.eye(128, dtype=np.float32)
    k = np.arange(128)
    c["c_tri"] = np.where(k[None, :] >= k[:, None], 0.0, -30000.0).astype(np.float32)
    base = np.stack([t % 256, 256 * (t // 256), np.ones(S), np.ones(S)]).astype(np.float32)
    c["c_krow"] = np.stack([base * (2.0 ** (-(hd + 1))) for hd in range(8)]).astype(np.float32)
    c["c_qrow"] = np.stack([np.ones(S), np.ones(S), -(t % 256), -256 * (t // 256)]).astype(np.float32)
    c["c_onehot"] = (np.arange(8)[:, None] == (t // 256)[None, :]).astype(np.float32)
    past = np.zeros((128, 4, 16), np.float32)
    for q4 in range(4):
        qb = q4 + 4
        for hh in range(2):
            past[:, q4, hh * 8 + qb:hh * 8 + 8] = -1e30
    c["c_past"] = past
    return c


def host_params(inp):
    pvec = np.zeros((DEPTH, 128, PV_N), np.float32)
    wabd = np.zeros((DEPTH, 2, 128, 4, 128), np.float32)
    for l in range(DEPTH):
        pvec[l, :, PV_MIXG:PV_MIXG + 8] = _chunked(inp["mix_norm_gain"][l])
        pvec[l, :, PV_FFNG:PV_FFNG + 8] = _chunked(inp["ffn_norm_gain"][l])
        pvec[l, :, PV_MEMG:PV_MEMG + 8] = _chunked(inp["mem_norm_gain"][l])
        pvec[l, :, PV_FING:PV_FING + 8] = _chunked(inp["final_norm_gain"])
        for k in range(4):
            pvec[l, :, PV_LCW + k * 4:PV_LCW + k * 4 + 4] = _chunked(inp["lru_conv_w"][l, k])
        pvec[l, :, PV_LCB:PV_LCB + 4] = _chunked(inp["lru_conv_b"][l])
        pvec[l, :, PV_LBA:PV_LBA + 4] = _chunked(inp["lru_b_a"][l].reshape(-1))
        pvec[l, :, PV_LBX:PV_LBX + 4] = _chunked(inp["lru_b_x"][l].reshape(-1))
        pvec[l, :, PV_LAM:PV_LAM + 4] = _chunked(inp["lru_lambda"][l])
        for k in range(3):
            pvec[l, :, PV_FCW + k * 24:PV_FCW + k * 24 + 24] = _chunked(inp["ffn_conv_w"][l, k])
        pvec[l, :, PV_FCB:PV_FCB + 24] = _chunked(inp["ffn_conv_b"][l])
        for g, nm in enumerate(("lru_w_a", "lru_w_x")):
            w = inp[nm][l]
            for hd in range(8):
                c, o = hd // 2, (hd % 2) * 64
                wabd[l, g, o:o + 64, c, o:o + 64] = w[hd]
    return pvec, wabd


_CACHE = {}


def kernel(**inputs):
    inp = {k: np.asarray(v) for k, v in inputs.items()}
    if "nc" not in _CACHE:
        _CACHE["nc"] = build_program()[0]
    nc = _CACHE["nc"]
    pvec, wabd = host_params(inp)
    consts = host_consts()
    shared = dict(consts)
    shared.update(pvec=pvec, wabd=wabd)
    for k in ("w_in", "w_mem_kv", "w_branch", "w_out", "w_ffn_gate", "w_ffn_up", "w_ffn_down"):
        shared[k] = np.ascontiguousarray(inp[k], dtype=np.float32)
    in_maps = []
    for c in range(NCORES):
        m = dict(shared)
        m["x"] = np.ascontiguousarray(inp["x"][c * SEQ_PER_CORE:(c + 1) * SEQ_PER_CORE], dtype=np.float32)
        m["mem"] = np.ascontiguousarray(inp["mem"][c * SEQ_PER_CORE:(c + 1) * SEQ_PER_CORE], dtype=np.float32)
        in_maps.append(m)
    res = run_bass_kernel_spmd(nc, in_maps, core_ids=list(range(NCORES)))
    out = np.concatenate([np.asarray(r["out"]) for r in res.results], axis=0)
    return out.astype(np.float32)
```

```python
import math
from contextlib import ExitStack
import numpy as np
import concourse.bass as bass
import concourse.mybir as mybir
from concourse.bass_utils import run_bass_kernel_spmd

F32 = mybir.dt.float32
BF16 = mybir.dt.bfloat16
U8 = mybir.dt.uint8
AF = mybir.ActivationFunctionType
ALU = mybir.AluOpType
AX = mybir.AxisListType

NCORES = 8
SEQ_PER_CORE = 2
S = 2048
D = 1024
DEPTH = 2
NT = 4
TT = 512
GELU = AF.Gelu_apprx_tanh

ENGS = ["pe", "act", "dve", "pool", "sp"]
NDMA = 24
GRAN = 256


class Dep:
    __slots__ = ("w", "rs")

    def __init__(self):
        self.w = {}
        self.rs = {}


class _Rec:
    def __getattr__(self, name):
        def f(*a, **k):
            self.call = (name, a, k)
            return self
        return f


class Prog:
    def __init__(self, nc, stack):
        self.nc = nc
        self.streams = {e: [] for e in ENGS}
        self.cnt = {e: 0 for e in ENGS}
        self.seen = {e: {} for e in ENGS}
        self.sem = {}
        for e in ENGS:
            self.sem[e] = stack.enter_context(nc.semaphore("s_" + e))
        self.dsem = []
        self.dtot = []
        for i in range(NDMA):
            self.dsem.append(stack.enter_context(nc.semaphore("d_%d" % i)))
            self.dtot.append(0)
            self.sem[("dma", i)] = self.dsem[i]
        self.dnext = 0
        self.ninst = 0

    def _need(self, eng, reads, writes):
        need = {}
        for d in reads:
            for k, v in d.w.items():
                if need.get(k, 0) < v:
                    need[k] = v
        for d in writes:
            for k, v in d.w.items():
                if need.get(k, 0) < v:
                    need[k] = v
            for k, v in d.rs.items():
                if need.get(k, 0) < v:
                    need[k] = v
        out = []
        seen = self.seen[eng]
        raw_self = 0
        if eng in ("act", "dve", "pool"):
            for d in reads:
                v = d.w.get(eng, 0)
                if v > raw_self:
                    raw_self = v
        for k, v in need.items():
            if k == eng:
                if raw_self == 0:
                    continue
                v = raw_self
            if seen.get(k, 0) >= v:
                continue
            seen[k] = v
            out.append((self.sem[k], v))
        return out

    @staticmethod
    def _mark(key, val, reads, writes):
        for d in reads:
            if d.rs.get(key, 0) < val:
                d.rs[key] = val
        for d in writes:
            if d.w.get(key, 0) < val:
                d.w[key] = val

    def op(self, eng, fn, reads=(), writes=(), inc=True):
        waits = self._need(eng, reads, writes)
        if inc:
            self.cnt[eng] += 1
            val = self.cnt[eng]
        else:
            val = self.cnt[eng] + 1
        self._mark(eng, val, reads, writes)
        sem = self.sem[eng]
        self.ninst += 1
        rec = _Rec()
        fn(rec)
        cname, cargs, ckw = rec.call

        def thunk(h):
            for s, v in waits[:-1]:
                h.wait_ge(s, v)
            inst = getattr(h, cname)(*cargs, **ckw)
            if waits:
                inst._wait_ge(*waits[-1])
            if inc:
                inst.then_inc(sem, 1)
        self.streams[eng].append(thunk)

    def dma(self, eng, out, in_, reads=(), writes=(), **kw):
        k = self.dnext
        self.dnext = (self.dnext + 1) % NDMA
        key = ("dma", k)
        waits = self._need(eng, reads, writes)
        prev = self.dtot[k]
        if prev > 0 and self.seen[eng].get(key, 0) < prev:
            self.seen[eng][key] = prev
            waits.append((self.dsem[k], prev))
        self.dtot[k] += 16
        val = self.dtot[k]
        self._mark(key, val, reads, writes)
        sem = self.dsem[k]
        self.ninst += 1

        def thunk(h):
            for s, v in waits:
                h.wait_ge(s, v)
            h.dma_start(out=out, in_=in_, **kw).then_inc(sem, 16)
        self.streams[eng].append(thunk)

    def finish(self):
        waits = [(self.dsem[k], self.dtot[k]) for k in range(NDMA) if self.dtot[k] > 0]
        others = [(self.sem[e], self.cnt[e]) for e in ENGS if e != "sp" and self.cnt[e] > 0]

        def fthunk(h):
            for s, v in waits + others:
                h.wait_ge(s, v)
        self.streams["sp"].append(fthunk)
        nc = self.nc
        st = self.streams
        with nc.Block() as block:
            @block.tensor
            def _(h):
                for t in st["pe"]:
                    t(h)

            @block.scalar
            def _(h):
                for t in st["act"]:
                    t(h)

            @block.vector
            def _(h):
                for t in st["dve"]:
                    t(h)

            @block.gpsimd
            def _(h):
                for t in st["pool"]:
                    t(h)

            @block.sync
            def _(h):
                for t in st["sp"]:
                    t(h)


class SBT:
    def __init__(self, arena, gran, off, shape, dt):
        self.esz = 4 if dt == F32 else 2
        self.off = off
        self.shape = shape
        self.dt = dt
        n = 1
        for s_ in shape:
            n *= s_
        self.nbytes = n * self.esz
        v = arena[:, off:off + self.nbytes].bitcast(dt)
        if len(shape) == 2:
            v = v.rearrange("p (a b) -> p a b", a=shape[0])
        elif len(shape) == 3:
            v = v.rearrange("p (a b c) -> p a b c", a=shape[0], b=shape[1])
        self.v = v
        self.gran = gran

    def dall(self):
        return self.gran[self.off // GRAN:(self.off + self.nbytes + GRAN - 1) // GRAN]

    def d(self, lo, hi):
        a = self.off + lo * self.esz
        b = self.off + hi * self.esz
        return self.gran[a // GRAN:(b + GRAN - 1) // GRAN]

    def d2(self, i0, lo, hi):
        n1 = self.shape[-1] if len(self.shape) == 2 else self.shape[1] * self.shape[2]
        return self.d(i0 * n1 + lo, i0 * n1 + hi)

    def d2s(self, i0s, lo, hi):
        out = []
        for i0 in i0s:
            out += self.d2(i0, lo, hi)
        return out

    def d3(self, i0, i1, lo, hi):
        n2 = self.shape[2]
        base = (i0 * self.shape[1] + i1) * n2
        return self.d(base + lo, base + hi)


class Arena:
    def __init__(self, nc, nbytes):
        self.t = nc.alloc_sbuf_tensor("arena", [128, nbytes], U8)
        self.nbytes = nbytes
        self.gran = [Dep() for _ in range((nbytes + GRAN - 1) // GRAN + 1)]
        self.top = 0

    def alloc(self, shape, dt):
        off = (self.top + GRAN - 1) // GRAN * GRAN
        n = 4 if dt == F32 else 2
        for s_ in shape:
            n *= s_
        assert off + n <= self.nbytes, ("SBUF arena overflow", off, n, self.nbytes)
        b = SBT(self.t, self.gran, off, list(shape), dt)
        self.top = off + b.nbytes
        self.peak = max(getattr(self, "peak", 0), self.top)
        return b

    def mark(self):
        return self.top

    def release(self, m):
        self.top = m


PV_MIXG, PV_FFNG, PV_MEMG, PV_FING = 0, 8, 16, 24
PV_LCW, PV_LCB, PV_LBA, PV_LBX, PV_LAM = 32, 48, 52, 56, 60
PV_FCW, PV_FCB = 64, 136
PV_N = 160


def _chunked(v):
    return np.ascontiguousarray(v.reshape(-1, 128).T)


def build_program(n_layers=DEPTH, n_seq=SEQ_PER_CORE, debug=None):
    nc = bass.Bass("TRN2", target_bir_lowering=False)
    dram = {}

    def din(name, shape):
        dram[name] = nc.dram_tensor(name, list(shape), F32, kind="ExternalInput").ap()
        return dram[name]

    x_d = din("x", [SEQ_PER_CORE, S, D])
    mem_d = din("mem", [SEQ_PER_CORE, 256, D])
    w_in_d = din("w_in", [DEPTH, D, 6144])
    w_mkv_d = din("w_mem_kv", [DEPTH, D, 1024])
    w_br_d = din("w_branch", [DEPTH, 3, 512, D])
    w_out_d = din("w_out", [DEPTH, D, D])
    w_fg_d = din("w_ffn_gate", [DEPTH, D, 3072])
    w_fu_d = din("w_ffn_up", [DEPTH, D, 3072])
    w_fd_d = din("w_ffn_down", [DEPTH, 3072, D])
    pvec_d = din("pvec", [DEPTH, 128, PV_N])
    wabd_d = din("wabd", [DEPTH, 2, 128, 4, 128])
    cident_d = din("c_ident", [128, 128])
    ctri_d = din("c_tri", [128, 128])
    ckrow_d = din("c_krow", [8, 4, S])
    cqrow_d = din("c_qrow", [4, S])
    coneh_d = din("c_onehot", [8, S])
    cpast_d = din("c_past", [128, 4, 16])
    out_d = nc.dram_tensor("out", [SEQ_PER_CORE, S, D], F32, kind="ExternalOutput").ap()
    dbg_d = {}
    if debug:
        for name, shape in debug.items():
            dbg_d[name] = nc.dram_tensor("dbg_" + name, list(shape), F32, kind="ExternalOutput").ap()

    stack = ExitStack()
    P = Prog(nc, stack)
    total = nc.sbuf_top - nc.sbuf_base - 64
    AR = Arena(nc, total // GRAN * GRAN - GRAN)

    psb = [nc.alloc_psum_tensor("ps%d" % i, [128, 512], F32) for i in range(8)]
    psd = [Dep() for _ in range(8)]

    class Ring:
        def __init__(self, banks):
            self.banks = banks
            self.i = 0

        def next(self):
            b = self.banks[self.i % len(self.banks)]
            self.i += 1
            return psb[b], [psd[b]]

    xT = AR.alloc([8, S], F32)
    hT = AR.alloc([8, S], BF16)
    pv = AR.alloc([DEPTH, PV_N], F32)
    identf = AR.alloc([128], F32)
    identb = AR.alloc([128], BF16)
    onesf = AR.alloc([128], F32)
    onesb = AR.alloc([128], BF16)
    trib = AR.alloc([128], BF16)
    pastm = AR.alloc([4, 16], F32)
    cst = AR.alloc([8], F32)
    lruc = AR.alloc([DEPTH, 8], F32)
    wabd = AR.alloc([2, 4 * 128], BF16)
    gtail = AR.alloc([24, 2], F32)
    RING_SLOTS = 8
    SLOT = 4096
    ring_off = (AR.top + GRAN - 1) // GRAN * GRAN
    AR.top = ring_off + RING_SLOTS * SLOT
    ring_state = {"i": 0}

    def ring_alloc(shape, dt=BF16):
        n = 1
        for s_ in shape:
            n *= s_
        nb = n * 2
        ns = (nb + SLOT - 1) // SLOT
        i = ring_state["i"]
        if i + ns > RING_SLOTS:
            i = 0
        ring_state["i"] = i + ns
        return SBT(AR.t, AR.gran, ring_off + i * SLOT, list(shape), dt)

    phase_base = AR.mark()

    def dbg(name, sbt_ap, deps, dram_ap=None):
        if debug and name in dbg_d:
            P.dma("sp", dbg_d[name] if dram_ap is None else dram_ap, sbt_ap, reads=deps)

    def mm_group(ps_ap, ps_deps, pairs, rdeps):
        n = len(pairs)
        for i, (l_ap, r_ap) in enumerate(pairs):
            P.op("pe", (lambda h, l_ap=l_ap, r_ap=r_ap, i=i: h.matmul(ps_ap, l_ap, r_ap, start=(i == 0), stop=(i == n - 1))),
                 reads=rdeps if i == 0 else (), writes=ps_deps, inc=(i == n - 1))

    def wload(dst, src_ap):
        P.dma("pool", dst.v, src_ap, writes=dst.dall())

    P.dma("sp", pv.v, pvec_d.rearrange("l p n -> p l n"), writes=pv.dall())
    P.dma("sp", identf.v, cident_d, writes=identf.dall())
    P.dma("pool", identb.v, cident_d, writes=identb.dall())
    P.dma("pool", trib.v, ctri_d, writes=trib.dall())
    P.dma("sp", pastm.v, cpast_d, writes=pastm.dall())
    P.op("dve", lambda h: h.memset(onesf.v, 1.0), writes=onesf.dall())
    P.op("dve", lambda h: h.memset(onesb.v, 1.0), writes=onesb.dall())
    P.op("dve", lambda h: h.memset(cst.v[:, 0:1], 1e-6), writes=cst.dall())
    P.op("dve", lambda h: h.memset(cst.v[:, 1:2], 1.0), writes=cst.dall())
    P.op("dve", lambda h: h.memset(gtail.v, 0.0), writes=gtail.dall())

    for l in range(n_layers):
        m0 = AR.mark()
        t_ = AR.alloc([4], F32); e_ = AR.alloc([4], F32); z_ = AR.alloc([4], F32)
        z2 = AR.alloc([4], F32); pl = AR.alloc([4], F32); ab = AR.alloc([4], F32)
        lam = pv.v[:, l, PV_LAM:PV_LAM + 4]
        dd = t_.dall() + e_.dall() + z_.dall() + z2.dall() + pl.dall() + ab.dall() + pv.dall() + lruc.dall()
        P.op("dve", lambda h, lam=lam: h.tensor_scalar(t_.v, lam, -1.0, None, op0=ALU.mult), dd, dd)
        P.op("dve", lambda h, lam=lam: h.tensor_tensor(ab.v, t_.v, lam, op=ALU.max), dd, dd)
        P.op("act", lambda h: h.activation(out=e_.v, in_=ab.v, func=AF.Exp, scale=-1.0), dd, dd)
        P.op("dve", lambda h: h.tensor_scalar(z_.v, e_.v, 2.0, None, op0=ALU.add), dd, dd)
        P.op("dve", lambda h: h.reciprocal(z_.v, z_.v), dd, dd)
        P.op("dve", lambda h: h.tensor_tensor(z_.v, z_.v, e_.v, op=ALU.mult), dd, dd)
        P.op("dve", lambda h: h.tensor_tensor(z2.v, z_.v, z_.v, op=ALU.mult), dd, dd)
        P.op("dve", lambda h: h.tensor_scalar(pl.v, z2.v, 1.0 / 13, 1.0 / 11, op0=ALU.mult, op1=ALU.add), dd, dd)
        for cf in (1.0 / 9, 1.0 / 7, 1.0 / 5, 1.0 / 3, 1.0):
            P.op("dve", lambda h: h.tensor_tensor(pl.v, pl.v, z2.v, op=ALU.mult), dd, dd)
            P.op("dve", lambda h, cf=cf: h.tensor_scalar(pl.v, pl.v, cf, None, op0=ALU.add), dd, dd)
        P.op("dve", lambda h: h.tensor_tensor(pl.v, pl.v, z_.v, op=ALU.mult), dd, dd)
        P.op("dve", lambda h: h.tensor_scalar(pl.v, pl.v, 2.0, None, op0=ALU.mult), dd, dd)
        P.op("dve", lambda h: h.tensor_scalar(t_.v, t_.v, 0.0, None, op0=ALU.max), dd, dd)
        P.op("dve", lambda h: h.tensor_tensor(pl.v, pl.v, t_.v, op=ALU.add), dd, dd)
        P.op("dve", lambda h, l=l: h.tensor_scalar(lruc.v[:, l, 0:4], pl.v, -8.0, None, op0=ALU.mult), dd, dd)
        P.op("dve", lambda h, l=l: h.tensor_scalar(lruc.v[:, l, 4:8], pl.v, -16.0, None, op0=ALU.mult), dd, dd)
        AR.release(m0)

    P.marks = []

    def mark(name):
        P.marks.append((name, len(P.streams['pe']), len(P.streams['act']), len(P.streams['dve'])))

    R_MM = Ring([0, 1, 2, 3])
    R_AUX = Ring([4, 5])
    R_ACC = Ring([6, 7])

    def rmsnorm_to_hT(gain_col, l):
        m0 = AR.mark()
        sq = [AR.alloc([TT], F32) for _ in range(2)]
        rs = [AR.alloc([TT], F32) for _ in range(2)]
        for j in range(NT):
            t0 = j * TT
            ps, pd = R_MM.next()
            for c in range(8):
                s_ = sq[c % 2]
                P.op("act", lambda h, s_=s_, c=c: h.activation(out=s_.v, in_=xT.v[:, c, t0:t0 + TT], func=AF.Square),
                     reads=xT.d2(c, t0, t0 + TT), writes=s_.dall())
                P.op("pe", lambda h, s_=s_, c=c, ps=ps: h.matmul(ps[:, :], onesf.v, s_.v, start=(c == 0), stop=(c == 7)),
                     reads=s_.dall() + onesf.dall(), writes=pd)
            r_ = rs[j % 2]
            P.op("act", lambda h, r_=r_, ps=ps: h.activation(out=r_.v, in_=ps[:, :], func=AF.Sqrt, scale=1.0 / D, bias=cst.v[:, 0:1]),
                 reads=pd + cst.dall(), writes=r_.dall())
            P.op("dve", lambda h, r_=r_: h.reciprocal(r_.v, r_.v), reads=r_.dall(), writes=r_.dall())
            for c in range(8):
                P.op("dve", lambda h, r_=r_, c=c: h.scalar_tensor_tensor(
                    out=hT.v[:, c, t0:t0 + TT], in0=xT.v[:, c, t0:t0 + TT], scalar=pv.v[:, l, gain_col + c:gain_col + c + 1],
                    in1=r_.v, op0=ALU.mult, op1=ALU.mult),
                    reads=xT.d2(c, t0, t0 + TT) + r_.dall() + pv.dall(), writes=hT.d2(c, t0, t0 + TT))
        AR.release(m0)

    def hT_rhs(k, t0, n):
        return hT.v[:, k, t0:t0 + n]

    def hT_deps(t0, n):
        return hT.d2s(range(8), t0, t0 + n)

    def load_x(b):
        m0 = AR.mark()
        stg = [AR.alloc([D], F32) for _ in range(2)]
        for tt in range(S // 128):
            s_ = stg[tt % 2]
            P.dma("sp", s_.v, x_d[b, tt * 128:(tt + 1) * 128, :], writes=s_.dall())
            for g in range(2):
                ps, pd = R_MM.next()
                for cc in range(4):
                    c = g * 4 + cc
                    P.op("pe", lambda h, s_=s_, c=c, cc=cc, ps=ps: h.transpose(ps[:, cc * 128:(cc + 1) * 128], s_.v[:, c * 128:(c + 1) * 128], identf.v),
                         reads=s_.dall() + identf.dall(), writes=pd, inc=(cc == 3))
                eng = "act" if g == 0 else "dve"
                outap = xT.v[:, g * 4:(g + 1) * 4, tt * 128:(tt + 1) * 128]
                inap = ps[:, :].rearrange("p (a b) -> p a b", a=4)
                wd = xT.d2s(range(g * 4, g * 4 + 4), tt * 128, (tt + 1) * 128)
                if eng == "act":
                    P.op("act", lambda h, outap=outap, inap=inap: h.copy(out=outap, in_=inap), reads=pd, writes=wd)
                else:
                    P.op("dve", lambda h, outap=outap, inap=inap: h.tensor_copy(out=outap, in_=inap), reads=pd, writes=wd)
        AR.release(m0)

    def final_store(b):
        m0 = AR.mark()
        sq = [AR.alloc([TT], F32) for _ in range(2)]
        rs = [AR.alloc([TT], F32) for _ in range(2)]
        nrm = [AR.alloc([8, TT], F32) for _ in range(2)]
        stg = [AR.alloc([D], F32) for _ in range(2)]
        si = 0
        for j in range(NT):
            t0 = j * TT
            ps, pd = R_MM.next()
            for c in range(8):
                s_ = sq[c % 2]
                P.op("act", lambda h, s_=s_, c=c: h.activation(out=s_.v, in_=xT.v[:, c, t0:t0 + TT], func=AF.Square),
                     reads=xT.d2(c, t0, t0 + TT), writes=s_.dall())
                P.op("pe", lambda h, s_=s_, c=c, ps=ps: h.matmul(ps[:, :], onesf.v, s_.v, start=(c == 0), stop=(c == 7)),
                     reads=s_.dall() + onesf.dall(), writes=pd)
            r_ = rs[j % 2]
            P.op("act", lambda h, r_=r_, ps=ps: h.activation(out=r_.v, in_=ps[:, :], func=AF.Sqrt, scale=1.0 / D, bias=cst.v[:, 0:1]),
                 reads=pd + cst.dall(), writes=r_.dall())
            P.op("dve", lambda h, r_=r_: h.reciprocal(r_.v, r_.v), reads=r_.dall(), writes=r_.dall())
            n_ = nrm[j % 2]
            for c in range(8):
                P.op("dve", lambda h, r_=r_, c=c, n_=n_: h.scalar_tensor_tensor(
                    out=n_.v[:, c, :], in0=xT.v[:, c, t0:t0 + TT], scalar=pv.v[:, 0, PV_FING + c:PV_FING + c + 1],
                    in1=r_.v, op0=ALU.mult, op1=ALU.mult),
                    reads=xT.d2(c, t0, t0 + TT) + r_.dall() + pv.dall(), writes=n_.d2(c, 0, TT))
            for sub in range(4):
                s_ = stg[si % 2]
                si += 1
                for g in range(2):
                    ps2, pd2 = R_MM.next()
                    for cc in range(4):
                        c = g * 4 + cc
                        P.op("pe", lambda h, n_=n_, c=c, cc=cc, ps2=ps2, sub=sub: h.transpose(
                            ps2[:, cc * 128:(cc + 1) * 128], n_.v[:, c, sub * 128:(sub + 1) * 128], identf.v),
                            reads=n_.d2(c, 0, TT) + identf.dall(), writes=pd2, inc=(cc == 3))
                    if g == 0:
                        P.op("act", lambda h, s_=s_, ps2=ps2: h.copy(out=s_.v[:, 0:512], in_=ps2[:, :]), reads=pd2, writes=s_.dall())
                    else:
                        P.op("dve", lambda h, s_=s_, ps2=ps2: h.tensor_copy(out=s_.v[:, 512:1024], in_=ps2[:, :]), reads=pd2, writes=s_.dall())
                P.dma("sp", out_d[b, t0 + sub * 128:t0 + (sub + 1) * 128, :], s_.v, reads=s_.dall())
        AR.release(m0)

    def lru_phase(l, yA):
        m0 = AR.mark()
        xat = [AR.alloc([4, 3 + TT], F32) for _ in range(2)]
        P.op("dve", lambda h: h.memset(xat[0].v[:, :, 0:3], 0.0), writes=xat[0].dall())
        P.dma("pool", wabd.v, wabd_d[l].rearrange("g p c o -> p g (c o)"), writes=wabd.dall())
        wga = ring_alloc([8, 512])
        wload(wga, w_in_d[l, :, 512:1024].rearrange("(k p) n -> p k n", p=128))
        wxa = ring_alloc([8, 512])
        wload(wxa, w_in_d[l, :, 0:512].rearrange("(k p) n -> p k n", p=128))
        for j in range(NT):
            t0 = j * TT
            for c in range(4):
                ps, pd = R_MM.next()
                mm_group(ps[:, :], pd, [(wga.v[:, k, c * 128:(c + 1) * 128], hT_rhs(k, t0, TT)) for k in range(8)],
                         wga.dall() + hT_deps(t0, TT))
                P.op("act", lambda h, ps=ps, c=c, t0=t0: h.activation(out=yA.v[:, c, t0:t0 + TT], in_=ps[:, :], func=GELU),
                     reads=pd, writes=yA.d2(c, t0, t0 + TT))
        NS = 2
        tmp = {}
        for nm in ("xc", "r", "i", "a", "m"):
            tmp[nm] = [AR.alloc([TT], F32) for _ in range(NS)]
        tmp["xb"] = [AR.alloc([TT], BF16) for _ in range(NS)]
        carry = AR.alloc([4], F32)
        r_gate = Ring([4, 5, 6, 7])
        iters = [(j, c) for j in range(NT) for c in range(4)]
        ctx = {}

        def head(idx):
            j, c = iters[idx]
            t0 = j * TT
            xa = xat[j % 2]
            xap = xat[(j - 1) % 2]
            q = idx % NS
            xc, xb = tmp["xc"][q], tmp["xb"][q]
            if j > 0:
                P.op("act", lambda h: h.copy(out=xa.v[:, c, 0:3], in_=xap.v[:, c, TT:TT + 3]),
                     reads=xap.d2(c, TT, TT + 3), writes=xa.d2(c, 0, 3))
            ps, pd = R_MM.next()
            mm_group(ps[:, :], pd, [(wxa.v[:, k, c * 128:(c + 1) * 128], hT_rhs(k, t0, TT)) for k in range(8)],
                     wxa.dall() + hT_deps(t0, TT))
            P.op("act", lambda h: h.copy(out=xa.v[:, c, 3:3 + TT], in_=ps[:, :]), reads=pd, writes=xa.d2(c, 3, 3 + TT))
            cw = lambda k: pv.v[:, l, PV_LCW + k * 4 + c:PV_LCW + k * 4 + c + 1]
            cb = pv.v[:, l, PV_LCB + c:PV_LCB + c + 1]
            xin = lambda k: xa.v[:, c, k:k + TT]
            xdeps = xa.d2(c, 0, TT + 3) + pv.dall()
            P.op("dve", lambda h: h.tensor_scalar(xc.v, xin(3), cw(3), cb, op0=ALU.mult, op1=ALU.add), reads=xdeps, writes=xc.dall())
            for k in range(3):
                P.op("dve", lambda h, k=k: h.scalar_tensor_tensor(out=xc.v, in0=xin(k), scalar=cw(k), in1=xc.v, op0=ALU.mult, op1=ALU.add),
                     reads=xdeps + xc.dall(), writes=xc.dall())
            P.op("dve", lambda h: h.tensor_copy(out=xb.v, in_=xc.v), reads=xc.dall(), writes=xb.dall())
            psa, pda = r_gate.next()
            P.op("pe", lambda h: h.matmul(psa[:, :], wabd.v[:, 0, c * 128:(c + 1) * 128], xb.v, start=True, stop=True),
                 reads=xb.dall() + wabd.dall(), writes=pda)
            psx, pdx = r_gate.next()
            P.op("pe", lambda h: h.matmul(psx[:, :], wabd.v[:, 1, c * 128:(c + 1) * 128], xb.v, start=True, stop=True),
                 reads=xb.dall() + wabd.dall(), writes=pdx)
            ctx[idx] = (psa, pda, psx, pdx)

        def tail(idx):
            j, c = iters[idx]
            t0 = j * TT
            q = idx % NS
            xc, r_, i_, a_, m_ = (tmp[n][q] for n in ("xc", "r", "i", "a", "m"))
            psa, pda, psx, pdx = ctx.pop(idx)
            P.op("act", lambda h: h.activation(out=r_.v, in_=psa[:, :], func=AF.Sigmoid, bias=pv.v[:, l, PV_LBA + c:PV_LBA + c + 1]),
                 reads=pda + pv.dall(), writes=r_.dall())
            P.op("act", lambda h: h.activation(out=i_.v, in_=psx[:, :], func=AF.Sigmoid, bias=pv.v[:, l, PV_LBX + c:PV_LBX + c + 1]),
                 reads=pdx + pv.dall(), writes=i_.dall())
            P.op("act", lambda h: h.activation(out=a_.v, in_=r_.v, func=AF.Exp, scale=lruc.v[:, l, c:c + 1]),
                 reads=r_.dall() + lruc.dall(), writes=a_.dall())
            P.op("act", lambda h: h.activation(out=m_.v, in_=r_.v, func=AF.Exp, scale=lruc.v[:, l, 4 + c:5 + c]),
                 reads=r_.dall() + lruc.dall(), writes=m_.dall())
            P.op("act", lambda h: h.activation(out=m_.v, in_=m_.v, func=AF.Ln, scale=-1.0, bias=cst.v[:, 1:2]),
                 reads=m_.dall() + cst.dall(), writes=m_.dall())
            P.op("act", lambda h: h.activation(out=m_.v, in_=m_.v, func=AF.Exp, scale=0.5), reads=m_.dall(), writes=m_.dall())
            P.op("dve", lambda h: h.tensor_tensor(i_.v, i_.v, xc.v, op=ALU.mult), reads=i_.dall() + xc.dall(), writes=i_.dall())
            P.op("dve", lambda h: h.tensor_tensor(i_.v, i_.v, m_.v, op=ALU.mult), reads=i_.dall() + m_.dall(), writes=i_.dall())
            hcur = r_
            if j == 0:
                P.op("dve", lambda h: h.tensor_tensor_scan(hcur.v, a_.v, i_.v, 0.0, ALU.mult, ALU.add),
                     reads=a_.dall() + i_.dall(), writes=hcur.dall())
            else:
                P.op("dve", lambda h: h.tensor_tensor_scan(hcur.v, a_.v, i_.v, carry.v[:, c:c + 1], ALU.mult, ALU.add),
                     reads=a_.dall() + i_.dall() + carry.dall(), writes=hcur.dall())
            P.op("dve", lambda h: h.tensor_copy(out=carry.v[:, c:c + 1], in_=hcur.v[:, TT - 1:TT]), reads=hcur.dall(), writes=carry.dall())
            P.op("dve", lambda h: h.tensor_tensor(yA.v[:, c, t0:t0 + TT], yA.v[:, c, t0:t0 + TT], hcur.v, op=ALU.mult),
                 reads=hcur.dall() + yA.d2(c, t0, t0 + TT), writes=yA.d2(c, t0, t0 + TT))

        for idx in range(len(iters)):
            if idx == 0:
                head(0)
            if idx + 1 < len(iters):
                head(idx + 1)
            tail(idx)
        AR.release(m0)

    def xattn_phase(l, b, yC):
        m0 = AR.mark()
        memK = AR.alloc([4, 256], BF16)
        memV = AR.alloc([2, 512], BF16)
        memh = AR.alloc([8, 256], BF16)
        m1 = AR.mark()
        mstg = [AR.alloc([D], F32)] * 2
        memT = AR.alloc([8, 256], F32)
        sq = [AR.alloc([256], F32) for _ in range(2)]
        rs = AR.alloc([256], F32)
        wk = ring_alloc([8, 512])
        wload(wk, w_mkv_d[l, :, 0:512].rearrange("(k p) n -> p k n", p=128))
        wv = ring_alloc([8, 512])
        wload(wv, w_mkv_d[l, :, 512:1024].rearrange("(k p) n -> p k n", p=128))
        wq = ring_alloc([8, 512])
        wload(wq, w_in_d[l, :, 2560:3072].rearrange("(k p) n -> p k n", p=128))
        for mt in range(2):
            s_ = mstg[mt]
            P.dma("sp", s_.v, mem_d[b, mt * 128:(mt + 1) * 128, :], writes=s_.dall())
            for g in range(2):
                ps, pd = R_MM.next()
                for cc in range(4):
                    c = g * 4 + cc
                    P.op("pe", lambda h, s_=s_, c=c, cc=cc, ps=ps: h.transpose(ps[:, cc * 128:(cc + 1) * 128], s_.v[:, c * 128:(c + 1) * 128], identf.v),
                         reads=s_.dall() + identf.dall(), writes=pd, inc=(cc == 3))
                outap = memT.v[:, g * 4:(g + 1) * 4, mt * 128:(mt + 1) * 128]
                inap = ps[:, :].rearrange("p (a b) -> p a b", a=4)
                P.op("act", lambda h, outap=outap, inap=inap: h.copy(out=outap, in_=inap), reads=pd, writes=memT.dall())
        ps, pd = R_MM.next()
        for c in range(8):
            s_ = sq[c % 2]
            P.op("act", lambda h, s_=s_, c=c: h.activation(out=s_.v, in_=memT.v[:, c, :], func=AF.Square),
                 reads=memT.dall(), writes=s_.dall())
            P.op("pe", lambda h, s_=s_, c=c, ps=ps: h.matmul(ps[:, 0:256], onesf.v, s_.v, start=(c == 0), stop=(c == 7)),
                 reads=s_.dall() + onesf.dall(), writes=pd)
        P.op("act", lambda h, ps=ps: h.activation(out=rs.v, in_=ps[:, 0:256], func=AF.Sqrt, scale=1.0 / D, bias=cst.v[:, 0:1]),
             reads=pd + cst.dall(), writes=rs.dall())
        P.op("dve", lambda h: h.reciprocal(rs.v, rs.v), reads=rs.dall(), writes=rs.dall())
        for c in range(8):
            P.op("dve", lambda h, c=c: h.scalar_tensor_tensor(
                out=memh.v[:, c, :], in0=memT.v[:, c, :], scalar=pv.v[:, l, PV_MEMG + c:PV_MEMG + c + 1],
                in1=rs.v, op0=ALU.mult, op1=ALU.mult),
                reads=memT.dall() + rs.dall() + pv.dall(), writes=memh.dall())
        for hd in range(4):
            ps, pd = R_MM.next()
            mm_group(ps[:, 0:256], pd, [(wk.v[:, k, hd * 128:(hd + 1) * 128], memh.v[:, k, :]) for k in range(8)],
                     wk.dall() + memh.dall())
            P.op("act", lambda h, ps=ps, hd=hd: h.copy(out=memK.v[:, hd, :], in_=ps[:, 0:256]), reads=pd, writes=memK.dall())
        for mt in range(2):
            ps, pd = R_MM.next()
            mm_group(ps[:, :], pd, [(memh.v[:, k, mt * 128:(mt + 1) * 128], wv.v[:, k, :]) for k in range(8)],
                     wv.dall() + memh.dall())
            P.op("act", lambda h, ps=ps, mt=mt: h.copy(out=memV.v[:, mt, :], in_=ps[:, :]), reads=pd, writes=memV.dall())
        AR.release(m1)
        qx = [AR.alloc([TT], BF16) for _ in range(2)]
        pt = [AR.alloc([2, TT], BF16) for _ in range(2)]
        rd = [AR.alloc([TT], F32) for _ in range(2)]
        sc = 128 ** -0.5
        r_q = Ring([0, 1])
        r_s = Ring([2, 3, 4, 5])
        iters = [(j, hd) for j in range(NT) for hd in range(4)]
        ctx = {}

        def head(idx):
            j, hd = iters[idx]
            t0 = j * TT
            q_ = qx[idx % 2]
            ps, pd = r_q.next()
            mm_group(ps[:, :], pd, [(wq.v[:, k, hd * 128:(hd + 1) * 128], hT_rhs(k, t0, TT)) for k in range(8)],
                     wq.dall() + hT_deps(t0, TT))
            P.op("act", lambda h: h.copy(out=q_.v, in_=ps[:, :]), reads=pd, writes=q_.dall())
            sb = []
            for mt in range(2):
                sps, spd = r_s.next()
                P.op("pe", lambda h, sps=sps, mt=mt: h.matmul(sps[:, :], memK.v[:, hd, mt * 128:(mt + 1) * 128], q_.v, start=True, stop=True),
                     reads=q_.dall() + memK.dall(), writes=spd)
                sb.append((sps, spd))
            ctx[idx] = sb

        def tail(idx):
            j, hd = iters[idx]
            t0 = j * TT
            p_ = pt[idx % 2]; r_ = rd[idx % 2]
            sb = ctx.pop(idx)
            for mt in range(2):
                sps, spd = sb[mt]
                P.op("act", lambda h, sps=sps, mt=mt: h.activation(out=p_.v[:, mt, :], in_=sps[:, :], func=AF.Exp, scale=sc),
                     reads=spd, writes=p_.dall())
            ops, opd = psb[6], [psd[6]]
            mm_group(ops[:, :], opd, [(memV.v[:, mt, hd * 128:(hd + 1) * 128], p_.v[:, mt, :]) for mt in range(2)],
                     memV.dall() + p_.dall())
            dps, dpd = psb[7], [psd[7]]
            mm_group(dps[:, :], dpd, [(onesb.v, p_.v[:, mt, :]) for mt in range(2)], onesb.dall() + p_.dall())
            P.op("act", lambda h: h.activation(out=r_.v, in_=dps[:, :], func=AF.Ln), reads=dpd, writes=r_.dall())
            P.op("act", lambda h: h.activation(out=r_.v, in_=r_.v, func=AF.Exp, scale=-1.0), reads=r_.dall(), writes=r_.dall())
            P.op("dve", lambda h: h.tensor_tensor(yC.v[:, hd, t0:t0 + TT], ops[:, :], r_.v, op=ALU.mult),
                 reads=opd + r_.dall(), writes=yC.d2(hd, t0, t0 + TT))

        for idx in range(len(iters)):
            if idx == 0:
                head(0)
            if idx + 1 < len(iters):
                head(idx + 1)
            tail(idx)
        AR.release(m0)

    def moba_phase(l, yB):
        m0 = AR.mark()
        NSET = 2
        KA = [[AR.alloc([S], BF16) for _ in range(2)] for _ in range(NSET)]
        QA = [[AR.alloc([S], BF16) for _ in range(2)] for _ in range(NSET)]
        VT = AR.alloc([16, 2, 128], BF16)
        NPT = 4
        PT = [AR.alloc([2, 256], BF16) for _ in range(NPT)]
        kmT = [[AR.alloc([8], BF16) for _ in range(2)] for _ in range(NSET)]
        ksum = AR.alloc([8], F32)
        gm = AR.alloc([16], F32)
        top8 = AR.alloc([16], F32)
        stage = [[AR.alloc([2, 128], BF16) for _ in range(2)] for _ in range(4)]
        dsh = [AR.alloc([256], F32) for _ in range(2)]
        r_proj = Ring([0, 1])
        r_sc = Ring([3, 4, 5])
        r_acc = Ring([6, 7])
        gate_bank, gate_dep = psb[2], [psd[2]]
        for s_ in range(NSET):
            for hh in range(2):
                P.op("dve", lambda h, s_=s_, hh=hh: h.memset(QA[s_][hh].v[64:72, 0:1024], 0.0), writes=QA[s_][hh].d(0, 1024))
                P.dma("pool", QA[s_][hh].v[72:76, :], cqrow_d, writes=QA[s_][hh].dall())
                P.dma("pool", KA[s_][hh].v[64:72, :], coneh_d, writes=KA[s_][hh].dall())
        P.op("dve", lambda h: h.memset(VT.v, 1.0), writes=VT.dall())
        for q4 in range(4):
            for sub in range(2):
                t_ = stage[q4][sub]
                P.op("dve", lambda h, t_=t_: h.memset(t_.v, 0.0), writes=t_.dall())
        state = {"pti": 0, "acc": 0, "wv": {}}

        def project(pair):
            s_ = pair % NSET
            ka, qa, km = KA[s_], QA[s_], kmT[s_]
            wq = ring_alloc([8, 128]); wload(wq, w_in_d[l, :, 1024 + pair * 128:1024 + (pair + 1) * 128].rearrange("(k p) n -> p k n", p=128))
            wk = ring_alloc([8, 128]); wload(wk, w_in_d[l, :, 1536 + pair * 128:1536 + (pair + 1) * 128].rearrange("(k p) n -> p k n", p=128))
            for hh in range(2):
                P.dma("pool", ka[hh].v[72:76, :], ckrow_d[pair * 2 + hh], writes=ka[hh].dall())
            for j in range(NT):
                t0 = j * TT
                ps, pd = r_proj.next()
                mm_group(ps[:, :], pd, [(wk.v[:, k, :], hT_rhs(k, t0, TT)) for k in range(8)], wk.dall() + hT_deps(t0, TT))
                for hh in range(2):
                    P.op("act", lambda h, ps=ps, hh=hh: h.copy(out=ka[hh].v[0:64, t0:t0 + TT], in_=ps[hh * 64:(hh + 1) * 64, :]),
                         reads=pd, writes=ka[hh].d(t0, t0 + TT))
                ps, pd = r_proj.next()
                mm_group(ps[:, :], pd, [(wq.v[:, k, :], hT_rhs(k, t0, TT)) for k in range(8)], wq.dall() + hT_deps(t0, TT))
                for hh in range(2):
                    P.op("act", lambda h, ps=ps, hh=hh: h.mul(out=qa[hh].v[0:64, t0:t0 + TT], in_=ps[hh * 64:(hh + 1) * 64, :], mul=0.125),
                         reads=pd, writes=qa[hh].d(t0, t0 + TT))
            for hh in range(2):
                P.op("dve", lambda h, hh=hh: h.tensor_reduce(out=ksum.v[0:64, :], in_=ka[hh].v[0:64, :].rearrange("p (a b) -> p a b", a=8),
                                                              axis=AX.X, op=ALU.add),
                     reads=ka[hh].dall(), writes=ksum.dall())
                P.op("act", lambda h, hh=hh: h.mul(out=km[hh].v[0:64, :], in_=ksum.v[0:64, :], mul=1.0 / 256), reads=ksum.dall(), writes=km[hh].dall())
            for qb in range(4, 8):
                for sub in range(2):
                    qs = qb * 256 + sub * 128
                    gcol = ((qb - 4) * 2 + sub) * 16
                    for hh in range(2):
                        P.op("pe", lambda h, hh=hh, qs=qs, gcol=gcol: h.matmul(gate_bank[:, gcol + hh * 8:gcol + (hh + 1) * 8], qa[hh].v[0:64, qs:qs + 128],
                                                                             km[hh].v[0:64, :], start=True, stop=True),
                             reads=qa[hh].d(qs, qs + 128) + km[hh].dall(), writes=gate_dep)
                    P.op("dve", lambda h, qb=qb, gcol=gcol: h.tensor_tensor(gm.v, gate_bank[:, gcol:gcol + 16], pastm.v[:, qb - 4, :], op=ALU.add),
                         reads=gate_dep + pastm.dall(), writes=gm.dall())
                    st_ = stage[qb - 4][sub]
                    for hh in range(2):
                        P.op("dve", lambda h, hh=hh: h.max(out=top8.v[:, hh * 8:(hh + 1) * 8], in_=gm.v[:, hh * 8:(hh + 1) * 8]),
                             reads=gm.dall(), writes=top8.dall())
                        P.op("dve", lambda h, hh=hh, st_=st_, qb=qb: h.tensor_scalar(
                            st_.v[:, hh, 64:64 + qb], gm.v[:, hh * 8:hh * 8 + qb], top8.v[:, hh * 8 + 2:hh * 8 + 3], -30000.0,
                            op0=ALU.is_lt, op1=ALU.mult),
                            reads=gm.dall() + top8.dall(), writes=st_.dall())

        def project_v(pair):
            vt = VT
            wv = ring_alloc([8, 128]); wload(wv, w_in_d[l, :, 2048 + pair * 128:2048 + (pair + 1) * 128].rearrange("(k p) n -> p k n", p=128))
            for j in range(NT):
                ps, pd = r_proj.next()
                for sub in range(4):
                    tk = j * 4 + sub
                    mm_group(ps[:, sub * 128:(sub + 1) * 128], pd, [(hT_rhs(k, tk * 128, 128), wv.v[:, k, :]) for k in range(8)],
                             wv.dall() + hT_deps(tk * 128, 128))
                pv4 = ps[:, :].rearrange("p (a b) -> p a b", a=4)
                P.op("dve", lambda h, pv4=pv4, j=j: h.tensor_copy(out=vt.v[:, j * 4:(j + 1) * 4, 0, 0:64], in_=pv4[:, :, 0:64]),
                     reads=pd, writes=vt.d2s(range(j * 4, j * 4 + 4), 0, 256))
                P.op("dve", lambda h, pv4=pv4, j=j: h.tensor_copy(out=vt.v[:, j * 4:(j + 1) * 4, 1, 64:128], in_=pv4[:, :, 64:128]),
                     reads=pd, writes=vt.d2s(range(j * 4, j * 4 + 4), 0, 256))

        def mask_rows(pair):
            s_ = pair % NSET
            qa = QA[s_]
            for qb in range(4, 8):
                for sub in range(2):
                    qs = qb * 256 + sub * 128
                    st_ = stage[qb - 4][sub]
                    tps, tpd = r_sc.next()
                    tpb = tps[:, :].bitcast(BF16)
                    for hh in range(2):
                        P.op("pe", lambda h, tpb=tpb, st_=st_, hh=hh: h.transpose(tpb[:, hh * 128:(hh + 1) * 128], st_.v[:, hh, :], identb.v),
                             reads=st_.dall() + identb.dall(), writes=tpd, inc=(hh == 1))
                    for hh in range(2):
                        P.op("act", lambda h, tpb=tpb, hh=hh, qs=qs: h.copy(out=qa[hh].v[64:72, qs:qs + 128], in_=tpb[64:72, hh * 128:(hh + 1) * 128]),
                             reads=tpd, writes=qa[hh].d(qs, qs + 128))

        def attention(pair, qbs):
            s_ = pair % NSET
            ka, qa, vt = KA[s_], QA[s_], VT
            units = []
            for qb in qbs:
                q0 = qb * 256
                for hh in range(2):
                    ai = state["acc"]; state["acc"] += 1
                    ops, opd = r_acc.next()
                    ds_ = dsh[ai % 2]
                    nun = qb + 1
                    for ui in range(nun):
                        units.append(dict(qb=qb, q0=q0, hh=hh, ui=ui, nun=nun, ops=ops, opd=opd, ds=ds_))
            for u in units:
                u["p"] = PT[state["pti"] % NPT]; state["pti"] += 1
                u["sps"], u["spd"] = None, None

            def emit_qk(u):
                sps, spd = r_sc.next()
                u["sps"], u["spd"] = sps, spd
                hh, q0, qb = u["hh"], u["q0"], u["qb"]
                if u["ui"] == 0:
                    k0 = 2 * qb
                    P.op("pe", lambda h: h.matmul(sps[:, 0:256], ka[hh].v[0:76, k0 * 128:(k0 + 1) * 128], qa[hh].v[0:76, q0:q0 + 256], start=True, stop=False),
                         reads=ka[hh].d(k0 * 128, (k0 + 1) * 128) + qa[hh].d(q0, q0 + 256) + trib.dall() + identb.dall(), writes=spd, inc=False)
                    P.op("pe", lambda h: h.matmul(sps[:, 0:128], identb.v, trib.v, start=False, stop=True), writes=spd, inc=False)
                    P.op("pe", lambda h: h.matmul(sps[:, 384:512], ka[hh].v[0:76, (k0 + 1) * 128:(k0 + 2) * 128], qa[hh].v[0:76, q0 + 128:q0 + 256], start=True, stop=False),
                         reads=ka[hh].d((k0 + 1) * 128, (k0 + 2) * 128) + qa[hh].d(q0, q0 + 256), writes=spd, inc=False)
                    P.op("pe", lambda h: h.matmul(sps[:, 384:512], identb.v, trib.v, start=False, stop=True), writes=spd)
                else:
                    kp = u["ui"] - 1
                    for i2 in range(2):
                        kt = 2 * kp + i2
                        P.op("pe", lambda h, kt=kt, i2=i2: h.matmul(sps[:, i2 * 256:(i2 + 1) * 256], ka[hh].v[0:76, kt * 128:(kt + 1) * 128],
                                                                  qa[hh].v[0:76, q0:q0 + 256], start=True, stop=True),
                             reads=ka[hh].d(kt * 128, (kt + 1) * 128) + qa[hh].d(q0, q0 + 256), writes=spd, inc=(i2 == 1))

            def emit_rest(u):
                sps, spd, p_ = u["sps"], u["spd"], u["p"]
                hh, q0, qb, ops, opd = u["hh"], u["q0"], u["qb"], u["ops"], u["opd"]
                last = (u["ui"] == u["nun"] - 1)
                if u["ui"] == 0:
                    k0 = 2 * qb
                    P.op("act", lambda h: h.activation(out=p_.v[:, 0, :], in_=sps[:, 0:256], func=AF.Exp), reads=spd, writes=p_.dall())
                    P.op("act", lambda h: h.activation(out=p_.v[:, 1, 128:256], in_=sps[:, 384:512], func=AF.Exp), reads=spd, writes=p_.dall())
                    P.op("pe", lambda h: h.matmul(ops[:, 0:256], vt.v[:, k0, hh, :], p_.v[:, 0, :], start=True, stop=False),
                         reads=vt.d2(k0, 0, 256) + p_.dall(), writes=opd, inc=False)
                    P.op("pe", lambda h: h.matmul(ops[:, 128:256], vt.v[:, k0 + 1, hh, :], p_.v[:, 1, 128:256], start=False, stop=last),
                         reads=vt.d2(k0 + 1, 0, 256) + p_.dall(), writes=opd, inc=True)
                else:
                    kp = u["ui"] - 1
                    P.op("act", lambda h: h.activation(out=p_.v.rearrange("p a b -> p (a b)"), in_=sps[:, :], func=AF.Exp),
                         reads=spd, writes=p_.dall())
                    for i2 in range(2):
                        kt = 2 * kp + i2
                        P.op("pe", lambda h, kt=kt, i2=i2: h.matmul(ops[:, 0:256], vt.v[:, kt, hh, :], p_.v[:, i2, :], start=False, stop=(last and i2 == 1)),
                             reads=vt.d2(kt, 0, 256) + p_.dall(), writes=opd, inc=(i2 == 1))
                if last:
                    ds_ = u["ds"]
                    olo, dlo = (0, 64) if hh == 0 else (64, 0)
                    P.op("act", lambda h: h.activation(out=ds_.v[olo:olo + 64, :], in_=ops[dlo:dlo + 64, 0:256], func=AF.Ln), reads=opd, writes=ds_.dall())
                    P.op("act", lambda h: h.activation(out=ds_.v[olo:olo + 64, :], in_=ds_.v[olo:olo + 64, :], func=AF.Exp, scale=-1.0), reads=ds_.dall(), writes=ds_.dall())
                    P.op("dve", lambda h: h.tensor_tensor(yB.v[olo:olo + 64, pair, q0:q0 + 256], ops[olo:olo + 64, 0:256], ds_.v[olo:olo + 64, :], op=ALU.mult),
                         reads=opd + ds_.dall(), writes=yB.d2(pair, q0, q0 + 256))

            for i, u in enumerate(units):
                if i == 0:
                    emit_qk(u)
                if i + 1 < len(units):
                    emit_qk(units[i + 1])
                emit_rest(u)

        project(0)
        for pair in range(4):
            project_v(pair)
            attention(pair, range(0, 4))
            mask_rows(pair)
            if pair + 1 < 4:
                project(pair + 1)
            attention(pair, range(4, 8))
        AR.release(m0)

    def merge_phase(l, yA, yB, yC):
        m0 = AR.mark()
        mg = AR.alloc([8, 1024], BF16)
        NGT = 2 if (AR.nbytes - AR.top) >= 2 * 3 * 2048 + 1024 else 1
        gt = [[AR.alloc([TT], F32) for _ in range(3)] for _ in range(NGT)]
        gi = 0
        ys = [yA, yB, yC]
        gview = w_in_d[l, :, 3072:6144].rearrange("(k p) (n d) -> p k n d", p=128, n=3)
        bview = w_br_d[l].rearrange("n (k p) d -> p k n d", p=128)
        for hf in range(2):
            for dc in range(8):
                G = ring_alloc([8, 3, 128])
                B = ring_alloc([4, 3, 128])
                for n in range(3):
                    P.dma("pool", G.v[:, :, n, :], gview[:, :, n, dc * 128:(dc + 1) * 128], writes=G.dall())
                    P.dma("pool", B.v[:, :, n, :], bview[:, :, n, dc * 128:(dc + 1) * 128], writes=B.dall())
                for jj in range(2):
                    t0 = hf * 1024 + jj * TT
                    gts = gt[gi % NGT]; gi += 1
                    for n in range(3):
                        ps, pd = R_MM.next()
                        mm_group(ps[:, :], pd, [(G.v[:, k, n, :], hT_rhs(k, t0, TT)) for k in range(8)],
                                 G.dall() + hT_deps(t0, TT))
                        P.op("act", lambda h, ps=ps, n=n, gts=gts: h.activation(out=gts[n].v, in_=ps[:, :], func=AF.Sigmoid), reads=pd, writes=gts[n].dall())
                        ps2, pd2 = R_MM.next()
                        mm_group(ps2[:, :], pd2, [(B.v[:, k, n, :], ys[n].v[:, k, t0:t0 + TT]) for k in range(4)],
                                 B.dall() + ys[n].d2s(range(4), t0, t0 + TT))
                        P.op("dve", lambda h, ps2=ps2, n=n, gts=gts: h.tensor_tensor(gts[n].v, ps2[:, :], gts[n].v, op=ALU.mult),
                             reads=pd2 + gts[n].dall(), writes=gts[n].dall())
                    P.op("dve", lambda h, gts=gts: h.tensor_tensor(gts[0].v, gts[0].v, gts[1].v, op=ALU.add),
                         reads=gts[0].dall() + gts[1].dall(), writes=gts[0].dall())
                    P.op("dve", lambda h, dc=dc, jj=jj, gts=gts: h.tensor_tensor(mg.v[:, dc, jj * TT:(jj + 1) * TT], gts[0].v, gts[2].v, op=ALU.add),
                         reads=gts[0].dall() + gts[2].dall(), writes=mg.d2(dc, jj * TT, (jj + 1) * TT))
            for pp in range(2):
                Wo = ring_alloc([8, 512])
                wload(Wo, w_out_d[l, :, pp * 512:(pp + 1) * 512].rearrange("(k p) n -> p k n", p=128))
                for jj in range(2):
                    t0 = hf * 1024 + jj * TT
                    for dci in range(4):
                        dc = pp * 4 + dci
                        ps, pd = R_MM.next()
                        mm_group(ps[:, :], pd, [(Wo.v[:, k, dci * 128:(dci + 1) * 128], mg.v[:, k, jj * TT:(jj + 1) * TT]) for k in range(8)],
                                 Wo.dall() + mg.d2s(range(8), jj * TT, (jj + 1) * TT))
                        P.op("dve", lambda h, ps=ps, dc=dc, t0=t0: h.tensor_tensor(xT.v[:, dc, t0:t0 + TT], ps[:, :], xT.v[:, dc, t0:t0 + TT], op=ALU.add),
                             reads=pd + xT.d2(dc, t0, t0 + TT), writes=xT.d2(dc, t0, t0 + TT))
        AR.release(m0)

    def ffn_phase(l):
        m0 = AR.mark()
        act = AR.alloc([24, 1024], BF16)
        gbuf = [AR.alloc([2 + 1024], F32) for _ in range(2)]
        acc = [AR.alloc([TT], F32) for _ in range(2)]
        gel = [AR.alloc([TT], F32) for _ in range(2)]
        it = 0
        for hf in range(2):
            for ffg in range(12):
                Wg = ring_alloc([8, 256]); wload(Wg, w_fg_d[l, :, ffg * 256:(ffg + 1) * 256].rearrange("(k p) n -> p k n", p=128))
                Wu = ring_alloc([8, 256]); wload(Wu, w_fu_d[l, :, ffg * 256:(ffg + 1) * 256].rearrange("(k p) n -> p k n", p=128))
                for fci in range(2):
                    fc = ffg * 2 + fci
                    gb = gbuf[fc % 2]
                    if hf == 0:
                        P.op("dve", lambda h, gb=gb: h.memset(gb.v[:, 0:2], 0.0), writes=gb.d(0, 2))
                    else:
                        P.op("dve", lambda h, gb=gb, fc=fc: h.tensor_copy(out=gb.v[:, 0:2], in_=gtail.v[:, fc, :]), reads=gtail.dall(), writes=gb.d(0, 2))
                    for jj in range(2):
                        t0 = hf * 1024 + jj * TT
                        g0 = 2 + jj * TT
                        ps, pd = R_MM.next()
                        mm_group(ps[:, :], pd, [(Wg.v[:, k, fci * 128:(fci + 1) * 128], hT_rhs(k, t0, TT)) for k in range(8)],
                                 Wg.dall() + hT_deps(t0, TT))
                        ups, upd = R_MM.next()
                        mm_group(ups[:, :], upd, [(Wu.v[:, k, fci * 128:(fci + 1) * 128], hT_rhs(k, t0, TT)) for k in range(8)],
                                 Wu.dall() + hT_deps(t0, TT))
                        P.op("act", lambda h, ps=ps, gb=gb, g0=g0: h.copy(out=gb.v[:, g0:g0 + TT], in_=ps[:, :]), reads=pd, writes=gb.d(g0, g0 + TT))
                        a_ = acc[it % 2]; ge = gel[it % 2]; it += 1
                        cw = lambda k, fc=fc: pv.v[:, l, PV_FCW + k * 24 + fc:PV_FCW + k * 24 + fc + 1]
                        cb = pv.v[:, l, PV_FCB + fc:PV_FCB + fc + 1]
                        gd = gb.d(g0 - 2, g0 + TT) + pv.dall()
                        P.op("dve", lambda h, a_=a_, gb=gb, g0=g0, cw=cw, cb=cb: h.tensor_scalar(a_.v, gb.v[:, g0:g0 + TT], cw(2), cb, op0=ALU.mult, op1=ALU.add),
                             reads=gd, writes=a_.dall())
                        P.op("dve", lambda h, a_=a_, gb=gb, g0=g0, cw=cw: h.scalar_tensor_tensor(out=a_.v, in0=gb.v[:, g0 - 1:g0 - 1 + TT], scalar=cw(1), in1=a_.v,
                                                                                               op0=ALU.mult, op1=ALU.add),
                             reads=gd + a_.dall(), writes=a_.dall())
                        P.op("dve", lambda h, a_=a_, gb=gb, g0=g0, cw=cw: h.scalar_tensor_tensor(out=a_.v, in0=gb.v[:, g0 - 2:g0 - 2 + TT], scalar=cw(0), in1=a_.v,
                                                                                               op0=ALU.mult, op1=ALU.add),
                             reads=gd + a_.dall(), writes=a_.dall())
                        P.op("act", lambda h, a_=a_, ge=ge: h.activation(out=ge.v, in_=a_.v, func=GELU), reads=a_.dall(), writes=ge.dall())
                        P.op("dve", lambda h, ups=ups, ge=ge, fc=fc, jj=jj: h.tensor_tensor(act.v[:, fc, jj * TT:(jj + 1) * TT], ups[:, :], ge.v, op=ALU.mult),
                             reads=upd + ge.dall(), writes=act.d2(fc, jj * TT, (jj + 1) * TT))
                    if hf == 0:
                        P.op("dve", lambda h, gb=gb, fc=fc: h.tensor_copy(out=gtail.v[:, fc, :], in_=gb.v[:, 1024:1026]), reads=gb.d(1024, 1026), writes=gtail.dall())
            for dcg in range(4):
                Wd = ring_alloc([24, 256]); wload(Wd, w_fd_d[l, :, dcg * 256:(dcg + 1) * 256].rearrange("(k p) n -> p k n", p=128))
                for jj in range(2):
                    t0 = hf * 1024 + jj * TT
                    for dci in range(2):
                        dc = dcg * 2 + dci
                        ps, pd = R_MM.next()
                        mm_group(ps[:, :], pd, [(Wd.v[:, k, dci * 128:(dci + 1) * 128], act.v[:, k, jj * TT:(jj + 1) * TT]) for k in range(24)],
                                 Wd.dall() + act.d2s(range(24), jj * TT, (jj + 1) * TT))
                        P.op("dve", lambda h, ps=ps, dc=dc, t0=t0: h.tensor_tensor(xT.v[:, dc, t0:t0 + TT], ps[:, :], xT.v[:, dc, t0:t0 + TT], op=ALU.add),
                             reads=pd + xT.d2(dc, t0, t0 + TT), writes=xT.d2(dc, t0, t0 + TT))
        AR.release(m0)

    for b in range(n_seq):
        mark('load'); load_x(b)
        for l in range(n_layers):
            mark('norm1'); rmsnorm_to_hT(PV_MIXG, l)
            mY = AR.mark()
            yB = AR.alloc([4, S], BF16)
            mark('moba'); moba_phase(l, yB)
            yA = AR.alloc([4, S], BF16)
            mark('lru'); lru_phase(l, yA)
            yC = AR.alloc([4, S], BF16)
            mark('xattn'); xattn_phase(l, b, yC)
            if debug and b == 0:
                for nm, y_ in (("yA%d" % l, yA), ("yB%d" % l, yB), ("yC%d" % l, yC)):
                    if nm in dbg_d:
                        P.dma("pool", dbg_d[nm].rearrange("(c p) t -> p c t", p=128), y_.v, reads=y_.dall())
            mark('merge'); merge_phase(l, yA, yB, yC)
            AR.release(mY)
            if debug and b == 0 and ("xmid%d" % l) in dbg_d:
                P.dma("sp", dbg_d["xmid%d" % l].rearrange("(c p) t -> p c t", p=128), xT.v, reads=xT.dall())
            mark('norm2'); rmsnorm_to_hT(PV_FFNG, l)
            mark('ffn'); ffn_phase(l)
            if debug and b == 0 and ("xout%d" % l) in dbg_d:
                P.dma("sp", dbg_d["xout%d" % l].rearrange("(c p) t -> p c t", p=128), xT.v, reads=xT.dall())
        mark('final'); final_store(b)
    mark('end')
    P.finish()
    stack.close()
    return nc, P


def host_consts():
    t = np.arange(S)
    c = {}
    c["c_ident"] = np.eye(128, dtype=np.float32)
    k = np.arange(128)
    c["c_tri"] = np.where(k[None, :] >= k[:, None], 0.0, -30000.0).astype(np.float32)
    base = np.stack([t % 256, 256 * (t // 256), np.ones(S), np.ones(S)]).astype(np.float32)
    c["c_krow"] = np.stack([base * (2.0 ** (-(hd + 1))) for hd in range(8)]).astype(np.float32)
    c["c_qrow"] = np.stack([np.ones(S), np.ones(S), -(t % 256), -256 * (t // 256)]).astype(np.float32)
    c["c_onehot"] = (np.arange(8)[:, None] == (t // 256)[None, :]).astype(np.float32)
    past = np.zeros((128, 4, 16), np.float32)
    for q4 in range(4):
        qb = q4 + 4
        for hh in range(2):
            past[:, q4, hh * 8 + qb:hh * 8 + 8] = -1e30
    c["c_past"] = past
    return c


def host_params(inp):
    pvec = np.zeros((DEPTH, 128, PV_N), np.float32)
    wabd = np.zeros((DEPTH, 2, 128, 4, 128), np.float32)
    for l in range(DEPTH):
        pvec[l, :, PV_MIXG:PV_MIXG + 8] = _chunked(inp["mix_norm_gain"][l])
        pvec[l, :, PV_FFNG:PV_FFNG + 8] = _chunked(inp["ffn_norm_gain"][l])
        pvec[l, :, PV_MEMG:PV_MEMG + 8] = _chunked(inp["mem_norm_gain"][l])
        pvec[l, :, PV_FING:PV_FING + 8] = _chunked(inp["final_norm_gain"])
        for k in range(4):
            pvec[l, :, PV_LCW + k * 4:PV_LCW + k * 4 + 4] = _chunked(inp["lru_conv_w"][l, k])
        pvec[l, :, PV_LCB:PV_LCB + 4] = _chunked(inp["lru_conv_b"][l])
        pvec[l, :, PV_LBA:PV_LBA + 4] = _chunked(inp["lru_b_a"][l].reshape(-1))
        pvec[l, :, PV_LBX:PV_LBX + 4] = _chunked(inp["lru_b_x"][l].reshape(-1))
        pvec[l, :, PV_LAM:PV_LAM + 4] = _chunked(inp["lru_lambda"][l])
        for k in range(3):
            pvec[l, :, PV_FCW + k * 24:PV_FCW + k * 24 + 24] = _chunked(inp["ffn_conv_w"][l, k])
        pvec[l, :, PV_FCB:PV_FCB + 24] = _chunked(inp["ffn_conv_b"][l])
        for g, nm in enumerate(("lru_w_a", "lru_w_x")):
            w = inp[nm][l]
            for hd in range(8):
                c, o = hd // 2, (hd % 2) * 64
                wabd[l, g, o:o + 64, c, o:o + 64] = w[hd]
    return pvec, wabd


_CACHE = {}


def kernel(**inputs):
    inp = {k: np.asarray(v) for k, v in inputs.items()}
    if "nc" not in _CACHE:
        _CACHE["nc"] = build_program()[0]
    nc = _CACHE["nc"]
    pvec, wabd = host_params(inp)
    consts = host_consts()
    shared = dict(consts)
    shared.update(pvec=pvec, wabd=wabd)
    for k in ("w_in", "w_mem_kv", "w_branch", "w_out", "w_ffn_gate", "w_ffn_up", "w_ffn_down"):
        shared[k] = np.ascontiguousarray(inp[k], dtype=np.float32)
    in_maps = []
    for c in range(NCORES):
        m = dict(shared)
        m["x"] = np.ascontiguousarray(inp["x"][c * SEQ_PER_CORE:(c + 1) * SEQ_PER_CORE], dtype=np.float32)
        m["mem"] = np.ascontiguousarray(inp["mem"][c * SEQ_PER_CORE:(c + 1) * SEQ_PER_CORE], dtype=np.float32)
        in_maps.append(m)
    res = run_bass_kernel_spmd(nc, in_maps, core_ids=list(range(NCORES)))
    out = np.concatenate([np.asarray(r["out"]) for r in res.results], axis=0)
    return out.astype(np.float32)
```

```python
import math
from contextlib import ExitStack
import numpy as np
import concourse.bass as bass
import concourse.mybir as mybir
from concourse.bass_utils import run_bass_kernel_spmd

F32 = mybir.dt.float32
BF16 = mybir.dt.bfloat16
U8 = mybir.dt.uint8
AF = mybir.ActivationFunctionType
ALU = mybir.AluOpType
AX = mybir.AxisListType

NCORES = 8
SEQ_PER_CORE = 2
S = 2048
D = 1024
DEPTH = 2
NT = 4
TT = 512
GELU = AF.Gelu_apprx_tanh

ENGS = ["pe", "act", "dve", "pool", "sp"]
NDMA = 24
GRAN = 256


class Dep:
    __slots__ = ("w", "rs")

    def __init__(self):
        self.w = {}
        self.rs = {}


class _Rec:
    def __getattr__(self, name):
        def f(*a, **k):
            self.call = (name, a, k)
            return self
        return f


class Prog:
    def __init__(self, nc, stack):
        self.nc = nc
        self.streams = {e: [] for e in ENGS}
        self.cnt = {e: 0 for e in ENGS}
        self.seen = {e: {} for e in ENGS}
        self.sem = {}
        for e in ENGS:
            self.sem[e] = stack.enter_context(nc.semaphore("s_" + e))
        self.dsem = []
        self.dtot = []
        for i in range(NDMA):
            self.dsem.append(stack.enter_context(nc.semaphore("d_%d" % i)))
            self.dtot.append(0)
            self.sem[("dma", i)] = self.dsem[i]
        self.dnext = 0
        self.ninst = 0

    def _need(self, eng, reads, writes):
        need = {}
        for d in reads:
            for k, v in d.w.items():
                if need.get(k, 0) < v:
                    need[k] = v
        for d in writes:
            for k, v in d.w.items():
                if need.get(k, 0) < v:
                    need[k] = v
            for k, v in d.rs.items():
                if need.get(k, 0) < v:
                    need[k] = v
        out = []
        seen = self.seen[eng]
        raw_self = 0
        if eng in ("act", "dve", "pool"):
            for d in reads:
                v = d.w.get(eng, 0)
                if v > raw_self:
                    raw_self = v
        for k, v in need.items():
            if k == eng:
                if raw_self == 0:
                    continue
                v = raw_self
            if seen.get(k, 0) >= v:
                continue
            seen[k] = v
            out.append((self.sem[k], v))
        return out

    @staticmethod
    def _mark(key, val, reads, writes):
        for d in reads:
            if d.rs.get(key, 0) < val:
                d.rs[key] = val
        for d in writes:
            if d.w.get(key, 0) < val:
                d.w[key] = val

    def op(self, eng, fn, reads=(), writes=(), inc=True):
        waits = self._need(eng, reads, writes)
        if inc:
            self.cnt[eng] += 1
            val = self.cnt[eng]
        else:
            val = self.cnt[eng] + 1
        self._mark(eng, val, reads, writes)
        sem = self.sem[eng]
        self.ninst += 1
        rec = _Rec()
        fn(rec)
        cname, cargs, ckw = rec.call

        def thunk(h):
            for s, v in waits[:-1]:
                h.wait_ge(s, v)
            inst = getattr(h, cname)(*cargs, **ckw)
            if waits:
                inst._wait_ge(*waits[-1])
            if inc:
                inst.then_inc(sem, 1)
        self.streams[eng].append(thunk)

    def dma(self, eng, out, in_, reads=(), writes=(), **kw):
        k = self.dnext
        self.dnext = (self.dnext + 1) % NDMA
        key = ("dma", k)
        waits = self._need(eng, reads, writes)
        prev = self.dtot[k]
        if prev > 0 and self.seen[eng].get(key, 0) < prev:
            self.seen[eng][key] = prev
            waits.append((self.dsem[k], prev))
        self.dtot[k] += 16
        val = self.dtot[k]
        self._mark(key, val, reads, writes)
        sem = self.dsem[k]
        self.ninst += 1

        def thunk(h):
            for s, v in waits:
                h.wait_ge(s, v)
            h.dma_start(out=out, in_=in_, **kw).then_inc(sem, 16)
        self.streams[eng].append(thunk)

    def finish(self):
        waits = [(self.dsem[k], self.dtot[k]) for k in range(NDMA) if self.dtot[k] > 0]
        others = [(self.sem[e], self.cnt[e]) for e in ENGS if e != "sp" and self.cnt[e] > 0]

        def fthunk(h):
            for s, v in waits + others:
                h.wait_ge(s, v)
        self.streams["sp"].append(fthunk)
        nc = self.nc
        st = self.streams
        with nc.Block() as block:
            @block.tensor
            def _(h):
                for t in st["pe"]:
                    t(h)

            @block.scalar
            def _(h):
                for t in st["act"]:
                    t(h)

            @block.vector
            def _(h):
                for t in st["dve"]:
                    t(h)

            @block.gpsimd
            def _(h):
                for t in st["pool"]:
                    t(h)

            @block.sync
            def _(h):
                for t in st["sp"]:
                    t(h)


class SBT:
    def __init__(self, arena, gran, off, shape, dt):
        self.esz = 4 if dt == F32 else 2
        self.off = off
        self.shape = shape
        self.dt = dt
        n = 1
        for s_ in shape:
            n *= s_
        self.nbytes = n * self.esz
        v = arena[:, off:off + self.nbytes].bitcast(dt)
        if len(shape) == 2:
            v = v.rearrange("p (a b) -> p a b", a=shape[0])
        elif len(shape) == 3:
            v = v.rearrange("p (a b c) -> p a b c", a=shape[0], b=shape[1])
        self.v = v
        self.gran = gran

    def dall(self):
        return self.gran[self.off // GRAN:(self.off + self.nbytes + GRAN - 1) // GRAN]

    def d(self, lo, hi):
        a = self.off + lo * self.esz
        b = self.off + hi * self.esz
        return self.gran[a // GRAN:(b + GRAN - 1) // GRAN]

    def d2(self, i0, lo, hi):
        n1 = self.shape[-1] if len(self.shape) == 2 else self.shape[1] * self.shape[2]
        return self.d(i0 * n1 + lo, i0 * n1 + hi)

    def d2s(self, i0s, lo, hi):
        out = []
        for i0 in i0s:
            out += self.d2(i0, lo, hi)
        return out

    def d3(self, i0, i1, lo, hi):
        n2 = self.shape[2]
        base = (i0 * self.shape[1] + i1) * n2
        return self.d(base + lo, base + hi)


class Arena:
    def __init__(self, nc, nbytes):
        self.t = nc.alloc_sbuf_tensor("arena", [128, nbytes], U8)
        self.nbytes = nbytes
        self.gran = [Dep() for _ in range((nbytes + GRAN - 1) // GRAN + 1)]
        self.top = 0

    def alloc(self, shape, dt):
        off = (self.top + GRAN - 1) // GRAN * GRAN
        n = 4 if dt == F32 else 2
        for s_ in shape:
            n *= s_
        assert off + n <= self.nbytes, ("SBUF arena overflow", off, n, self.nbytes)
        b = SBT(self.t, self.gran, off, list(shape), dt)
        self.top = off + b.nbytes
        self.peak = max(getattr(self, "peak", 0), self.top)
        return b

    def mark(self):
        return self.top

    def release(self, m):
        self.top = m


PV_MIXG, PV_FFNG, PV_MEMG, PV_FING = 0, 8, 16, 24
PV_LCW, PV_LCB, PV_LBA, PV_LBX, PV_LAM = 32, 48, 52, 56, 60
PV_FCW, PV_FCB = 64, 136
PV_N = 160


def _chunked(v):
    return np.ascontiguousarray(v.reshape(-1, 128).T)


def build_program(n_layers=DEPTH, n_seq=SEQ_PER_CORE, debug=None):
    nc = bass.Bass("TRN2", target_bir_lowering=False)
    dram = {}

    def din(name, shape):
        dram[name] = nc.dram_tensor(name, list(shape), F32, kind="ExternalInput").ap()
        return dram[name]

    x_d = din("x", [SEQ_PER_CORE, S, D])
    mem_d = din("mem", [SEQ_PER_CORE, 256, D])
    w_in_d = din("w_in", [DEPTH, D, 6144])
    w_mkv_d = din("w_mem_kv", [DEPTH, D, 1024])
    w_br_d = din("w_branch", [DEPTH, 3, 512, D])
    w_out_d = din("w_out", [DEPTH, D, D])
    w_fg_d = din("w_ffn_gate", [DEPTH, D, 3072])
    w_fu_d = din("w_ffn_up", [DEPTH, D, 3072])
    w_fd_d = din("w_ffn_down", [DEPTH, 3072, D])
    pvec_d = din("pvec", [DEPTH, 128, PV_N])
    wabd_d = din("wabd", [DEPTH, 2, 128, 4, 128])
    cident_d = din("c_ident", [128, 128])
    ctri_d = din("c_tri", [128, 128])
    ckrow_d = din("c_krow", [8, 4, S])
    cqrow_d = din("c_qrow", [4, S])
    coneh_d = din("c_onehot", [8, S])
    cpast_d = din("c_past", [128, 4, 16])
    out_d = nc.dram_tensor("out", [SEQ_PER_CORE, S, D], F32, kind="ExternalOutput").ap()
    dbg_d = {}
    if debug:
        for name, shape in debug.items():
            dbg_d[name] = nc.dram_tensor("dbg_" + name, list(shape), F32, kind="ExternalOutput").ap()

    stack = ExitStack()
    P = Prog(nc, stack)
    total = nc.sbuf_top - nc.sbuf_base - 64
    AR = Arena(nc, total // GRAN * GRAN - GRAN)

    psb = [nc.alloc_psum_tensor("ps%d" % i, [128, 512], F32) for i in range(8)]
    psd = [Dep() for _ in range(8)]

    class Ring:
        def __init__(self, banks):
            self.banks = banks
            self.i = 0

        def next(self):
            b = self.banks[self.i % len(self.banks)]
            self.i += 1
            return psb[b], [psd[b]]

    xT = AR.alloc([8, S], F32)
    hT = AR.alloc([8, S], BF16)
    pv = AR.alloc([DEPTH, PV_N], F32)
    identf = AR.alloc([128], F32)
    identb = AR.alloc([128], BF16)
    onesf = AR.alloc([128], F32)
    onesb = AR.alloc([128], BF16)
    trib = AR.alloc([128], BF16)
    pastm = AR.alloc([4, 16], F32)
    cst = AR.alloc([8], F32)
    lruc = AR.alloc([DEPTH, 8], F32)
    wabd = AR.alloc([2, 4 * 128], BF16)
    gtail = AR.alloc([24, 2], F32)
    RING_SLOTS = 8
    SLOT = 4096
    ring_off = (AR.top + GRAN - 1) // GRAN * GRAN
    AR.top = ring_off + RING_SLOTS * SLOT
    ring_state = {"i": 0}

    def ring_alloc(shape, dt=BF16):
        n = 1
        for s_ in shape:
            n *= s_
        nb = n * 2
        ns = (nb + SLOT - 1) // SLOT
        i = ring_state["i"]
        if i + ns > RING_SLOTS:
            i = 0
        ring_state["i"] = i + ns
        return SBT(AR.t, AR.gran, ring_off + i * SLOT, list(shape), dt)

    phase_base = AR.mark()

    def dbg(name, sbt_ap, deps, dram_ap=None):
        if debug and name in dbg_d:
            P.dma("sp", dbg_d[name] if dram_ap is None else dram_ap, sbt_ap, reads=deps)

    def mm_group(ps_ap, ps_deps, pairs, rdeps):
        n = len(pairs)
        for i, (l_ap, r_ap) in enumerate(pairs):
            P.op("pe", (lambda h, l_ap=l_ap, r_ap=r_ap, i=i: h.matmul(ps_ap, l_ap, r_ap, start=(i == 0), stop=(i == n - 1))),
                 reads=rdeps if i == 0 else (), writes=ps_deps, inc=(i == n - 1))

    def wload(dst, src_ap):
        P.dma("pool", dst.v, src_ap, writes=dst.dall())

    P.dma("sp", pv.v, pvec_d.rearrange("l p n -> p l n"), writes=pv.dall())
    P.dma("sp", identf.v, cident_d, writes=identf.dall())
    P.dma("pool", identb.v, cident_d, writes=identb.dall())
    P.dma("pool", trib.v, ctri_d, writes=trib.dall())
    P.dma("sp", pastm.v, cpast_d, writes=pastm.dall())
    P.op("dve", lambda h: h.memset(onesf.v, 1.0), writes=onesf.dall())
    P.op("dve", lambda h: h.memset(onesb.v, 1.0), writes=onesb.dall())
    P.op("dve", lambda h: h.memset(cst.v[:, 0:1], 1e-6), writes=cst.dall())
    P.op("dve", lambda h: h.memset(cst.v[:, 1:2], 1.0), writes=cst.dall())
    P.op("dve", lambda h: h.memset(gtail.v, 0.0), writes=gtail.dall())

    for l in range(n_layers):
        m0 = AR.mark()
        t_ = AR.alloc([4], F32); e_ = AR.alloc([4], F32); z_ = AR.alloc([4], F32)
        z2 = AR.alloc([4], F32); pl = AR.alloc([4], F32); ab = AR.alloc([4], F32)
        lam = pv.v[:, l, PV_LAM:PV_LAM + 4]
        dd = t_.dall() + e_.dall() + z_.dall() + z2.dall() + pl.dall() + ab.dall() + pv.dall() + lruc.dall()
        P.op("dve", lambda h, lam=lam: h.tensor_scalar(t_.v, lam, -1.0, None, op0=ALU.mult), dd, dd)
        P.op("dve", lambda h, lam=lam: h.tensor_tensor(ab.v, t_.v, lam, op=ALU.max), dd, dd)
        P.op("act", lambda h: h.activation(out=e_.v, in_=ab.v, func=AF.Exp, scale=-1.0), dd, dd)
        P.op("dve", lambda h: h.tensor_scalar(z_.v, e_.v, 2.0, None, op0=ALU.add), dd, dd)
        P.op("dve", lambda h: h.reciprocal(z_.v, z_.v), dd, dd)
        P.op("dve", lambda h: h.tensor_tensor(z_.v, z_.v, e_.v, op=ALU.mult), dd, dd)
        P.op("dve", lambda h: h.tensor_tensor(z2.v, z_.v, z_.v, op=ALU.mult), dd, dd)
        P.op("dve", lambda h: h.tensor_scalar(pl.v, z2.v, 1.0 / 13, 1.0 / 11, op0=ALU.mult, op1=ALU.add), dd, dd)
        for cf in (1.0 / 9, 1.0 / 7, 1.0 / 5, 1.0 / 3, 1.0):
            P.op("dve", lambda h: h.tensor_tensor(pl.v, pl.v, z2.v, op=ALU.mult), dd, dd)
            P.op("dve", lambda h, cf=cf: h.tensor_scalar(pl.v, pl.v, cf, None, op0=ALU.add), dd, dd)
        P.op("dve", lambda h: h.tensor_tensor(pl.v, pl.v, z_.v, op=ALU.mult), dd, dd)
        P.op("dve", lambda h: h.tensor_scalar(pl.v, pl.v, 2.0, None, op0=ALU.mult), dd, dd)
        P.op("dve", lambda h: h.tensor_scalar(t_.v, t_.v, 0.0, None, op0=ALU.max), dd, dd)
        P.op("dve", lambda h: h.tensor_tensor(pl.v, pl.v, t_.v, op=ALU.add), dd, dd)
        P.op("dve", lambda h, l=l: h.tensor_scalar(lruc.v[:, l, 0:4], pl.v, -8.0, None, op0=ALU.mult), dd, dd)
        P.op("dve", lambda h, l=l: h.tensor_scalar(lruc.v[:, l, 4:8], pl.v, -16.0, None, op0=ALU.mult), dd, dd)
        AR.release(m0)

    P.marks = []

    def mark(name):
        P.marks.append((name, len(P.streams['pe']), len(P.streams['act']), len(P.streams['dve'])))

    R_MM = Ring([0, 1, 2, 3])
    R_AUX = Ring([4, 5])
    R_ACC = Ring([6, 7])

    def rmsnorm_to_hT(gain_col, l):
        m0 = AR.mark()
        sq = [AR.alloc([TT], F32) for _ in range(2)]
        rs = [AR.alloc([TT], F32) for _ in range(2)]
        for j in range(NT):
            t0 = j * TT
            ps, pd = R_MM.next()
            for c in range(8):
                s_ = sq[c % 2]
                P.op("act", lambda h, s_=s_, c=c: h.activation(out=s_.v, in_=xT.v[:, c, t0:t0 + TT], func=AF.Square),
                     reads=xT.d2(c, t0, t0 + TT), writes=s_.dall())
                P.op("pe", lambda h, s_=s_, c=c, ps=ps: h.matmul(ps[:, :], onesf.v, s_.v, start=(c == 0), stop=(c == 7)),
                     reads=s_.dall() + onesf.dall(), writes=pd)
            r_ = rs[j % 2]
            P.op("act", lambda h, r_=r_, ps=ps: h.activation(out=r_.v, in_=ps[:, :], func=AF.Sqrt, scale=1.0 / D, bias=cst.v[:, 0:1]),
                 reads=pd + cst.dall(), writes=r_.dall())
            P.op("dve", lambda h, r_=r_: h.reciprocal(r_.v, r_.v), reads=r_.dall(), writes=r_.dall())
            for c in range(8):
                P.op("dve", lambda h, r_=r_, c=c: h.scalar_tensor_tensor(
                    out=hT.v[:, c, t0:t0 + TT], in0=xT.v[:, c, t0:t0 + TT], scalar=pv.v[:, l, gain_col + c:gain_col + c + 1],
                    in1=r_.v, op0=ALU.mult, op1=ALU.mult),
                    reads=xT.d2(c, t0, t0 + TT) + r_.dall() + pv.dall(), writes=hT.d2(c, t0, t0 + TT))
        AR.release(m0)

    def hT_rhs(k, t0, n):
        return hT.v[:, k, t0:t0 + n]

    def hT_deps(t0, n):
        return hT.d2s(range(8), t0, t0 + n)

    def load_x(b):
        m0 = AR.mark()
        stg = [AR.alloc([D], F32) for _ in range(2)]
        for tt in range(S // 128):
            s_ = stg[tt % 2]
            P.dma("sp", s_.v, x_d[b, tt * 128:(tt + 1) * 128, :], writes=s_.dall())
            for g in range(2):
                ps, pd = R_MM.next()
                for cc in range(4):
                    c = g * 4 + cc
                    P.op("pe", lambda h, s_=s_, c=c, cc=cc, ps=ps: h.transpose(ps[:, cc * 128:(cc + 1) * 128], s_.v[:, c * 128:(c + 1) * 128], identf.v),
                         reads=s_.dall() + identf.dall(), writes=pd, inc=(cc == 3))
                eng = "act" if g == 0 else "dve"
                outap = xT.v[:, g * 4:(g + 1) * 4, tt * 128:(tt + 1) * 128]
                inap = ps[:, :].rearrange("p (a b) -> p a b", a=4)
                wd = xT.d2s(range(g * 4, g * 4 + 4), tt * 128, (tt + 1) * 128)
                if eng == "act":
                    P.op("act", lambda h, outap=outap, inap=inap: h.copy(out=outap, in_=inap), reads=pd, writes=wd)
                else:
                    P.op("dve", lambda h, outap=outap, inap=inap: h.tensor_copy(out=outap, in_=inap), reads=pd, writes=wd)
        AR.release(m0)

    def final_store(b):
        m0 = AR.mark()
        sq = [AR.alloc([TT], F32) for _ in range(2)]
        rs = [AR.alloc([TT], F32) for _ in range(2)]
        nrm = [AR.alloc([8, TT], F32) for _ in range(2)]
        stg = [AR.alloc([D], F32) for _ in range(2)]
        si = 0
        for j in range(NT):
            t0 = j * TT
            ps, pd = R_MM.next()
            for c in range(8):
                s_ = sq[c % 2]
                P.op("act", lambda h, s_=s_, c=c: h.activation(out=s_.v, in_=xT.v[:, c, t0:t0 + TT], func=AF.Square),
                     reads=xT.d2(c, t0, t0 + TT), writes=s_.dall())
                P.op("pe", lambda h, s_=s_, c=c, ps=ps: h.matmul(ps[:, :], onesf.v, s_.v, start=(c == 0), stop=(c == 7)),
                     reads=s_.dall() + onesf.dall(), writes=pd)
            r_ = rs[j % 2]
            P.op("act", lambda h, r_=r_, ps=ps: h.activation(out=r_.v, in_=ps[:, :], func=AF.Sqrt, scale=1.0 / D, bias=cst.v[:, 0:1]),
                 reads=pd + cst.dall(), writes=r_.dall())
            P.op("dve", lambda h, r_=r_: h.reciprocal(r_.v, r_.v), reads=r_.dall(), writes=r_.dall())
            n_ = nrm[j % 2]
            for c in range(8):
                P.op("dve", lambda h, r_=r_, c=c, n_=n_: h.scalar_tensor_tensor(
                    out=n_.v[:, c, :], in0=xT.v[:, c, t0:t0 + TT], scalar=pv.v[:, 0, PV_FING + c:PV_FING + c + 1],
                    in1=r_.v, op0=ALU.mult, op1=ALU.mult),
                    reads=xT.d2(c, t0, t0 + TT) + r_.dall() + pv.dall(), writes=n_.d2(c, 0, TT))
            for sub in range(4):
                s_ = stg[si % 2]
                si += 1
                for g in range(2):
                    ps2, pd2 = R_MM.next()
                    for cc in range(4):
                        c = g * 4 + cc
                        P.op("pe", lambda h, n_=n_, c=c, cc=cc, ps2=ps2, sub=sub: h.transpose(
                            ps2[:, cc * 128:(cc + 1) * 128], n_.v[:, c, sub * 128:(sub + 1) * 128], identf.v),
                            reads=n_.d2(c, 0, TT) + identf.dall(), writes=pd2, inc=(cc == 3))
                    if g == 0:
                        P.op("act", lambda h, s_=s_, ps2=ps2: h.copy(out=s_.v[:, 0:512], in_=ps2[:, :]), reads=pd2, writes=s_.dall())
                    else:
                        P.op("dve", lambda h, s_=s_, ps2=ps2: h.tensor_copy(out=s_.v[:, 512:1024], in_=ps2[:, :]), reads=pd2, writes=s_.dall())
                P.dma("sp", out_d[b, t0 + sub * 128:t0 + (sub + 1) * 128, :], s_.v, reads=s_.dall())
        AR.release(m0)

    def lru_phase(l, yA):
        m0 = AR.mark()
        xat = [AR.alloc([4, 3 + TT], F32) for _ in range(2)]
        P.op("dve", lambda h: h.memset(xat[0].v[:, :, 0:3], 0.0), writes=xat[0].dall())
        P.dma("pool", wabd.v, wabd_d[l].rearrange("g p c o -> p g (c o)"), writes=wabd.dall())
        wga = ring_alloc([8, 512])
        wload(wga, w_in_d[l, :, 512:1024].rearrange("(k p) n -> p k n", p=128))
        wxa = ring_alloc([8, 512])
        wload(wxa, w_in_d[l, :, 0:512].rearrange("(k p) n -> p k n", p=128))
        for j in range(NT):
            t0 = j * TT
            for c in range(4):
                ps, pd = R_MM.next()
                mm_group(ps[:, :], pd, [(wga.v[:, k, c * 128:(c + 1) * 128], hT_rhs(k, t0, TT)) for k in range(8)],
                         wga.dall() + hT_deps(t0, TT))
                P.op("act", lambda h, ps=ps, c=c, t0=t0: h.activation(out=yA.v[:, c, t0:t0 + TT], in_=ps[:, :], func=GELU),
                     reads=pd, writes=yA.d2(c, t0, t0 + TT))
        NS = 2
        tmp = {}
        for nm in ("xc", "r", "i", "a", "m"):
            tmp[nm] = [AR.alloc([TT], F32) for _ in range(NS)]
        tmp["xb"] = [AR.alloc([TT], BF16) for _ in range(NS)]
        carry = AR.alloc([4], F32)
        r_gate = Ring([4, 5, 6, 7])
        iters = [(j, c) for j in range(NT) for c in range(4)]
        ctx = {}

        def head(idx):
            j, c = iters[idx]
            t0 = j * TT
            xa = xat[j % 2]
            xap = xat[(j - 1) % 2]
            q = idx % NS
            xc, xb = tmp["xc"][q], tmp["xb"][q]
            if j > 0:
                P.op("act", lambda h: h.copy(out=xa.v[:, c, 0:3], in_=xap.v[:, c, TT:TT + 3]),
                     reads=xap.d2(c, TT, TT + 3), writes=xa.d2(c, 0, 3))
            ps, pd = R_MM.next()
            mm_group(ps[:, :], pd, [(wxa.v[:, k, c * 128:(c + 1) * 128], hT_rhs(k, t0, TT)) for k in range(8)],
                     wxa.dall() + hT_deps(t0, TT))
            P.op("act", lambda h: h.copy(out=xa.v[:, c, 3:3 + TT], in_=ps[:, :]), reads=pd, writes=xa.d2(c, 3, 3 + TT))
            cw = lambda k: pv.v[:, l, PV_LCW + k * 4 + c:PV_LCW + k * 4 + c + 1]
            cb = pv.v[:, l, PV_LCB + c:PV_LCB + c + 1]
            xin = lambda k: xa.v[:, c, k:k + TT]
            xdeps = xa.d2(c, 0, TT + 3) + pv.dall()
            P.op("dve", lambda h: h.tensor_scalar(xc.v, xin(3), cw(3), cb, op0=ALU.mult, op1=ALU.add), reads=xdeps, writes=xc.dall())
            for k in range(3):
                P.op("dve", lambda h, k=k: h.scalar_tensor_tensor(out=xc.v, in0=xin(k), scalar=cw(k), in1=xc.v, op0=ALU.mult, op1=ALU.add),
                     reads=xdeps + xc.dall(), writes=xc.dall())
            P.op("dve", lambda h: h.tensor_copy(out=xb.v, in_=xc.v), reads=xc.dall(), writes=xb.dall())
            psa, pda = r_gate.next()
            P.op("pe", lambda h: h.matmul(psa[:, :], wabd.v[:, 0, c * 128:(c + 1) * 128], xb.v, start=True, stop=True),
                 reads=xb.dall() + wabd.dall(), writes=pda)
            psx, pdx = r_gate.next()
            P.op("pe", lambda h: h.matmul(psx[:, :], wabd.v[:, 1, c * 128:(c + 1) * 128], xb.v, start=True, stop=True),
                 reads=xb.dall() + wabd.dall(), writes=pdx)
            ctx[idx] = (psa, pda, psx, pdx)

        def tail(idx):
            j, c = iters[idx]
            t0 = j * TT
            q = idx % NS
            xc, r_, i_, a_, m_ = (tmp[n][q] for n in ("xc", "r", "i", "a", "m"))
            psa, pda, psx, pdx = ctx.pop(idx)
            P.op("act", lambda h: h.activation(out=r_.v, in_=psa[:, :], func=AF.Sigmoid, bias=pv.v[:, l, PV_LBA + c:PV_LBA + c + 1]),
                 reads=pda + pv.dall(), writes=r_.dall())
            P.op("act", lambda h: h.activation(out=i_.v, in_=psx[:, :], func=AF.Sigmoid, bias=pv.v[:, l, PV_LBX + c:PV_LBX + c + 1]),
                 reads=pdx + pv.dall(), writes=i_.dall())
            P.op("act", lambda h: h.activation(out=a_.v, in_=r_.v, func=AF.Exp, scale=lruc.v[:, l, c:c + 1]),
                 reads=r_.dall() + lruc.dall(), writes=a_.dall())
            P.op("act", lambda h: h.activation(out=m_.v, in_=r_.v, func=AF.Exp, scale=lruc.v[:, l, 4 + c:5 + c]),
                 reads=r_.dall() + lruc.dall(), writes=m_.dall())
            P.op("act", lambda h: h.activation(out=m_.v, in_=m_.v, func=AF.Ln, scale=-1.0, bias=cst.v[:, 1:2]),
                 reads=m_.dall() + cst.dall(), writes=m_.dall())
            P.op("act", lambda h: h.activation(out=m_.v, in_=m_.v, func=AF.Exp, scale=0.5), reads=m_.dall(), writes=m_.dall())
            P.op("dve", lambda h: h.tensor_tensor(i_.v, i_.v, xc.v, op=ALU.mult), reads=i_.dall() + xc.dall(), writes=i_.dall())
            P.op("dve", lambda h: h.tensor_tensor(i_.v, i_.v, m_.v, op=ALU.mult), reads=i_.dall() + m_.dall(), writes=i_.dall())
            hcur = r_
            if j == 0:
                P.op("dve", lambda h: h.tensor_tensor_scan(hcur.v, a_.v, i_.v, 0.0, ALU.mult, ALU.add),
                     reads=a_.dall() + i_.dall(), writes=hcur.dall())
            else:
                P.op("dve", lambda h: h.tensor_tensor_scan(hcur.v, a_.v, i_.v, carry.v[:, c:c + 1], ALU.mult, ALU.add),
                     reads=a_.dall() + i_.dall() + carry.dall(), writes=hcur.dall())
            P.op("dve", lambda h: h.tensor_copy(out=carry.v[:, c:c + 1], in_=hcur.v[:, TT - 1:TT]), reads=hcur.dall(), writes=carry.dall())
            P.op("dve", lambda h: h.tensor_tensor(yA.v[:, c, t0:t0 + TT], yA.v[:, c, t0:t0 + TT], hcur.v, op=ALU.mult),
                 reads=hcur.dall() + yA.d2(c, t0, t0 + TT), writes=yA.d2(c, t0, t0 + TT))

        for idx in range(len(iters)):
            if idx == 0:
                head(0)
            if idx + 1 < len(iters):
                head(idx + 1)
            tail(idx)
        AR.release(m0)

    def xattn_phase(l, b, yC):
        m0 = AR.mark()
        memK = AR.alloc([4, 256], BF16)
        memV = AR.alloc([2, 512], BF16)
        memh = AR.alloc([8, 256], BF16)
        m1 = AR.mark()
        mstg = [AR.alloc([D], F32)] * 2
        memT = AR.alloc([8, 256], F32)
        sq = [AR.alloc([256], F32) for _ in range(2)]
        rs = AR.alloc([256], F32)
        wk = ring_alloc([8, 512])
        wload(wk, w_mkv_d[l, :, 0:512].rearrange("(k p) n -> p k n", p=128))
        wv = ring_alloc([8, 512])
        wload(wv, w_mkv_d[l, :, 512:1024].rearrange("(k p) n -> p k n", p=128))
        wq = ring_alloc([8, 512])
        wload(wq, w_in_d[l, :, 2560:3072].rearrange("(k p) n -> p k n", p=128))
        for mt in range(2):
            s_ = mstg[mt]
            P.dma("sp", s_.v, mem_d[b, mt * 128:(mt + 1) * 128, :], writes=s_.dall())
            for g in range(2):
                ps, pd = R_MM.next()
                for cc in range(4):
                    c = g * 4 + cc
                    P.op("pe", lambda h, s_=s_, c=c, cc=cc, ps=ps: h.transpose(ps[:, cc * 128:(cc + 1) * 128], s_.v[:, c * 128:(c + 1) * 128], identf.v),
                         reads=s_.dall() + identf.dall(), writes=pd, inc=(cc == 3))
                outap = memT.v[:, g * 4:(g + 1) * 4, mt * 128:(mt + 1) * 128]
                inap = ps[:, :].rearrange("p (a b) -> p a b", a=4)
                P.op("act", lambda h, outap=outap, inap=inap: h.copy(out=outap, in_=inap), reads=pd, writes=memT.dall())
        ps, pd = R_MM.next()
        for c in range(8):
            s_ = sq[c % 2]
            P.op("act", lambda h, s_=s_, c=c: h.activation(out=s_.v, in_=memT.v[:, c, :], func=AF.Square),
                 reads=memT.dall(), writes=s_.dall())
            P.op("pe", lambda h, s_=s_, c=c, ps=ps: h.matmul(ps[:, 0:256], onesf.v, s_.v, start=(c == 0), stop=(c == 7)),
                 reads=s_.dall() + onesf.dall(), writes=pd)
        P.op("act", lambda h, ps=ps: h.activation(out=rs.v, in_=ps[:, 0:256], func=AF.Sqrt, scale=1.0 / D, bias=cst.v[:, 0:1]),
             reads=pd + cst.dall(), writes=rs.dall())
        P.op("dve", lambda h: h.reciprocal(rs.v, rs.v), reads=rs.dall(), writes=rs.dall())
        for c in range(8):
            P.op("dve", lambda h, c=c: h.scalar_tensor_tensor(
                out=memh.v[:, c, :], in0=memT.v[:, c, :], scalar=pv.v[:, l, PV_MEMG + c:PV_MEMG + c + 1],
                in1=rs.v, op0=ALU.mult, op1=ALU.mult),
                reads=memT.dall() + rs.dall() + pv.dall(), writes=memh.dall())
        for hd in range(4):
            ps, pd = R_MM.next()
            mm_group(ps[:, 0:256], pd, [(wk.v[:, k, hd * 128:(hd + 1) * 128], memh.v[:, k, :]) for k in range(8)],
                     wk.dall() + memh.dall())
            P.op("act", lambda h, ps=ps, hd=hd: h.copy(out=memK.v[:, hd, :], in_=ps[:, 0:256]), reads=pd, writes=memK.dall())
        for mt in range(2):
            ps, pd = R_MM.next()
            mm_group(ps[:, :], pd, [(memh.v[:, k, mt * 128:(mt + 1) * 128], wv.v[:, k, :]) for k in range(8)],
                     wv.dall() + memh.dall())
            P.op("act", lambda h, ps=ps, mt=mt: h.copy(out=memV.v[:, mt, :], in_=ps[:, :]), reads=pd, writes=memV.dall())
        AR.release(m1)
        qx = [AR.alloc([TT], BF16) for _ in range(2)]
        pt = [AR.alloc([2, TT], BF16) for _ in range(2)]
        rd = [AR.alloc([TT], F32) for _ in range(2)]
        sc = 128 ** -0.5
        r_q = Ring([0, 1])
        r_s = Ring([2, 3, 4, 5])
        iters = [(j, hd) for j in range(NT) for hd in range(4)]
        ctx = {}

        def head(idx):
            j, hd = iters[idx]
            t0 = j * TT
            q_ = qx[idx % 2]
            ps, pd = r_q.next()
            mm_group(ps[:, :], pd, [(wq.v[:, k, hd * 128:(hd + 1) * 128], hT_rhs(k, t0, TT)) for k in range(8)],
                     wq.dall() + hT_deps(t0, TT))
            P.op("act", lambda h: h.copy(out=q_.v, in_=ps[:, :]), reads=pd, writes=q_.dall())
            sb = []
            for mt in range(2):
                sps, spd = r_s.next()
                P.op("pe", lambda h, sps=sps, mt=mt: h.matmul(sps[:, :], memK.v[:, hd, mt * 128:(mt + 1) * 128], q_.v, start=True, stop=True),
                     reads=q_.dall() + memK.dall(), writes=spd)
                sb.append((sps, spd))
            ctx[idx] = sb

        def tail(idx):
            j, hd = iters[idx]
            t0 = j * TT
            p_ = pt[idx % 2]; r_ = rd[idx % 2]
            sb = ctx.pop(idx)
            for mt in range(2):
                sps, spd = sb[mt]
                P.op("act", lambda h, sps=sps, mt=mt: h.activation(out=p_.v[:, mt, :], in_=sps[:, :], func=AF.Exp, scale=sc),
                     reads=spd, writes=p_.dall())
            ops, opd = psb[6], [psd[6]]
            mm_group(ops[:, :], opd, [(memV.v[:, mt, hd * 128:(hd + 1) * 128], p_.v[:, mt, :]) for mt in range(2)],
                     memV.dall() + p_.dall())
            dps, dpd = psb[7], [psd[7]]
            mm_group(dps[:, :], dpd, [(onesb.v, p_.v[:, mt, :]) for mt in range(2)], onesb.dall() + p_.dall())
            P.op("act", lambda h: h.activation(out=r_.v, in_=dps[:, :], func=AF.Ln), reads=dpd, writes=r_.dall())
            P.op("act", lambda h: h.activation(out=r_.v, in_=r_.v, func=AF.Exp, scale=-1.0), reads=r_.dall(), writes=r_.dall())
            P.op("dve", lambda h: h.tensor_tensor(yC.v[:, hd, t0:t0 + TT], ops[:, :], r_.v, op=ALU.mult),
                 reads=opd + r_.dall(), writes=yC.d2(hd, t0, t0 + TT))

        for idx in range(len(iters)):
            if idx == 0:
                head(0)
            if idx + 1 < len(iters):
                head(idx + 1)
            tail(idx)
        AR.release(m0)

    def moba_phase(l, yB):
        m0 = AR.mark()
        NSET = 2
        KA = [[AR.alloc([S], BF16) for _ in range(2)] for _ in range(NSET)]
        QA = [[AR.alloc([S], BF16) for _ in range(2)] for _ in range(NSET)]
        VT = AR.alloc([16, 2, 128], BF16)
        NPT = 4
        PT = [AR.alloc([2, 256], BF16) for _ in range(NPT)]
        kmT = [[AR.alloc([8], BF16) for _ in range(2)] for _ in range(NSET)]
        ksum = AR.alloc([8], F32)
        gm = AR.alloc([16], F32)
        top8 = AR.alloc([16], F32)
        stage = [[AR.alloc([2, 128], BF16) for _ in range(2)] for _ in range(4)]
        dsh = [AR.alloc([256], F32) for _ in range(2)]
        r_proj = Ring([0, 1])
        r_sc = Ring([3, 4, 5])
        r_acc = Ring([6, 7])
        gate_bank, gate_dep = psb[2], [psd[2]]
        for s_ in range(NSET):
            for t_ in KA[s_] + QA[s_]:
                P.op("dve", lambda h, t_=t_: h.memset(t_.v, 0.0), writes=t_.dall())
            for hh in range(2):
                P.dma("pool", QA[s_][hh].v[64:68, :], cqrow_d, writes=QA[s_][hh].dall())
                P.dma("pool", KA[s_][hh].v[96:104, :], coneh_d, writes=KA[s_][hh].dall())
        P.op("dve", lambda h: h.memset(VT.v, 1.0), writes=VT.dall())
        for q4 in range(4):
            for sub in range(2):
                t_ = stage[q4][sub]
                P.op("dve", lambda h, t_=t_: h.memset(t_.v, 0.0), writes=t_.dall())
        state = {"pti": 0, "acc": 0, "wv": {}}

        def project(pair):
            s_ = pair % NSET
            ka, qa, km = KA[s_], QA[s_], kmT[s_]
            wq = ring_alloc([8, 128]); wload(wq, w_in_d[l, :, 1024 + pair * 128:1024 + (pair + 1) * 128].rearrange("(k p) n -> p k n", p=128))
            wk = ring_alloc([8, 128]); wload(wk, w_in_d[l, :, 1536 + pair * 128:1536 + (pair + 1) * 128].rearrange("(k p) n -> p k n", p=128))
            for hh in range(2):
                P.dma("pool", ka[hh].v[64:68, :], ckrow_d[pair * 2 + hh], writes=ka[hh].dall())
            for j in range(NT):
                t0 = j * TT
                ps, pd = r_proj.next()
                mm_group(ps[:, :], pd, [(wk.v[:, k, :], hT_rhs(k, t0, TT)) for k in range(8)], wk.dall() + hT_deps(t0, TT))
                for hh in range(2):
                    P.op("act", lambda h, ps=ps, hh=hh: h.copy(out=ka[hh].v[0:64, t0:t0 + TT], in_=ps[hh * 64:(hh + 1) * 64, :]),
                         reads=pd, writes=ka[hh].d(t0, t0 + TT))
                ps, pd = r_proj.next()
                mm_group(ps[:, :], pd, [(wq.v[:, k, :], hT_rhs(k, t0, TT)) for k in range(8)], wq.dall() + hT_deps(t0, TT))
                for hh in range(2):
                    P.op("act", lambda h, ps=ps, hh=hh: h.mul(out=qa[hh].v[0:64, t0:t0 + TT], in_=ps[hh * 64:(hh + 1) * 64, :], mul=0.125),
                         reads=pd, writes=qa[hh].d(t0, t0 + TT))
            for hh in range(2):
                P.op("dve", lambda h, hh=hh: h.tensor_reduce(out=ksum.v[0:64, :], in_=ka[hh].v[0:64, :].rearrange("p (a b) -> p a b", a=8),
                                                              axis=AX.X, op=ALU.add),
                     reads=ka[hh].dall(), writes=ksum.dall())
                P.op("act", lambda h, hh=hh: h.mul(out=km[hh].v[0:64, :], in_=ksum.v[0:64, :], mul=1.0 / 256), reads=ksum.dall(), writes=km[hh].dall())
            for qb in range(4, 8):
                for sub in range(2):
                    qs = qb * 256 + sub * 128
                    gcol = ((qb - 4) * 2 + sub) * 16
                    for hh in range(2):
                        P.op("pe", lambda h, hh=hh, qs=qs, gcol=gcol: h.matmul(gate_bank[:, gcol + hh * 8:gcol + (hh + 1) * 8], qa[hh].v[0:64, qs:qs + 128],
                                                                             km[hh].v[0:64, :], start=True, stop=True),
                             reads=qa[hh].d(qs, qs + 128) + km[hh].dall(), writes=gate_dep)
                    P.op("dve", lambda h, qb=qb, gcol=gcol: h.tensor_tensor(gm.v, gate_bank[:, gcol:gcol + 16], pastm.v[:, qb - 4, :], op=ALU.add),
                         reads=gate_dep + pastm.dall(), writes=gm.dall())
                    st_ = stage[qb - 4][sub]
                    for hh in range(2):
                        P.op("dve", lambda h, hh=hh: h.max(out=top8.v[:, hh * 8:(hh + 1) * 8], in_=gm.v[:, hh * 8:(hh + 1) * 8]),
                             reads=gm.dall(), writes=top8.dall())
                        P.op("dve", lambda h, hh=hh, st_=st_, qb=qb: h.tensor_scalar(
                            st_.v[:, hh, 96:96 + qb], gm.v[:, hh * 8:hh * 8 + qb], top8.v[:, hh * 8 + 2:hh * 8 + 3], -30000.0,
                            op0=ALU.is_lt, op1=ALU.mult),
                            reads=gm.dall() + top8.dall(), writes=st_.dall())

        def project_v(pair):
            vt = VT
            wv = ring_alloc([8, 128]); wload(wv, w_in_d[l, :, 2048 + pair * 128:2048 + (pair + 1) * 128].rearrange("(k p) n -> p k n", p=128))
            for j in range(NT):
                ps, pd = r_proj.next()
                for sub in range(4):
                    tk = j * 4 + sub
                    mm_group(ps[:, sub * 128:(sub + 1) * 128], pd, [(hT_rhs(k, tk * 128, 128), wv.v[:, k, :]) for k in range(8)],
                             wv.dall() + hT_deps(tk * 128, 128))
                pv4 = ps[:, :].rearrange("p (a b) -> p a b", a=4)
                P.op("dve", lambda h, pv4=pv4, j=j: h.tensor_copy(out=vt.v[:, j * 4:(j + 1) * 4, 0, 0:64], in_=pv4[:, :, 0:64]),
                     reads=pd, writes=vt.d2s(range(j * 4, j * 4 + 4), 0, 256))
                P.op("dve", lambda h, pv4=pv4, j=j: h.tensor_copy(out=vt.v[:, j * 4:(j + 1) * 4, 1, 64:128], in_=pv4[:, :, 64:128]),
                     reads=pd, writes=vt.d2s(range(j * 4, j * 4 + 4), 0, 256))

        def mask_rows(pair):
            s_ = pair % NSET
            qa = QA[s_]
            for qb in range(4, 8):
                for sub in range(2):
                    qs = qb * 256 + sub * 128
                    st_ = stage[qb - 4][sub]
                    tps, tpd = r_sc.next()
                    tpb = tps[:, :].bitcast(BF16)
                    for hh in range(2):
                        P.op("pe", lambda h, tpb=tpb, st_=st_, hh=hh: h.transpose(tpb[:, hh * 128:(hh + 1) * 128], st_.v[:, hh, :], identb.v),
                             reads=st_.dall() + identb.dall(), writes=tpd, inc=(hh == 1))
                    for hh in range(2):
                        P.op("act", lambda h, tpb=tpb, hh=hh, qs=qs: h.copy(out=qa[hh].v[96:104, qs:qs + 128], in_=tpb[96:104, hh * 128:(hh + 1) * 128]),
                             reads=tpd, writes=qa[hh].d(qs, qs + 128))

        def attention(pair, qbs):
            s_ = pair % NSET
            ka, qa, vt = KA[s_], QA[s_], VT
            units = []
            for qb in qbs:
                q0 = qb * 256
                for hh in range(2):
                    ai = state["acc"]; state["acc"] += 1
                    ops, opd = r_acc.next()
                    ds_ = dsh[ai % 2]
                    nun = qb + 1
                    for ui in range(nun):
                        units.append(dict(qb=qb, q0=q0, hh=hh, ui=ui, nun=nun, ops=ops, opd=opd, ds=ds_))
            for u in units:
                u["p"] = PT[state["pti"] % NPT]; state["pti"] += 1
                u["sps"], u["spd"] = None, None

            def emit_qk(u):
                sps, spd = r_sc.next()
                u["sps"], u["spd"] = sps, spd
                hh, q0, qb = u["hh"], u["q0"], u["qb"]
                if u["ui"] == 0:
                    k0 = 2 * qb
                    P.op("pe", lambda h: h.matmul(sps[:, 0:256], ka[hh].v[:, k0 * 128:(k0 + 1) * 128], qa[hh].v[:, q0:q0 + 256], start=True, stop=False),
                         reads=ka[hh].d(k0 * 128, (k0 + 1) * 128) + qa[hh].d(q0, q0 + 256) + trib.dall() + identb.dall(), writes=spd, inc=False)
                    P.op("pe", lambda h: h.matmul(sps[:, 0:128], identb.v, trib.v, start=False, stop=True), writes=spd, inc=False)
                    P.op("pe", lambda h: h.matmul(sps[:, 384:512], ka[hh].v[:, (k0 + 1) * 128:(k0 + 2) * 128], qa[hh].v[:, q0 + 128:q0 + 256], start=True, stop=False),
                         reads=ka[hh].d((k0 + 1) * 128, (k0 + 2) * 128) + qa[hh].d(q0, q0 + 256), writes=spd, inc=False)
                    P.op("pe", lambda h: h.matmul(sps[:, 384:512], identb.v, trib.v, start=False, stop=True), writes=spd)
                else:
                    kp = u["ui"] - 1
                    for i2 in range(2):
                        kt = 2 * kp + i2
                        P.op("pe", lambda h, kt=kt, i2=i2: h.matmul(sps[:, i2 * 256:(i2 + 1) * 256], ka[hh].v[:, kt * 128:(kt + 1) * 128],
                                                                  qa[hh].v[:, q0:q0 + 256], start=True, stop=True),
                             reads=ka[hh].d(kt * 128, (kt + 1) * 128) + qa[hh].d(q0, q0 + 256), writes=spd, inc=(i2 == 1))

            def emit_rest(u):
                sps, spd, p_ = u["sps"], u["spd"], u["p"]
                hh, q0, qb, ops, opd = u["hh"], u["q0"], u["qb"], u["ops"], u["opd"]
                last = (u["ui"] == u["nun"] - 1)
                if u["ui"] == 0:
                    k0 = 2 * qb
                    P.op("act", lambda h: h.activation(out=p_.v[:, 0, :], in_=sps[:, 0:256], func=AF.Exp), reads=spd, writes=p_.dall())
                    P.op("act", lambda h: h.activation(out=p_.v[:, 1, 128:256], in_=sps[:, 384:512], func=AF.Exp), reads=spd, writes=p_.dall())
                    P.op("pe", lambda h: h.matmul(ops[:, 0:256], vt.v[:, k0, hh, :], p_.v[:, 0, :], start=True, stop=False),
                         reads=vt.d2(k0, 0, 256) + p_.dall(), writes=opd, inc=False)
                    P.op("pe", lambda h: h.matmul(ops[:, 128:256], vt.v[:, k0 + 1, hh, :], p_.v[:, 1, 128:256], start=False, stop=last),
                         reads=vt.d2(k0 + 1, 0, 256) + p_.dall(), writes=opd, inc=True)
                else:
                    kp = u["ui"] - 1
                    P.op("act", lambda h: h.activation(out=p_.v.rearrange("p a b -> p (a b)"), in_=sps[:, :], func=AF.Exp),
                         reads=spd, writes=p_.dall())
                    for i2 in range(2):
                        kt = 2 * kp + i2
                        P.op("pe", lambda h, kt=kt, i2=i2: h.matmul(ops[:, 0:256], vt.v[:, kt, hh, :], p_.v[:, i2, :], start=False, stop=(last and i2 == 1)),
                             reads=vt.d2(kt, 0, 256) + p_.dall(), writes=opd, inc=(i2 == 1))
                if last:
                    ds_ = u["ds"]
                    olo, dlo = (0, 64) if hh == 0 else (64, 0)
                    P.op("act", lambda h: h.activation(out=ds_.v[olo:olo + 64, :], in_=ops[dlo:dlo + 64, 0:256], func=AF.Ln), reads=opd, writes=ds_.dall())
                    P.op("act", lambda h: h.activation(out=ds_.v[olo:olo + 64, :], in_=ds_.v[olo:olo + 64, :], func=AF.Exp, scale=-1.0), reads=ds_.dall(), writes=ds_.dall())
                    P.op("dve", lambda h: h.tensor_tensor(yB.v[olo:olo + 64, pair, q0:q0 + 256], ops[olo:olo + 64, 0:256], ds_.v[olo:olo + 64, :], op=ALU.mult),
                         reads=opd + ds_.dall(), writes=yB.d2(pair, q0, q0 + 256))

            DEPTH_QK = 2
            for i in range(min(DEPTH_QK, len(units))):
                emit_qk(units[i])
            for i, u in enumerate(units):
                if i + DEPTH_QK < len(units):
                    emit_qk(units[i + DEPTH_QK])
                emit_rest(u)

        project(0)
        for pair in range(4):
            project_v(pair)
            attention(pair, range(0, 4))
            mask_rows(pair)
            if pair + 1 < 4:
                project(pair + 1)
            attention(pair, range(4, 8))
        AR.release(m0)

    def merge_phase(l, yA, yB, yC):
        m0 = AR.mark()
        mg = AR.alloc([8, 1024], BF16)
        NGT = 2 if (AR.nbytes - AR.top) >= 2 * 3 * 2048 + 1024 else 1
        gt = [[AR.alloc([TT], F32) for _ in range(3)] for _ in range(NGT)]
        gi = 0
        ys = [yA, yB, yC]
        gview = w_in_d[l, :, 3072:6144].rearrange("(k p) (n d) -> p k n d", p=128, n=3)
        bview = w_br_d[l].rearrange("n (k p) d -> p k n d", p=128)
        for hf in range(2):
            for dc in range(8):
                G = ring_alloc([8, 3, 128])
                B = ring_alloc([4, 3, 128])
                for n in range(3):
                    P.dma("pool", G.v[:, :, n, :], gview[:, :, n, dc * 128:(dc + 1) * 128], writes=G.dall())
                    P.dma("pool", B.v[:, :, n, :], bview[:, :, n, dc * 128:(dc + 1) * 128], writes=B.dall())
                for jj in range(2):
                    t0 = hf * 1024 + jj * TT
                    gts = gt[gi % NGT]; gi += 1
                    for n in range(3):
                        ps, pd = R_MM.next()
                        mm_group(ps[:, :], pd, [(G.v[:, k, n, :], hT_rhs(k, t0, TT)) for k in range(8)],
                                 G.dall() + hT_deps(t0, TT))
                        P.op("act", lambda h, ps=ps, n=n, gts=gts: h.activation(out=gts[n].v, in_=ps[:, :], func=AF.Sigmoid), reads=pd, writes=gts[n].dall())
                        ps2, pd2 = R_MM.next()
                        mm_group(ps2[:, :], pd2, [(B.v[:, k, n, :], ys[n].v[:, k, t0:t0 + TT]) for k in range(4)],
                                 B.dall() + ys[n].d2s(range(4), t0, t0 + TT))
                        P.op("dve", lambda h, ps2=ps2, n=n, gts=gts: h.tensor_tensor(gts[n].v, ps2[:, :], gts[n].v, op=ALU.mult),
                             reads=pd2 + gts[n].dall(), writes=gts[n].dall())
                    P.op("dve", lambda h, gts=gts: h.tensor_tensor(gts[0].v, gts[0].v, gts[1].v, op=ALU.add),
                         reads=gts[0].dall() + gts[1].dall(), writes=gts[0].dall())
                    P.op("dve", lambda h, dc=dc, jj=jj, gts=gts: h.tensor_tensor(mg.v[:, dc, jj * TT:(jj + 1) * TT], gts[0].v, gts[2].v, op=ALU.add),
                         reads=gts[0].dall() + gts[2].dall(), writes=mg.d2(dc, jj * TT, (jj + 1) * TT))
            for pp in range(2):
                Wo = ring_alloc([8, 512])
                wload(Wo, w_out_d[l, :, pp * 512:(pp + 1) * 512].rearrange("(k p) n -> p k n", p=128))
                for jj in range(2):
                    t0 = hf * 1024 + jj * TT
                    for dci in range(4):
                        dc = pp * 4 + dci
                        ps, pd = R_MM.next()
                        mm_group(ps[:, :], pd, [(Wo.v[:, k, dci * 128:(dci + 1) * 128], mg.v[:, k, jj * TT:(jj + 1) * TT]) for k in range(8)],
                                 Wo.dall() + mg.d2s(range(8), jj * TT, (jj + 1) * TT))
                        P.op("dve", lambda h, ps=ps, dc=dc, t0=t0: h.tensor_tensor(xT.v[:, dc, t0:t0 + TT], ps[:, :], xT.v[:, dc, t0:t0 + TT], op=ALU.add),
                             reads=pd + xT.d2(dc, t0, t0 + TT), writes=xT.d2(dc, t0, t0 + TT))
        AR.release(m0)

    def ffn_phase(l):
        m0 = AR.mark()
        act = AR.alloc([24, 1024], BF16)
        gbuf = [AR.alloc([2 + 1024], F32) for _ in range(2)]
        acc = [AR.alloc([TT], F32) for _ in range(2)]
        gel = [AR.alloc([TT], F32) for _ in range(2)]
        it = 0
        for hf in range(2):
            for ffg in range(12):
                Wg = ring_alloc([8, 256]); wload(Wg, w_fg_d[l, :, ffg * 256:(ffg + 1) * 256].rearrange("(k p) n -> p k n", p=128))
                Wu = ring_alloc([8, 256]); wload(Wu, w_fu_d[l, :, ffg * 256:(ffg + 1) * 256].rearrange("(k p) n -> p k n", p=128))
                for fci in range(2):
                    fc = ffg * 2 + fci
                    gb = gbuf[fc % 2]
                    if hf == 0:
                        P.op("dve", lambda h, gb=gb: h.memset(gb.v[:, 0:2], 0.0), writes=gb.d(0, 2))
                    else:
                        P.op("dve", lambda h, gb=gb, fc=fc: h.tensor_copy(out=gb.v[:, 0:2], in_=gtail.v[:, fc, :]), reads=gtail.dall(), writes=gb.d(0, 2))
                    for jj in range(2):
                        t0 = hf * 1024 + jj * TT
                        g0 = 2 + jj * TT
                        ps, pd = R_MM.next()
                        mm_group(ps[:, :], pd, [(Wg.v[:, k, fci * 128:(fci + 1) * 128], hT_rhs(k, t0, TT)) for k in range(8)],
                                 Wg.dall() + hT_deps(t0, TT))
                        ups, upd = R_MM.next()
                        mm_group(ups[:, :], upd, [(Wu.v[:, k, fci * 128:(fci + 1) * 128], hT_rhs(k, t0, TT)) for k in range(8)],
                                 Wu.dall() + hT_deps(t0, TT))
                        P.op("act", lambda h, ps=ps, gb=gb, g0=g0: h.copy(out=gb.v[:, g0:g0 + TT], in_=ps[:, :]), reads=pd, writes=gb.d(g0, g0 + TT))
                        a_ = acc[it % 2]; ge = gel[it % 2]; it += 1
                        cw = lambda k, fc=fc: pv.v[:, l, PV_FCW + k * 24 + fc:PV_FCW + k * 24 + fc + 1]
                        cb = pv.v[:, l, PV_FCB + fc:PV_FCB + fc + 1]
                        gd = gb.d(g0 - 2, g0 + TT) + pv.dall()
                        P.op("dve", lambda h, a_=a_, gb=gb, g0=g0, cw=cw, cb=cb: h.tensor_scalar(a_.v, gb.v[:, g0:g0 + TT], cw(2), cb, op0=ALU.mult, op1=ALU.add),
                             reads=gd, writes=a_.dall())
                        P.op("dve", lambda h, a_=a_, gb=gb, g0=g0, cw=cw: h.scalar_tensor_tensor(out=a_.v, in0=gb.v[:, g0 - 1:g0 - 1 + TT], scalar=cw(1), in1=a_.v,
                                                                                               op0=ALU.mult, op1=ALU.add),
                             reads=gd + a_.dall(), writes=a_.dall())
                        P.op("dve", lambda h, a_=a_, gb=gb, g0=g0, cw=cw: h.scalar_tensor_tensor(out=a_.v, in0=gb.v[:, g0 - 2:g0 - 2 + TT], scalar=cw(0), in1=a_.v,
                                                                                               op0=ALU.mult, op1=ALU.add),
                             reads=gd + a_.dall(), writes=a_.dall())
                        P.op("act", lambda h, a_=a_, ge=ge: h.activation(out=ge.v, in_=a_.v, func=GELU), reads=a_.dall(), writes=ge.dall())
                        P.op("dve", lambda h, ups=ups, ge=ge, fc=fc, jj=jj: h.tensor_tensor(act.v[:, fc, jj * TT:(jj + 1) * TT], ups[:, :], ge.v, op=ALU.mult),
                             reads=upd + ge.dall(), writes=act.d2(fc, jj * TT, (jj + 1) * TT))
                    if hf == 0:
                        P.op("dve", lambda h, gb=gb, fc=fc: h.tensor_copy(out=gtail.v[:, fc, :], in_=gb.v[:, 1024:1026]), reads=gb.d(1024, 1026), writes=gtail.dall())
            for dcg in range(4):
                Wd = ring_alloc([24, 256]); wload(Wd, w_fd_d[l, :, dcg * 256:(dcg + 1) * 256].rearrange("(k p) n -> p k n", p=128))
                for jj in range(2):
                    t0 = hf * 1024 + jj * TT
                    for dci in range(2):
                        dc = dcg * 2 + dci
                        ps, pd = R_MM.next()
                        mm_group(ps[:, :], pd, [(Wd.v[:, k, dci * 128:(dci + 1) * 128], act.v[:, k, jj * TT:(jj + 1) * TT]) for k in range(24)],
                                 Wd.dall() + act.d2s(range(24), jj * TT, (jj + 1) * TT))
                        P.op("dve", lambda h, ps=ps, dc=dc, t0=t0: h.tensor_tensor(xT.v[:, dc, t0:t0 + TT], ps[:, :], xT.v[:, dc, t0:t0 + TT], op=ALU.add),
                             reads=pd + xT.d2(dc, t0, t0 + TT), writes=xT.d2(dc, t0, t0 + TT))
        AR.release(m0)

    for b in range(n_seq):
        mark('load'); load_x(b)
        for l in range(n_layers):
            mark('norm1'); rmsnorm_to_hT(PV_MIXG, l)
            mY = AR.mark()
            yB = AR.alloc([4, S], BF16)
            mark('moba'); moba_phase(l, yB)
            yA = AR.alloc([4, S], BF16)
            mark('lru'); lru_phase(l, yA)
            yC = AR.alloc([4, S], BF16)
            mark('xattn'); xattn_phase(l, b, yC)
            if debug and b == 0:
                for nm, y_ in (("yA%d" % l, yA), ("yB%d" % l, yB), ("yC%d" % l, yC)):
                    if nm in dbg_d:
                        P.dma("pool", dbg_d[nm].rearrange("(c p) t -> p c t", p=128), y_.v, reads=y_.dall())
            mark('merge'); merge_phase(l, yA, yB, yC)
            AR.release(mY)
            if debug and b == 0 and ("xmid%d" % l) in dbg_d:
                P.dma("sp", dbg_d["xmid%d" % l].rearrange("(c p) t -> p c t", p=128), xT.v, reads=xT.dall())
            mark('norm2'); rmsnorm_to_hT(PV_FFNG, l)
            mark('ffn'); ffn_phase(l)
            if debug and b == 0 and ("xout%d" % l) in dbg_d:
                P.dma("sp", dbg_d["xout%d" % l].rearrange("(c p) t -> p c t", p=128), xT.v, reads=xT.dall())
        mark('final'); final_store(b)
    mark('end')
    P.finish()
    stack.close()
    return nc, P


def host_consts():
    t = np.arange(S)
    c = {}
    c["c_ident"] = np.eye(128, dtype=np.float32)
    k = np.arange(128)
    c["c_tri"] = np.where(k[None, :] >= k[:, None], 0.0, -30000.0).astype(np.float32)
    base = np.stack([t % 256, 256 * (t // 256), np.ones(S), np.ones(S)]).astype(np.float32)
    c["c_krow"] = np.stack([base * (2.0 ** (-(hd + 1))) for hd in range(8)]).astype(np.float32)
    c["c_qrow"] = np.stack([np.ones(S), np.ones(S), -(t % 256), -256 * (t // 256)]).astype(np.float32)
    c["c_onehot"] = (np.arange(8)[:, None] == (t // 256)[None, :]).astype(np.float32)
    past = np.zeros((128, 4, 16), np.float32)
    for q4 in range(4):
        qb = q4 + 4
        for hh in range(2):
            past[:, q4, hh * 8 + qb:hh * 8 + 8] = -1e30
    c["c_past"] = past
    return c


def host_params(inp):
    pvec = np.zeros((DEPTH, 128, PV_N), np.float32)
    wabd = np.zeros((DEPTH, 2, 128, 4, 128), np.float32)
    for l in range(DEPTH):
        pvec[l, :, PV_MIXG:PV_MIXG + 8] = _chunked(inp["mix_norm_gain"][l])
        pvec[l, :, PV_FFNG:PV_FFNG + 8] = _chunked(inp["ffn_norm_gain"][l])
        pvec[l, :, PV_MEMG:PV_MEMG + 8] = _chunked(inp["mem_norm_gain"][l])
        pvec[l, :, PV_FING:PV_FING + 8] = _chunked(inp["final_norm_gain"])
        for k in range(4):
            pvec[l, :, PV_LCW + k * 4:PV_LCW + k * 4 + 4] = _chunked(inp["lru_conv_w"][l, k])
        pvec[l, :, PV_LCB:PV_LCB + 4] = _chunked(inp["lru_conv_b"][l])
        pvec[l, :, PV_LBA:PV_LBA + 4] = _chunked(inp["lru_b_a"][l].reshape(-1))
        pvec[l, :, PV_LBX:PV_LBX + 4] = _chunked(inp["lru_b_x"][l].reshape(-1))
        pvec[l, :, PV_LAM:PV_LAM + 4] = _chunked(inp["lru_lambda"][l])
        for k in range(3):
            pvec[l, :, PV_FCW + k * 24:PV_FCW + k * 24 + 24] = _chunked(inp["ffn_conv_w"][l, k])
        pvec[l, :, PV_FCB:PV_FCB + 24] = _chunked(inp["ffn_conv_b"][l])
        for g, nm in enumerate(("lru_w_a", "lru_w_x")):
            w = inp[nm][l]
            for hd in range(8):
                c, o = hd // 2, (hd % 2) * 64
                wabd[l, g, o:o + 64, c, o:o + 64] = w[hd]
    return pvec, wabd


_CACHE = {}


def kernel(**inputs):
    inp = {k: np.asarray(v) for k, v in inputs.items()}
    if "nc" not in _CACHE:
        _CACHE["nc"] = build_program()[0]
    nc = _CACHE["nc"]
    pvec, wabd = host_params(inp)
    consts = host_consts()
    shared = dict(consts)
    shared.update(pvec=pvec, wabd=wabd)
    for k in ("w_in", "w_mem_kv", "w_branch", "w_out", "w_ffn_gate", "w_ffn_up", "w_ffn_down"):
        shared[k] = np.ascontiguousarray(inp[k], dtype=np.float32)
    in_maps = []
    for c in range(NCORES):
        m = dict(shared)
        m["x"] = np.ascontiguousarray(inp["x"][c * SEQ_PER_CORE:(c + 1) * SEQ_PER_CORE], dtype=np.float32)
        m["mem"] = np.ascontiguousarray(inp["mem"][c * SEQ_PER_CORE:(c + 1) * SEQ_PER_CORE], dtype=np.float32)
        in_maps.append(m)
    res = run_bass_kernel_spmd(nc, in_maps, core_ids=list(range(NCORES)))
    out = np.concatenate([np.asarray(r["out"]) for r in res.results], axis=0)
    return out.astype(np.float32)
```

```python
import math
from contextlib import ExitStack
import numpy as np
import concourse.bass as bass
import concourse.mybir as mybir
from concourse.bass_utils import run_bass_kernel_spmd

F32 = mybir.dt.float32
BF16 = mybir.dt.bfloat16
U8 = mybir.dt.uint8
AF = mybir.ActivationFunctionType
ALU = mybir.AluOpType
AX = mybir.AxisListType

NCORES = 8
SEQ_PER_CORE = 2
S = 2048
D = 1024
DEPTH = 2
NT = 4
TT = 512
GELU = AF.Gelu_apprx_tanh

ENGS = ["pe", "act", "dve", "pool", "sp"]
NDMA = 24
GRAN = 256


class Dep:
    __slots__ = ("w", "rs")

    def __init__(self):
        self.w = {}
        self.rs = {}


class _Rec:
    def __getattr__(self, name):
        def f(*a, **k):
            self.call = (name, a, k)
            return self
        return f


class Prog:
    def __init__(self, nc, stack):
        self.nc = nc
        self.streams = {e: [] for e in ENGS}
        self.cnt = {e: 0 for e in ENGS}
        self.seen = {e: {} for e in ENGS}
        self.sem = {}
        for e in ENGS:
            self.sem[e] = stack.enter_context(nc.semaphore("s_" + e))
        self.dsem = []
        self.dtot = []
        for i in range(NDMA):
            self.dsem.append(stack.enter_context(nc.semaphore("d_%d" % i)))
            self.dtot.append(0)
            self.sem[("dma", i)] = self.dsem[i]
        self.dnext = 0
        self.ninst = 0

    def _need(self, eng, reads, writes):
        need = {}
        for d in reads:
            for k, v in d.w.items():
                if need.get(k, 0) < v:
                    need[k] = v
        for d in writes:
            for k, v in d.w.items():
                if need.get(k, 0) < v:
                    need[k] = v
            for k, v in d.rs.items():
                if need.get(k, 0) < v:
                    need[k] = v
        out = []
        seen = self.seen[eng]
        raw_self = 0
        if eng in ("act", "dve", "pool"):
            for d in reads:
                v = d.w.get(eng, 0)
                if v > raw_self:
                    raw_self = v
        for k, v in need.items():
            if k == eng:
                if raw_self == 0:
                    continue
                v = raw_self
            if seen.get(k, 0) >= v:
                continue
            seen[k] = v
            out.append((self.sem[k], v))
        return out

    @staticmethod
    def _mark(key, val, reads, writes):
        for d in reads:
            if d.rs.get(key, 0) < val:
                d.rs[key] = val
        for d in writes:
            if d.w.get(key, 0) < val:
                d.w[key] = val

    def op(self, eng, fn, reads=(), writes=(), inc=True):
        waits = self._need(eng, reads, writes)
        if inc:
            self.cnt[eng] += 1
            val = self.cnt[eng]
        else:
            val = self.cnt[eng] + 1
        self._mark(eng, val, reads, writes)
        sem = self.sem[eng]
        self.ninst += 1
        rec = _Rec()
        fn(rec)
        cname, cargs, ckw = rec.call

        def thunk(h):
            for s, v in waits[:-1]:
                h.wait_ge(s, v)
            inst = getattr(h, cname)(*cargs, **ckw)
            if waits:
                inst._wait_ge(*waits[-1])
            if inc:
                inst.then_inc(sem, 1)
        self.streams[eng].append(thunk)

    def dma(self, eng, out, in_, reads=(), writes=(), **kw):
        k = self.dnext
        self.dnext = (self.dnext + 1) % NDMA
        key = ("dma", k)
        waits = self._need(eng, reads, writes)
        prev = self.dtot[k]
        if prev > 0 and self.seen[eng].get(key, 0) < prev:
            self.seen[eng][key] = prev
            waits.append((self.dsem[k], prev))
        self.dtot[k] += 16
        val = self.dtot[k]
        self._mark(key, val, reads, writes)
        sem = self.dsem[k]
        self.ninst += 1

        def thunk(h):
            for s, v in waits:
                h.wait_ge(s, v)
            h.dma_start(out=out, in_=in_, **kw).then_inc(sem, 16)
        self.streams[eng].append(thunk)

    def finish(self):
        waits = [(self.dsem[k], self.dtot[k]) for k in range(NDMA) if self.dtot[k] > 0]
        others = [(self.sem[e], self.cnt[e]) for e in ENGS if e != "sp" and self.cnt[e] > 0]

        def fthunk(h):
            for s, v in waits + others:
                h.wait_ge(s, v)
        self.streams["sp"].append(fthunk)
        nc = self.nc
        st = self.streams
        with nc.Block() as block:
            @block.tensor
            def _(h):
                for t in st["pe"]:
                    t(h)

            @block.scalar
            def _(h):
                for t in st["act"]:
                    t(h)

            @block.vector
            def _(h):
                for t in st["dve"]:
                    t(h)

            @block.gpsimd
            def _(h):
                for t in st["pool"]:
                    t(h)

            @block.sync
            def _(h):
                for t in st["sp"]:
                    t(h)


class SBT:
    def __init__(self, arena, gran, off, shape, dt):
        self.esz = 4 if dt == F32 else 2
        self.off = off
        self.shape = shape
        self.dt = dt
        n = 1
        for s_ in shape:
            n *= s_
        self.nbytes = n * self.esz
        v = arena[:, off:off + self.nbytes].bitcast(dt)
        if len(shape) == 2:
            v = v.rearrange("p (a b) -> p a b", a=shape[0])
        elif len(shape) == 3:
            v = v.rearrange("p (a b c) -> p a b c", a=shape[0], b=shape[1])
        self.v = v
        self.gran = gran

    def dall(self):
        return self.gran[self.off // GRAN:(self.off + self.nbytes + GRAN - 1) // GRAN]

    def d(self, lo, hi):
        a = self.off + lo * self.esz
        b = self.off + hi * self.esz
        return self.gran[a // GRAN:(b + GRAN - 1) // GRAN]

    def d2(self, i0, lo, hi):
        n1 = self.shape[-1] if len(self.shape) == 2 else self.shape[1] * self.shape[2]
        return self.d(i0 * n1 + lo, i0 * n1 + hi)

    def d2s(self, i0s, lo, hi):
        out = []
        for i0 in i0s:
            out += self.d2(i0, lo, hi)
        return out

    def d3(self, i0, i1, lo, hi):
        n2 = self.shape[2]
        base = (i0 * self.shape[1] + i1) * n2
        return self.d(base + lo, base + hi)


class Arena:
    def __init__(self, nc, nbytes):
        self.t = nc.alloc_sbuf_tensor("arena", [128, nbytes], U8)
        self.nbytes = nbytes
        self.gran = [Dep() for _ in range((nbytes + GRAN - 1) // GRAN + 1)]
        self.top = 0

    def alloc(self, shape, dt):
        off = (self.top + GRAN - 1) // GRAN * GRAN
        n = 4 if dt == F32 else 2
        for s_ in shape:
            n *= s_
        assert off + n <= self.nbytes, ("SBUF arena overflow", off, n, self.nbytes)
        b = SBT(self.t, self.gran, off, list(shape), dt)
        self.top = off + b.nbytes
        self.peak = max(getattr(self, "peak", 0), self.top)
        return b

    def mark(self):
        return self.top

    def release(self, m):
        self.top = m


PV_MIXG, PV_FFNG, PV_MEMG, PV_FING = 0, 8, 16, 24
PV_LCW, PV_LCB, PV_LBA, PV_LBX, PV_LAM = 32, 48, 52, 56, 60
PV_FCW, PV_FCB = 64, 136
PV_N = 160


def _chunked(v):
    return np.ascontiguousarray(v.reshape(-1, 128).T)


def build_program(n_layers=DEPTH, n_seq=SEQ_PER_CORE, debug=None):
    nc = bass.Bass("TRN2", target_bir_lowering=False)
    dram = {}

    def din(name, shape):
        dram[name] = nc.dram_tensor(name, list(shape), F32, kind="ExternalInput").ap()
        return dram[name]

    x_d = din("x", [SEQ_PER_CORE, S, D])
    mem_d = din("mem", [SEQ_PER_CORE, 256, D])
    w_in_d = din("w_in", [DEPTH, D, 6144])
    w_mkv_d = din("w_mem_kv", [DEPTH, D, 1024])
    w_br_d = din("w_branch", [DEPTH, 3, 512, D])
    w_out_d = din("w_out", [DEPTH, D, D])
    w_fg_d = din("w_ffn_gate", [DEPTH, D, 3072])
    w_fu_d = din("w_ffn_up", [DEPTH, D, 3072])
    w_fd_d = din("w_ffn_down", [DEPTH, 3072, D])
    pvec_d = din("pvec", [DEPTH, 128, PV_N])
    wabd_d = din("wabd", [DEPTH, 2, 128, 4, 128])
    cident_d = din("c_ident", [128, 128])
    ctri_d = din("c_tri", [128, 128])
    ckrow_d = din("c_krow", [8, 4, S])
    cqrow_d = din("c_qrow", [4, S])
    coneh_d = din("c_onehot", [8, S])
    cpast_d = din("c_past", [128, 4, 16])
    out_d = nc.dram_tensor("out", [SEQ_PER_CORE, S, D], F32, kind="ExternalOutput").ap()
    dbg_d = {}
    if debug:
        for name, shape in debug.items():
            dbg_d[name] = nc.dram_tensor("dbg_" + name, list(shape), F32, kind="ExternalOutput").ap()

    stack = ExitStack()
    P = Prog(nc, stack)
    total = nc.sbuf_top - nc.sbuf_base - 64
    AR = Arena(nc, total // GRAN * GRAN - GRAN)

    psb = [nc.alloc_psum_tensor("ps%d" % i, [128, 512], F32) for i in range(8)]
    psd = [Dep() for _ in range(8)]

    class Ring:
        def __init__(self, banks):
            self.banks = banks
            self.i = 0

        def next(self):
            b = self.banks[self.i % len(self.banks)]
            self.i += 1
            return psb[b], [psd[b]]

    xT = AR.alloc([8, S], F32)
    hT = AR.alloc([8, S], BF16)
    pv = AR.alloc([DEPTH, PV_N], F32)
    identf = AR.alloc([128], F32)
    identb = AR.alloc([128], BF16)
    onesf = AR.alloc([128], F32)
    onesb = AR.alloc([128], BF16)
    trib = AR.alloc([128], BF16)
    pastm = AR.alloc([4, 16], F32)
    cst = AR.alloc([8], F32)
    lruc = AR.alloc([DEPTH, 8], F32)
    wabd = AR.alloc([2, 4 * 128], BF16)
    gtail = AR.alloc([24, 2], F32)
    RING_SLOTS = 8
    SLOT = 4096
    ring_off = (AR.top + GRAN - 1) // GRAN * GRAN
    AR.top = ring_off + RING_SLOTS * SLOT
    ring_state = {"i": 0}

    def ring_alloc(shape, dt=BF16):
        n = 1
        for s_ in shape:
            n *= s_
        nb = n * 2
        ns = (nb + SLOT - 1) // SLOT
        i = ring_state["i"]
        if i + ns > RING_SLOTS:
            i = 0
        ring_state["i"] = i + ns
        return SBT(AR.t, AR.gran, ring_off + i * SLOT, list(shape), dt)

    phase_base = AR.mark()

    def dbg(name, sbt_ap, deps, dram_ap=None):
        if debug and name in dbg_d:
            P.dma("sp", dbg_d[name] if dram_ap is None else dram_ap, sbt_ap, reads=deps)

    def mm_group(ps_ap, ps_deps, pairs, rdeps):
        n = len(pairs)
        for i, (l_ap, r_ap) in enumerate(pairs):
            P.op("pe", (lambda h, l_ap=l_ap, r_ap=r_ap, i=i: h.matmul(ps_ap, l_ap, r_ap, start=(i == 0), stop=(i == n - 1))),
                 reads=rdeps if i == 0 else (), writes=ps_deps, inc=(i == n - 1))

    def wload(dst, src_ap):
        P.dma("pool", dst.v, src_ap, writes=dst.dall())

    P.dma("sp", pv.v, pvec_d.rearrange("l p n -> p l n"), writes=pv.dall())
    P.dma("sp", identf.v, cident_d, writes=identf.dall())
    P.dma("pool", identb.v, cident_d, writes=identb.dall())
    P.dma("pool", trib.v, ctri_d, writes=trib.dall())
    P.dma("sp", pastm.v, cpast_d, writes=pastm.dall())
    P.op("dve", lambda h: h.memset(onesf.v, 1.0), writes=onesf.dall())
    P.op("dve", lambda h: h.memset(onesb.v, 1.0), writes=onesb.dall())
    P.op("dve", lambda h: h.memset(cst.v[:, 0:1], 1e-6), writes=cst.dall())
    P.op("dve", lambda h: h.memset(cst.v[:, 1:2], 1.0), writes=cst.dall())
    P.op("dve", lambda h: h.memset(gtail.v, 0.0), writes=gtail.dall())

    for l in range(n_layers):
        m0 = AR.mark()
        t_ = AR.alloc([4], F32); e_ = AR.alloc([4], F32); z_ = AR.alloc([4], F32)
        z2 = AR.alloc([4], F32); pl = AR.alloc([4], F32); ab = AR.alloc([4], F32)
        lam = pv.v[:, l, PV_LAM:PV_LAM + 4]
        dd = t_.dall() + e_.dall() + z_.dall() + z2.dall() + pl.dall() + ab.dall() + pv.dall() + lruc.dall()
        P.op("dve", lambda h, lam=lam: h.tensor_scalar(t_.v, lam, -1.0, None, op0=ALU.mult), dd, dd)
        P.op("dve", lambda h, lam=lam: h.tensor_tensor(ab.v, t_.v, lam, op=ALU.max), dd, dd)
        P.op("act", lambda h: h.activation(out=e_.v, in_=ab.v, func=AF.Exp, scale=-1.0), dd, dd)
        P.op("dve", lambda h: h.tensor_scalar(z_.v, e_.v, 2.0, None, op0=ALU.add), dd, dd)
        P.op("dve", lambda h: h.reciprocal(z_.v, z_.v), dd, dd)
        P.op("dve", lambda h: h.tensor_tensor(z_.v, z_.v, e_.v, op=ALU.mult), dd, dd)
        P.op("dve", lambda h: h.tensor_tensor(z2.v, z_.v, z_.v, op=ALU.mult), dd, dd)
        P.op("dve", lambda h: h.tensor_scalar(pl.v, z2.v, 1.0 / 13, 1.0 / 11, op0=ALU.mult, op1=ALU.add), dd, dd)
        for cf in (1.0 / 9, 1.0 / 7, 1.0 / 5, 1.0 / 3, 1.0):
            P.op("dve", lambda h: h.tensor_tensor(pl.v, pl.v, z2.v, op=ALU.mult), dd, dd)
            P.op("dve", lambda h, cf=cf: h.tensor_scalar(pl.v, pl.v, cf, None, op0=ALU.add), dd, dd)
        P.op("dve", lambda h: h.tensor_tensor(pl.v, pl.v, z_.v, op=ALU.mult), dd, dd)
        P.op("dve", lambda h: h.tensor_scalar(pl.v, pl.v, 2.0, None, op0=ALU.mult), dd, dd)
        P.op("dve", lambda h: h.tensor_scalar(t_.v, t_.v, 0.0, None, op0=ALU.max), dd, dd)
        P.op("dve", lambda h: h.tensor_tensor(pl.v, pl.v, t_.v, op=ALU.add), dd, dd)
        P.op("dve", lambda h, l=l: h.tensor_scalar(lruc.v[:, l, 0:4], pl.v, -8.0, None, op0=ALU.mult), dd, dd)
        P.op("dve", lambda h, l=l: h.tensor_scalar(lruc.v[:, l, 4:8], pl.v, -16.0, None, op0=ALU.mult), dd, dd)
        AR.release(m0)

    P.marks = []

    def mark(name):
        P.marks.append((name, len(P.streams['pe']), len(P.streams['act']), len(P.streams['dve'])))

    R_MM = Ring([0, 1, 2, 3])
    R_AUX = Ring([4, 5])
    R_ACC = Ring([6, 7])

    def rmsnorm_to_hT(gain_col, l, nrs=2):
        m0 = AR.mark()
        sq = [AR.alloc([TT], F32) for _ in range(2)]
        rs = [AR.alloc([TT], F32) for _ in range(nrs)] * (2 // nrs)
        for j in range(NT):
            t0 = j * TT
            ps, pd = R_MM.next()
            for c in range(8):
                s_ = sq[c % 2]
                P.op("act", lambda h, s_=s_, c=c: h.activation(out=s_.v, in_=xT.v[:, c, t0:t0 + TT], func=AF.Square),
                     reads=xT.d2(c, t0, t0 + TT), writes=s_.dall())
                P.op("pe", lambda h, s_=s_, c=c, ps=ps: h.matmul(ps[:, :], onesf.v, s_.v, start=(c == 0), stop=(c == 7)),
                     reads=s_.dall() + onesf.dall(), writes=pd)
            r_ = rs[j % 2]
            P.op("act", lambda h, r_=r_, ps=ps: h.activation(out=r_.v, in_=ps[:, :], func=AF.Sqrt, scale=1.0 / D, bias=cst.v[:, 0:1]),
                 reads=pd + cst.dall(), writes=r_.dall())
            P.op("dve", lambda h, r_=r_: h.reciprocal(r_.v, r_.v), reads=r_.dall(), writes=r_.dall())
            for c in range(8):
                P.op("dve", lambda h, r_=r_, c=c: h.scalar_tensor_tensor(
                    out=hT.v[:, c, t0:t0 + TT], in0=xT.v[:, c, t0:t0 + TT], scalar=pv.v[:, l, gain_col + c:gain_col + c + 1],
                    in1=r_.v, op0=ALU.mult, op1=ALU.mult),
                    reads=xT.d2(c, t0, t0 + TT) + r_.dall() + pv.dall(), writes=hT.d2(c, t0, t0 + TT))
        AR.release(m0)

    def hT_rhs(k, t0, n):
        return hT.v[:, k, t0:t0 + n]

    def hT_deps(t0, n):
        return hT.d2s(range(8), t0, t0 + n)

    def load_x(b):
        m0 = AR.mark()
        stg = [AR.alloc([D], F32) for _ in range(2)]
        for tt in range(S // 128):
            s_ = stg[tt % 2]
            P.dma("sp", s_.v, x_d[b, tt * 128:(tt + 1) * 128, :], writes=s_.dall())
            for g in range(2):
                ps, pd = R_MM.next()
                for cc in range(4):
                    c = g * 4 + cc
                    P.op("pe", lambda h, s_=s_, c=c, cc=cc, ps=ps: h.transpose(ps[:, cc * 128:(cc + 1) * 128], s_.v[:, c * 128:(c + 1) * 128], identf.v),
                         reads=s_.dall() + identf.dall(), writes=pd, inc=(cc == 3))
                eng = "act" if g == 0 else "dve"
                outap = xT.v[:, g * 4:(g + 1) * 4, tt * 128:(tt + 1) * 128]
                inap = ps[:, :].rearrange("p (a b) -> p a b", a=4)
                wd = xT.d2s(range(g * 4, g * 4 + 4), tt * 128, (tt + 1) * 128)
                if eng == "act":
                    P.op("act", lambda h, outap=outap, inap=inap: h.copy(out=outap, in_=inap), reads=pd, writes=wd)
                else:
                    P.op("dve", lambda h, outap=outap, inap=inap: h.tensor_copy(out=outap, in_=inap), reads=pd, writes=wd)
        AR.release(m0)

    def final_store(b):
        m0 = AR.mark()
        sq = [AR.alloc([TT], F32) for _ in range(2)]
        rs = [AR.alloc([TT], F32) for _ in range(2)]
        nrm = [AR.alloc([8, TT], F32) for _ in range(2)]
        stg = [AR.alloc([D], F32) for _ in range(2)]
        si = 0
        for j in range(NT):
            t0 = j * TT
            ps, pd = R_MM.next()
            for c in range(8):
                s_ = sq[c % 2]
                P.op("act", lambda h, s_=s_, c=c: h.activation(out=s_.v, in_=xT.v[:, c, t0:t0 + TT], func=AF.Square),
                     reads=xT.d2(c, t0, t0 + TT), writes=s_.dall())
                P.op("pe", lambda h, s_=s_, c=c, ps=ps: h.matmul(ps[:, :], onesf.v, s_.v, start=(c == 0), stop=(c == 7)),
                     reads=s_.dall() + onesf.dall(), writes=pd)
            r_ = rs[j % 2]
            P.op("act", lambda h, r_=r_, ps=ps: h.activation(out=r_.v, in_=ps[:, :], func=AF.Sqrt, scale=1.0 / D, bias=cst.v[:, 0:1]),
                 reads=pd + cst.dall(), writes=r_.dall())
            P.op("dve", lambda h, r_=r_: h.reciprocal(r_.v, r_.v), reads=r_.dall(), writes=r_.dall())
            n_ = nrm[j % 2]
            for c in range(8):
                P.op("dve", lambda h, r_=r_, c=c, n_=n_: h.scalar_tensor_tensor(
                    out=n_.v[:, c, :], in0=xT.v[:, c, t0:t0 + TT], scalar=pv.v[:, 0, PV_FING + c:PV_FING + c + 1],
                    in1=r_.v, op0=ALU.mult, op1=ALU.mult),
                    reads=xT.d2(c, t0, t0 + TT) + r_.dall() + pv.dall(), writes=n_.d2(c, 0, TT))
            for sub in range(4):
                s_ = stg[si % 2]
                si += 1
                for g in range(2):
                    ps2, pd2 = R_MM.next()
                    for cc in range(4):
                        c = g * 4 + cc
                        P.op("pe", lambda h, n_=n_, c=c, cc=cc, ps2=ps2, sub=sub: h.transpose(
                            ps2[:, cc * 128:(cc + 1) * 128], n_.v[:, c, sub * 128:(sub + 1) * 128], identf.v),
                            reads=n_.d2(c, 0, TT) + identf.dall(), writes=pd2, inc=(cc == 3))
                    if g == 0:
                        P.op("act", lambda h, s_=s_, ps2=ps2: h.copy(out=s_.v[:, 0:512], in_=ps2[:, :]), reads=pd2, writes=s_.dall())
                    else:
                        P.op("dve", lambda h, s_=s_, ps2=ps2: h.tensor_copy(out=s_.v[:, 512:1024], in_=ps2[:, :]), reads=pd2, writes=s_.dall())
                P.dma("sp", out_d[b, t0 + sub * 128:t0 + (sub + 1) * 128, :], s_.v, reads=s_.dall())
        AR.release(m0)

    def lru_phase(l, yA):
        m0 = AR.mark()
        xat = [AR.alloc([4, 3 + TT], F32) for _ in range(2)]
        P.op("dve", lambda h: h.memset(xat[0].v[:, :, 0:3], 0.0), writes=xat[0].dall())
        P.dma("pool", wabd.v, wabd_d[l].rearrange("g p c o -> p g (c o)"), writes=wabd.dall())
        wga = ring_alloc([8, 512])
        wload(wga, w_in_d[l, :, 512:1024].rearrange("(k p) n -> p k n", p=128))
        wxa = ring_alloc([8, 512])
        wload(wxa, w_in_d[l, :, 0:512].rearrange("(k p) n -> p k n", p=128))
        for j in range(NT):
            t0 = j * TT
            for c in range(4):
                ps, pd = R_MM.next()
                mm_group(ps[:, :], pd, [(wga.v[:, k, c * 128:(c + 1) * 128], hT_rhs(k, t0, TT)) for k in range(8)],
                         wga.dall() + hT_deps(t0, TT))
                P.op("act", lambda h, ps=ps, c=c, t0=t0: h.activation(out=yA.v[:, c, t0:t0 + TT], in_=ps[:, :], func=GELU),
                     reads=pd, writes=yA.d2(c, t0, t0 + TT))
        NS = 2
        tmp = {}
        for nm in ("xc", "r", "i", "a", "m"):
            tmp[nm] = [AR.alloc([TT], F32) for _ in range(NS)]
        tmp["xb"] = [AR.alloc([TT], BF16) for _ in range(NS)]
        carry = AR.alloc([4], F32)
        r_gate = Ring([4, 5, 6, 7])
        iters = [(j, c) for j in range(NT) for c in range(4)]
        ctx = {}

        def head(idx):
            j, c = iters[idx]
            t0 = j * TT
            xa = xat[j % 2]
            xap = xat[(j - 1) % 2]
            q = idx % NS
            xc, xb = tmp["xc"][q], tmp["xb"][q]
            if j > 0:
                P.op("act", lambda h: h.copy(out=xa.v[:, c, 0:3], in_=xap.v[:, c, TT:TT + 3]),
                     reads=xap.d2(c, TT, TT + 3), writes=xa.d2(c, 0, 3))
            ps, pd = R_MM.next()
            mm_group(ps[:, :], pd, [(wxa.v[:, k, c * 128:(c + 1) * 128], hT_rhs(k, t0, TT)) for k in range(8)],
                     wxa.dall() + hT_deps(t0, TT))
            P.op("act", lambda h: h.copy(out=xa.v[:, c, 3:3 + TT], in_=ps[:, :]), reads=pd, writes=xa.d2(c, 3, 3 + TT))
            cw = lambda k: pv.v[:, l, PV_LCW + k * 4 + c:PV_LCW + k * 4 + c + 1]
            cb = pv.v[:, l, PV_LCB + c:PV_LCB + c + 1]
            xin = lambda k: xa.v[:, c, k:k + TT]
            xdeps = xa.d2(c, 0, TT + 3) + pv.dall()
            P.op("dve", lambda h: h.tensor_scalar(xc.v, xin(3), cw(3), cb, op0=ALU.mult, op1=ALU.add), reads=xdeps, writes=xc.dall())
            for k in range(3):
                P.op("dve", lambda h, k=k: h.scalar_tensor_tensor(out=xc.v, in0=xin(k), scalar=cw(k), in1=xc.v, op0=ALU.mult, op1=ALU.add),
                     reads=xdeps + xc.dall(), writes=xc.dall())
            P.op("dve", lambda h: h.tensor_copy(out=xb.v, in_=xc.v), reads=xc.dall(), writes=xb.dall())
            psa, pda = r_gate.next()
            P.op("pe", lambda h: h.matmul(psa[:, :], wabd.v[:, 0, c * 128:(c + 1) * 128], xb.v, start=True, stop=True),
                 reads=xb.dall() + wabd.dall(), writes=pda)
            psx, pdx = r_gate.next()
            P.op("pe", lambda h: h.matmul(psx[:, :], wabd.v[:, 1, c * 128:(c + 1) * 128], xb.v, start=True, stop=True),
                 reads=xb.dall() + wabd.dall(), writes=pdx)
            ctx[idx] = (psa, pda, psx, pdx)

        def tail(idx):
            j, c = iters[idx]
            t0 = j * TT
            q = idx % NS
            xc, r_, i_, a_, m_ = (tmp[n][q] for n in ("xc", "r", "i", "a", "m"))
            psa, pda, psx, pdx = ctx.pop(idx)
            P.op("act", lambda h: h.activation(out=r_.v, in_=psa[:, :], func=AF.Sigmoid, bias=pv.v[:, l, PV_LBA + c:PV_LBA + c + 1]),
                 reads=pda + pv.dall(), writes=r_.dall())
            P.op("act", lambda h: h.activation(out=i_.v, in_=psx[:, :], func=AF.Sigmoid, bias=pv.v[:, l, PV_LBX + c:PV_LBX + c + 1]),
                 reads=pdx + pv.dall(), writes=i_.dall())
            P.op("act", lambda h: h.activation(out=a_.v, in_=r_.v, func=AF.Exp, scale=lruc.v[:, l, c:c + 1]),
                 reads=r_.dall() + lruc.dall(), writes=a_.dall())
            P.op("act", lambda h: h.activation(out=m_.v, in_=r_.v, func=AF.Exp, scale=lruc.v[:, l, 4 + c:5 + c]),
                 reads=r_.dall() + lruc.dall(), writes=m_.dall())
            P.op("act", lambda h: h.activation(out=m_.v, in_=m_.v, func=AF.Ln, scale=-1.0, bias=cst.v[:, 1:2]),
                 reads=m_.dall() + cst.dall(), writes=m_.dall())
            P.op("act", lambda h: h.activation(out=m_.v, in_=m_.v, func=AF.Exp, scale=0.5), reads=m_.dall(), writes=m_.dall())
            P.op("dve", lambda h: h.tensor_tensor(i_.v, i_.v, xc.v, op=ALU.mult), reads=i_.dall() + xc.dall(), writes=i_.dall())
            P.op("dve", lambda h: h.tensor_tensor(i_.v, i_.v, m_.v, op=ALU.mult), reads=i_.dall() + m_.dall(), writes=i_.dall())
            hcur = r_
            if j == 0:
                P.op("dve", lambda h: h.tensor_tensor_scan(hcur.v, a_.v, i_.v, 0.0, ALU.mult, ALU.add),
                     reads=a_.dall() + i_.dall(), writes=hcur.dall())
            else:
                P.op("dve", lambda h: h.tensor_tensor_scan(hcur.v, a_.v, i_.v, carry.v[:, c:c + 1], ALU.mult, ALU.add),
                     reads=a_.dall() + i_.dall() + carry.dall(), writes=hcur.dall())
            P.op("dve", lambda h: h.tensor_copy(out=carry.v[:, c:c + 1], in_=hcur.v[:, TT - 1:TT]), reads=hcur.dall(), writes=carry.dall())
            P.op("dve", lambda h: h.tensor_tensor(yA.v[:, c, t0:t0 + TT], yA.v[:, c, t0:t0 + TT], hcur.v, op=ALU.mult),
                 reads=hcur.dall() + yA.d2(c, t0, t0 + TT), writes=yA.d2(c, t0, t0 + TT))

        for idx in range(len(iters)):
            if idx == 0:
                head(0)
            if idx + 1 < len(iters):
                head(idx + 1)
            tail(idx)
        AR.release(m0)

    def xattn_phase(l, b, yC):
        m0 = AR.mark()
        memK = AR.alloc([4, 256], BF16)
        memV = AR.alloc([2, 512], BF16)
        memh = AR.alloc([8, 256], BF16)
        m1 = AR.mark()
        mstg = [AR.alloc([D], F32)] * 2
        memT = AR.alloc([8, 256], F32)
        sq = [AR.alloc([256], F32) for _ in range(2)]
        rs = AR.alloc([256], F32)
        wk = ring_alloc([8, 512])
        wload(wk, w_mkv_d[l, :, 0:512].rearrange("(k p) n -> p k n", p=128))
        wv = ring_alloc([8, 512])
        wload(wv, w_mkv_d[l, :, 512:1024].rearrange("(k p) n -> p k n", p=128))
        wq = ring_alloc([8, 512])
        wload(wq, w_in_d[l, :, 2560:3072].rearrange("(k p) n -> p k n", p=128))
        for mt in range(2):
            s_ = mstg[mt]
            P.dma("sp", s_.v, mem_d[b, mt * 128:(mt + 1) * 128, :], writes=s_.dall())
            for g in range(2):
                ps, pd = R_MM.next()
                for cc in range(4):
                    c = g * 4 + cc
                    P.op("pe", lambda h, s_=s_, c=c, cc=cc, ps=ps: h.transpose(ps[:, cc * 128:(cc + 1) * 128], s_.v[:, c * 128:(c + 1) * 128], identf.v),
                         reads=s_.dall() + identf.dall(), writes=pd, inc=(cc == 3))
                outap = memT.v[:, g * 4:(g + 1) * 4, mt * 128:(mt + 1) * 128]
                inap = ps[:, :].rearrange("p (a b) -> p a b", a=4)
                P.op("act", lambda h, outap=outap, inap=inap: h.copy(out=outap, in_=inap), reads=pd, writes=memT.dall())
        ps, pd = R_MM.next()
        for c in range(8):
            s_ = sq[c % 2]
            P.op("act", lambda h, s_=s_, c=c: h.activation(out=s_.v, in_=memT.v[:, c, :], func=AF.Square),
                 reads=memT.dall(), writes=s_.dall())
            P.op("pe", lambda h, s_=s_, c=c, ps=ps: h.matmul(ps[:, 0:256], onesf.v, s_.v, start=(c == 0), stop=(c == 7)),
                 reads=s_.dall() + onesf.dall(), writes=pd)
        P.op("act", lambda h, ps=ps: h.activation(out=rs.v, in_=ps[:, 0:256], func=AF.Sqrt, scale=1.0 / D, bias=cst.v[:, 0:1]),
             reads=pd + cst.dall(), writes=rs.dall())
        P.op("dve", lambda h: h.reciprocal(rs.v, rs.v), reads=rs.dall(), writes=rs.dall())
        for c in range(8):
            P.op("dve", lambda h, c=c: h.scalar_tensor_tensor(
                out=memh.v[:, c, :], in0=memT.v[:, c, :], scalar=pv.v[:, l, PV_MEMG + c:PV_MEMG + c + 1],
                in1=rs.v, op0=ALU.mult, op1=ALU.mult),
                reads=memT.dall() + rs.dall() + pv.dall(), writes=memh.dall())
        for hd in range(4):
            ps, pd = R_MM.next()
            mm_group(ps[:, 0:256], pd, [(wk.v[:, k, hd * 128:(hd + 1) * 128], memh.v[:, k, :]) for k in range(8)],
                     wk.dall() + memh.dall())
            P.op("act", lambda h, ps=ps, hd=hd: h.copy(out=memK.v[:, hd, :], in_=ps[:, 0:256]), reads=pd, writes=memK.dall())
        for mt in range(2):
            ps, pd = R_MM.next()
            mm_group(ps[:, :], pd, [(memh.v[:, k, mt * 128:(mt + 1) * 128], wv.v[:, k, :]) for k in range(8)],
                     wv.dall() + memh.dall())
            P.op("act", lambda h, ps=ps, mt=mt: h.copy(out=memV.v[:, mt, :], in_=ps[:, :]), reads=pd, writes=memV.dall())
        AR.release(m1)
        qx = [AR.alloc([TT], BF16) for _ in range(2)]
        pt = [AR.alloc([2, TT], BF16) for _ in range(2)]
        rd = [AR.alloc([TT], F32) for _ in range(2)]
        sc = 128 ** -0.5
        r_q = Ring([0, 1])
        r_s = Ring([2, 3, 4, 5])
        iters = [(j, hd) for j in range(NT) for hd in range(4)]
        ctx = {}

        def head(idx):
            j, hd = iters[idx]
            t0 = j * TT
            q_ = qx[idx % 2]
            ps, pd = r_q.next()
            mm_group(ps[:, :], pd, [(wq.v[:, k, hd * 128:(hd + 1) * 128], hT_rhs(k, t0, TT)) for k in range(8)],
                     wq.dall() + hT_deps(t0, TT))
            P.op("act", lambda h: h.copy(out=q_.v, in_=ps[:, :]), reads=pd, writes=q_.dall())
            sb = []
            for mt in range(2):
                sps, spd = r_s.next()
                P.op("pe", lambda h, sps=sps, mt=mt: h.matmul(sps[:, :], memK.v[:, hd, mt * 128:(mt + 1) * 128], q_.v, start=True, stop=True),
                     reads=q_.dall() + memK.dall(), writes=spd)
                sb.append((sps, spd))
            ctx[idx] = sb

        def tail(idx):
            j, hd = iters[idx]
            t0 = j * TT
            p_ = pt[idx % 2]; r_ = rd[idx % 2]
            sb = ctx.pop(idx)
            for mt in range(2):
                sps, spd = sb[mt]
                P.op("act", lambda h, sps=sps, mt=mt: h.activation(out=p_.v[:, mt, :], in_=sps[:, :], func=AF.Exp, scale=sc),
                     reads=spd, writes=p_.dall())
            ops, opd = psb[6], [psd[6]]
            mm_group(ops[:, :], opd, [(memV.v[:, mt, hd * 128:(hd + 1) * 128], p_.v[:, mt, :]) for mt in range(2)],
                     memV.dall() + p_.dall())
            dps, dpd = psb[7], [psd[7]]
            mm_group(dps[:, :], dpd, [(onesb.v, p_.v[:, mt, :]) for mt in range(2)], onesb.dall() + p_.dall())
            P.op("act", lambda h: h.activation(out=r_.v, in_=dps[:, :], func=AF.Ln), reads=dpd, writes=r_.dall())
            P.op("act", lambda h: h.activation(out=r_.v, in_=r_.v, func=AF.Exp, scale=-1.0), reads=r_.dall(), writes=r_.dall())
            P.op("dve", lambda h: h.tensor_tensor(yC.v[:, hd, t0:t0 + TT], ops[:, :], r_.v, op=ALU.mult),
                 reads=opd + r_.dall(), writes=yC.d2(hd, t0, t0 + TT))

        for idx in range(len(iters)):
            if idx == 0:
                head(0)
            if idx + 1 < len(iters):
                head(idx + 1)
            tail(idx)
        AR.release(m0)

    def moba_phase(l, yB):
        m0 = AR.mark()
        NSET = 2
        KA = [[AR.alloc([S], BF16) for _ in range(2)] for _ in range(NSET)]
        QA = [[AR.alloc([S], BF16) for _ in range(2)] for _ in range(NSET)]
        VT = AR.alloc([16, 2, 128], BF16)
        NPT = 3
        PT = [AR.alloc([2, 256], BF16) for _ in range(NPT)]
        kmT = [[AR.alloc([8], BF16) for _ in range(2)] for _ in range(NSET)]
        ksum = AR.alloc([8], F32)
        gm = AR.alloc([16], F32)
        top8 = AR.alloc([16], F32)
        stage = [[AR.alloc([2, 128], BF16) for _ in range(2)] for _ in range(4)]
        dsh = [AR.alloc([256], F32) for _ in range(2)]
        r_proj = Ring([0, 1])
        r_sc = Ring([3, 4, 5])
        r_acc = Ring([6, 7])
        gate_bank, gate_dep = psb[2], [psd[2]]
        for s_ in range(NSET):
            for t_ in KA[s_] + QA[s_]:
                P.op("dve", lambda h, t_=t_: h.memset(t_.v, 0.0), writes=t_.dall())
            for hh in range(2):
                P.dma("pool", QA[s_][hh].v[64:68, :], cqrow_d, writes=QA[s_][hh].dall())
                P.dma("pool", KA[s_][hh].v[96:104, :], coneh_d, writes=KA[s_][hh].dall())
        P.op("dve", lambda h: h.memset(VT.v, 1.0), writes=VT.dall())
        for q4 in range(4):
            for sub in range(2):
                t_ = stage[q4][sub]
                P.op("dve", lambda h, t_=t_: h.memset(t_.v, 0.0), writes=t_.dall())
        state = {"pti": 0, "acc": 0, "wv": {}}
        yield

        def project(pair):
            s_ = pair % NSET
            ka, qa, km = KA[s_], QA[s_], kmT[s_]
            wq = ring_alloc([8, 128]); wload(wq, w_in_d[l, :, 1024 + pair * 128:1024 + (pair + 1) * 128].rearrange("(k p) n -> p k n", p=128))
            wk = ring_alloc([8, 128]); wload(wk, w_in_d[l, :, 1536 + pair * 128:1536 + (pair + 1) * 128].rearrange("(k p) n -> p k n", p=128))
            for hh in range(2):
                P.dma("pool", ka[hh].v[64:68, :], ckrow_d[pair * 2 + hh], writes=ka[hh].dall())
            for j in range(NT):
                t0 = j * TT
                ps, pd = r_proj.next()
                mm_group(ps[:, :], pd, [(wk.v[:, k, :], hT_rhs(k, t0, TT)) for k in range(8)], wk.dall() + hT_deps(t0, TT))
                for hh in range(2):
                    P.op("act", lambda h, ps=ps, hh=hh: h.copy(out=ka[hh].v[0:64, t0:t0 + TT], in_=ps[hh * 64:(hh + 1) * 64, :]),
                         reads=pd, writes=ka[hh].d(t0, t0 + TT))
                ps, pd = r_proj.next()
                mm_group(ps[:, :], pd, [(wq.v[:, k, :], hT_rhs(k, t0, TT)) for k in range(8)], wq.dall() + hT_deps(t0, TT))
                for hh in range(2):
                    P.op("act", lambda h, ps=ps, hh=hh: h.mul(out=qa[hh].v[0:64, t0:t0 + TT], in_=ps[hh * 64:(hh + 1) * 64, :], mul=0.125),
                         reads=pd, writes=qa[hh].d(t0, t0 + TT))
            for hh in range(2):
                P.op("dve", lambda h, hh=hh: h.tensor_reduce(out=ksum.v[0:64, :], in_=ka[hh].v[0:64, :].rearrange("p (a b) -> p a b", a=8),
                                                              axis=AX.X, op=ALU.add),
                     reads=ka[hh].dall(), writes=ksum.dall())
                P.op("act", lambda h, hh=hh: h.mul(out=km[hh].v[0:64, :], in_=ksum.v[0:64, :], mul=1.0 / 256), reads=ksum.dall(), writes=km[hh].dall())
            for qb in range(4, 8):
                for sub in range(2):
                    qs = qb * 256 + sub * 128
                    gcol = ((qb - 4) * 2 + sub) * 16
                    for hh in range(2):
                        P.op("pe", lambda h, hh=hh, qs=qs, gcol=gcol: h.matmul(gate_bank[:, gcol + hh * 8:gcol + (hh + 1) * 8], qa[hh].v[0:64, qs:qs + 128],
                                                                             km[hh].v[0:64, :], start=True, stop=True),
                             reads=qa[hh].d(qs, qs + 128) + km[hh].dall(), writes=gate_dep)
                    P.op("dve", lambda h, qb=qb, gcol=gcol: h.tensor_tensor(gm.v, gate_bank[:, gcol:gcol + 16], pastm.v[:, qb - 4, :], op=ALU.add),
                         reads=gate_dep + pastm.dall(), writes=gm.dall())
                    st_ = stage[qb - 4][sub]
                    for hh in range(2):
                        P.op("dve", lambda h, hh=hh: h.max(out=top8.v[:, hh * 8:(hh + 1) * 8], in_=gm.v[:, hh * 8:(hh + 1) * 8]),
                             reads=gm.dall(), writes=top8.dall())
                        P.op("dve", lambda h, hh=hh, st_=st_, qb=qb: h.tensor_scalar(
                            st_.v[:, hh, 96:96 + qb], gm.v[:, hh * 8:hh * 8 + qb], top8.v[:, hh * 8 + 2:hh * 8 + 3], -30000.0,
                            op0=ALU.is_lt, op1=ALU.mult),
                            reads=gm.dall() + top8.dall(), writes=st_.dall())

        def project_v(pair):
            vt = VT
            wv = ring_alloc([8, 128]); wload(wv, w_in_d[l, :, 2048 + pair * 128:2048 + (pair + 1) * 128].rearrange("(k p) n -> p k n", p=128))
            for j in range(NT):
                ps, pd = r_proj.next()
                for sub in range(4):
                    tk = j * 4 + sub
                    mm_group(ps[:, sub * 128:(sub + 1) * 128], pd, [(hT_rhs(k, tk * 128, 128), wv.v[:, k, :]) for k in range(8)],
                             wv.dall() + hT_deps(tk * 128, 128))
                pv4 = ps[:, :].rearrange("p (a b) -> p a b", a=4)
                P.op("dve", lambda h, pv4=pv4, j=j: h.tensor_copy(out=vt.v[:, j * 4:(j + 1) * 4, 0, 0:64], in_=pv4[:, :, 0:64]),
                     reads=pd, writes=vt.d2s(range(j * 4, j * 4 + 4), 0, 256))
                P.op("dve", lambda h, pv4=pv4, j=j: h.tensor_copy(out=vt.v[:, j * 4:(j + 1) * 4, 1, 64:128], in_=pv4[:, :, 64:128]),
                     reads=pd, writes=vt.d2s(range(j * 4, j * 4 + 4), 0, 256))

        def mask_rows(pair):
            s_ = pair % NSET
            qa = QA[s_]
            for qb in range(4, 8):
                for sub in range(2):
                    qs = qb * 256 + sub * 128
                    st_ = stage[qb - 4][sub]
                    tps, tpd = r_sc.next()
                    tpb = tps[:, :].bitcast(BF16)
                    for hh in range(2):
                        P.op("pe", lambda h, tpb=tpb, st_=st_, hh=hh: h.transpose(tpb[:, hh * 128:(hh + 1) * 128], st_.v[:, hh, :], identb.v),
                             reads=st_.dall() + identb.dall(), writes=tpd, inc=(hh == 1))
                    for hh in range(2):
                        P.op("act", lambda h, tpb=tpb, hh=hh, qs=qs: h.copy(out=qa[hh].v[96:104, qs:qs + 128], in_=tpb[96:104, hh * 128:(hh + 1) * 128]),
                             reads=tpd, writes=qa[hh].d(qs, qs + 128))

        def attention(pair, qbs):
            s_ = pair % NSET
            ka, qa, vt = KA[s_], QA[s_], VT
            units = []
            for qb in qbs:
                q0 = qb * 256
                for hh in range(2):
                    ai = state["acc"]; state["acc"] += 1
                    ops, opd = r_acc.next()
                    ds_ = dsh[ai % 2]
                    nun = qb + 1
                    for ui in range(nun):
                        units.append(dict(qb=qb, q0=q0, hh=hh, ui=ui, nun=nun, ops=ops, opd=opd, ds=ds_))
            for u in units:
                u["p"] = PT[state["pti"] % NPT]; state["pti"] += 1
                u["sps"], u["spd"] = None, None

            def emit_qk(u):
                sps, spd = r_sc.next()
                u["sps"], u["spd"] = sps, spd
                hh, q0, qb = u["hh"], u["q0"], u["qb"]
                if u["ui"] == 0:
                    k0 = 2 * qb
                    P.op("pe", lambda h: h.matmul(sps[:, 0:256], ka[hh].v[:, k0 * 128:(k0 + 1) * 128], qa[hh].v[:, q0:q0 + 256], start=True, stop=False),
                         reads=ka[hh].d(k0 * 128, (k0 + 1) * 128) + qa[hh].d(q0, q0 + 256) + trib.dall() + identb.dall(), writes=spd, inc=False)
                    P.op("pe", lambda h: h.matmul(sps[:, 0:128], identb.v, trib.v, start=False, stop=True), writes=spd, inc=False)
                    P.op("pe", lambda h: h.matmul(sps[:, 384:512], ka[hh].v[:, (k0 + 1) * 128:(k0 + 2) * 128], qa[hh].v[:, q0 + 128:q0 + 256], start=True, stop=False),
                         reads=ka[hh].d((k0 + 1) * 128, (k0 + 2) * 128) + qa[hh].d(q0, q0 + 256), writes=spd, inc=False)
                    P.op("pe", lambda h: h.matmul(sps[:, 384:512], identb.v, trib.v, start=False, stop=True), writes=spd)
                else:
                    kp = u["ui"] - 1
                    for i2 in range(2):
                        kt = 2 * kp + i2
                        P.op("pe", lambda h, kt=kt, i2=i2: h.matmul(sps[:, i2 * 256:(i2 + 1) * 256], ka[hh].v[:, kt * 128:(kt + 1) * 128],
                                                                  qa[hh].v[:, q0:q0 + 256], start=True, stop=True),
                             reads=ka[hh].d(kt * 128, (kt + 1) * 128) + qa[hh].d(q0, q0 + 256), writes=spd, inc=(i2 == 1))

            def emit_rest(u):
                sps, spd, p_ = u["sps"], u["spd"], u["p"]
                hh, q0, qb, ops, opd = u["hh"], u["q0"], u["qb"], u["ops"], u["opd"]
                last = (u["ui"] == u["nun"] - 1)
                if u["ui"] == 0:
                    k0 = 2 * qb
                    P.op("act", lambda h: h.activation(out=p_.v[:, 0, :], in_=sps[:, 0:256], func=AF.Exp), reads=spd, writes=p_.dall())
                    P.op("act", lambda h: h.activation(out=p_.v[:, 1, 128:256], in_=sps[:, 384:512], func=AF.Exp), reads=spd, writes=p_.dall())
                    P.op("pe", lambda h: h.matmul(ops[:, 0:256], vt.v[:, k0, hh, :], p_.v[:, 0, :], start=True, stop=False),
                         reads=vt.d2(k0, 0, 256) + p_.dall(), writes=opd, inc=False)
                    P.op("pe", lambda h: h.matmul(ops[:, 128:256], vt.v[:, k0 + 1, hh, :], p_.v[:, 1, 128:256], start=False, stop=last),
                         reads=vt.d2(k0 + 1, 0, 256) + p_.dall(), writes=opd, inc=True)
                else:
                    kp = u["ui"] - 1
                    P.op("act", lambda h: h.activation(out=p_.v.rearrange("p a b -> p (a b)"), in_=sps[:, :], func=AF.Exp),
                         reads=spd, writes=p_.dall())
                    for i2 in range(2):
                        kt = 2 * kp + i2
                        P.op("pe", lambda h, kt=kt, i2=i2: h.matmul(ops[:, 0:256], vt.v[:, kt, hh, :], p_.v[:, i2, :], start=False, stop=(last and i2 == 1)),
                             reads=vt.d2(kt, 0, 256) + p_.dall(), writes=opd, inc=(i2 == 1))
                if last:
                    ds_ = u["ds"]
                    olo, dlo = (0, 64) if hh == 0 else (64, 0)
                    P.op("act", lambda h: h.activation(out=ds_.v[olo:olo + 64, :], in_=ops[dlo:dlo + 64, 0:256], func=AF.Ln), reads=opd, writes=ds_.dall())
                    P.op("act", lambda h: h.activation(out=ds_.v[olo:olo + 64, :], in_=ds_.v[olo:olo + 64, :], func=AF.Exp, scale=-1.0), reads=ds_.dall(), writes=ds_.dall())
                    P.op("dve", lambda h: h.tensor_tensor(yB.v[olo:olo + 64, pair, q0:q0 + 256], ops[olo:olo + 64, 0:256], ds_.v[olo:olo + 64, :], op=ALU.mult),
                         reads=opd + ds_.dall(), writes=yB.d2(pair, q0, q0 + 256))

            DEPTH_QK = 2
            for i in range(min(DEPTH_QK, len(units))):
                emit_qk(units[i])
            for i, u in enumerate(units):
                if i + DEPTH_QK < len(units):
                    emit_qk(units[i + DEPTH_QK])
                emit_rest(u)

        project(0)
        for pair in range(4):
            project_v(pair)
            attention(pair, range(0, 4))
            mask_rows(pair)
            if pair + 1 < 4:
                project(pair + 1)
            attention(pair, range(4, 8))
        AR.release(m0)

    def merge_phase(l, yA, yB, yC):
        m0 = AR.mark()
        mg = AR.alloc([8, 1024], BF16)
        NGT = 2 if (AR.nbytes - AR.top) >= 2 * 3 * 2048 + 1024 else 1
        gt = [[AR.alloc([TT], F32) for _ in range(3)] for _ in range(NGT)]
        gi = 0
        ys = [yA, yB, yC]
        gview = w_in_d[l, :, 3072:6144].rearrange("(k p) (n d) -> p k n d", p=128, n=3)
        bview = w_br_d[l].rearrange("n (k p) d -> p k n d", p=128)
        for hf in range(2):
            for dc in range(8):
                G = ring_alloc([8, 3, 128])
                B = ring_alloc([4, 3, 128])
                for n in range(3):
                    P.dma("pool", G.v[:, :, n, :], gview[:, :, n, dc * 128:(dc + 1) * 128], writes=G.dall())
                    P.dma("pool", B.v[:, :, n, :], bview[:, :, n, dc * 128:(dc + 1) * 128], writes=B.dall())
                for jj in range(2):
                    t0 = hf * 1024 + jj * TT
                    gts = gt[gi % NGT]; gi += 1
                    for n in range(3):
                        ps, pd = R_MM.next()
                        mm_group(ps[:, :], pd, [(G.v[:, k, n, :], hT_rhs(k, t0, TT)) for k in range(8)],
                                 G.dall() + hT_deps(t0, TT))
                        P.op("act", lambda h, ps=ps, n=n, gts=gts: h.activation(out=gts[n].v, in_=ps[:, :], func=AF.Sigmoid), reads=pd, writes=gts[n].dall())
                        ps2, pd2 = R_MM.next()
                        mm_group(ps2[:, :], pd2, [(B.v[:, k, n, :], ys[n].v[:, k, t0:t0 + TT]) for k in range(4)],
                                 B.dall() + ys[n].d2s(range(4), t0, t0 + TT))
                        P.op("dve", lambda h, ps2=ps2, n=n, gts=gts: h.tensor_tensor(gts[n].v, ps2[:, :], gts[n].v, op=ALU.mult),
                             reads=pd2 + gts[n].dall(), writes=gts[n].dall())
                    P.op("dve", lambda h, gts=gts: h.tensor_tensor(gts[0].v, gts[0].v, gts[1].v, op=ALU.add),
                         reads=gts[0].dall() + gts[1].dall(), writes=gts[0].dall())
                    P.op("dve", lambda h, dc=dc, jj=jj, gts=gts: h.tensor_tensor(mg.v[:, dc, jj * TT:(jj + 1) * TT], gts[0].v, gts[2].v, op=ALU.add),
                         reads=gts[0].dall() + gts[2].dall(), writes=mg.d2(dc, jj * TT, (jj + 1) * TT))
            for pp in range(2):
                Wo = ring_alloc([8, 512])
                wload(Wo, w_out_d[l, :, pp * 512:(pp + 1) * 512].rearrange("(k p) n -> p k n", p=128))
                for jj in range(2):
                    t0 = hf * 1024 + jj * TT
                    for dci in range(4):
                        dc = pp * 4 + dci
                        ps, pd = R_MM.next()
                        mm_group(ps[:, :], pd, [(Wo.v[:, k, dci * 128:(dci + 1) * 128], mg.v[:, k, jj * TT:(jj + 1) * TT]) for k in range(8)],
                                 Wo.dall() + mg.d2s(range(8), jj * TT, (jj + 1) * TT))
                        P.op("dve", lambda h, ps=ps, dc=dc, t0=t0: h.tensor_tensor(xT.v[:, dc, t0:t0 + TT], ps[:, :], xT.v[:, dc, t0:t0 + TT], op=ALU.add),
                             reads=pd + xT.d2(dc, t0, t0 + TT), writes=xT.d2(dc, t0, t0 + TT))
        AR.release(m0)

    def ffn_phase(l):
        m0 = AR.mark()
        act = AR.alloc([24, 1024], BF16)
        gbuf = [AR.alloc([2 + 1024], F32) for _ in range(2)]
        acc = [AR.alloc([TT], F32) for _ in range(2)]
        gel = [AR.alloc([TT], F32) for _ in range(2)]
        it = 0
        R_F = Ring([0, 1, 2, 3, 4, 5, 6, 7])
        for hf in range(2):
            for ffg in range(12):
                Wg = ring_alloc([8, 256]); wload(Wg, w_fg_d[l, :, ffg * 256:(ffg + 1) * 256].rearrange("(k p) n -> p k n", p=128))
                Wu = ring_alloc([8, 256]); wload(Wu, w_fu_d[l, :, ffg * 256:(ffg + 1) * 256].rearrange("(k p) n -> p k n", p=128))
                for fci in range(2):
                    fc = ffg * 2 + fci
                    gb = gbuf[fc % 2]
                    if hf == 0:
                        P.op("dve", lambda h, gb=gb: h.memset(gb.v[:, 0:2], 0.0), writes=gb.d(0, 2))
                    else:
                        P.op("dve", lambda h, gb=gb, fc=fc: h.tensor_copy(out=gb.v[:, 0:2], in_=gtail.v[:, fc, :]), reads=gtail.dall(), writes=gb.d(0, 2))
                    for jj in range(2):
                        t0 = hf * 1024 + jj * TT
                        g0 = 2 + jj * TT
                        ps, pd = R_F.next()
                        mm_group(ps[:, :], pd, [(Wg.v[:, k, fci * 128:(fci + 1) * 128], hT_rhs(k, t0, TT)) for k in range(8)],
                                 Wg.dall() + hT_deps(t0, TT))
                        ups, upd = R_F.next()
                        mm_group(ups[:, :], upd, [(Wu.v[:, k, fci * 128:(fci + 1) * 128], hT_rhs(k, t0, TT)) for k in range(8)],
                                 Wu.dall() + hT_deps(t0, TT))
                        P.op("act", lambda h, ps=ps, gb=gb, g0=g0: h.copy(out=gb.v[:, g0:g0 + TT], in_=ps[:, :]), reads=pd, writes=gb.d(g0, g0 + TT))
                        a_ = acc[it % 2]; ge = gel[it % 2]; it += 1
                        cw = lambda k, fc=fc: pv.v[:, l, PV_FCW + k * 24 + fc:PV_FCW + k * 24 + fc + 1]
                        cb = pv.v[:, l, PV_FCB + fc:PV_FCB + fc + 1]
                        gd = gb.d(g0 - 2, g0 + TT) + pv.dall()
                        P.op("dve", lambda h, a_=a_, gb=gb, g0=g0, cw=cw, cb=cb: h.tensor_scalar(a_.v, gb.v[:, g0:g0 + TT], cw(2), cb, op0=ALU.mult, op1=ALU.add),
                             reads=gd, writes=a_.dall())
                        P.op("dve", lambda h, a_=a_, gb=gb, g0=g0, cw=cw: h.scalar_tensor_tensor(out=a_.v, in0=gb.v[:, g0 - 1:g0 - 1 + TT], scalar=cw(1), in1=a_.v,
                                                                                               op0=ALU.mult, op1=ALU.add),
                             reads=gd + a_.dall(), writes=a_.dall())
                        P.op("dve", lambda h, a_=a_, gb=gb, g0=g0, cw=cw: h.scalar_tensor_tensor(out=a_.v, in0=gb.v[:, g0 - 2:g0 - 2 + TT], scalar=cw(0), in1=a_.v,
                                                                                               op0=ALU.mult, op1=ALU.add),
                             reads=gd + a_.dall(), writes=a_.dall())
                        P.op("act", lambda h, a_=a_, ge=ge: h.activation(out=ge.v, in_=a_.v, func=GELU), reads=a_.dall(), writes=ge.dall())
                        P.op("dve", lambda h, ups=ups, ge=ge, fc=fc, jj=jj: h.tensor_tensor(act.v[:, fc, jj * TT:(jj + 1) * TT], ups[:, :], ge.v, op=ALU.mult),
                             reads=upd + ge.dall(), writes=act.d2(fc, jj * TT, (jj + 1) * TT))
                    if hf == 0:
                        P.op("dve", lambda h, gb=gb, fc=fc: h.tensor_copy(out=gtail.v[:, fc, :], in_=gb.v[:, 1024:1026]), reads=gb.d(1024, 1026), writes=gtail.dall())
            for dcg in range(4):
                Wd = ring_alloc([24, 256]); wload(Wd, w_fd_d[l, :, dcg * 256:(dcg + 1) * 256].rearrange("(k p) n -> p k n", p=128))
                for jj in range(2):
                    t0 = hf * 1024 + jj * TT
                    for dci in range(2):
                        dc = dcg * 2 + dci
                        ps, pd = R_F.next()
                        mm_group(ps[:, :], pd, [(Wd.v[:, k, dci * 128:(dci + 1) * 128], act.v[:, k, jj * TT:(jj + 1) * TT]) for k in range(24)],
                                 Wd.dall() + act.d2s(range(24), jj * TT, (jj + 1) * TT))
                        P.op("dve", lambda h, ps=ps, dc=dc, t0=t0: h.tensor_tensor(xT.v[:, dc, t0:t0 + TT], ps[:, :], xT.v[:, dc, t0:t0 + TT], op=ALU.add),
                             reads=pd + xT.d2(dc, t0, t0 + TT), writes=xT.d2(dc, t0, t0 + TT))
        AR.release(m0)

    for b in range(n_seq):
        mark('load'); load_x(b)
        for l in range(n_layers):
            mY = AR.mark()
            yB = AR.alloc([4, S], BF16)
            mark('norm1')
            gen = moba_phase(l, yB)
            next(gen)
            rmsnorm_to_hT(PV_MIXG, l, nrs=1)
            mark('moba')
            for _ in gen:
                pass
            yA = AR.alloc([4, S], BF16)
            mark('lru'); lru_phase(l, yA)
            yC = AR.alloc([4, S], BF16)
            mark('xattn'); xattn_phase(l, b, yC)
            if debug and b == 0:
                for nm, y_ in (("yA%d" % l, yA), ("yB%d" % l, yB), ("yC%d" % l, yC)):
                    if nm in dbg_d:
                        P.dma("pool", dbg_d[nm].rearrange("(c p) t -> p c t", p=128), y_.v, reads=y_.dall())
            mark('merge'); merge_phase(l, yA, yB, yC)
            AR.release(mY)
            if debug and b == 0 and ("xmid%d" % l) in dbg_d:
                P.dma("sp", dbg_d["xmid%d" % l].rearrange("(c p) t -> p c t", p=128), xT.v, reads=xT.dall())
            mark('norm2'); rmsnorm_to_hT(PV_FFNG, l)
            mark('ffn'); ffn_phase(l)
            if debug and b == 0 and ("xout%d" % l) in dbg_d:
                P.dma("sp", dbg_d["xout%d" % l].rearrange("(c p) t -> p c t", p=128), xT.v, reads=xT.dall())
        mark('final'); final_store(b)
    mark('end')
    P.finish()
    stack.close()
    return nc, P


def host_consts():
    t = np.arange(S)
    c = {}
    c["c_ident"] = np.eye(128, dtype=np.float32)
    k = np.arange(128)
    c["c_tri"] = np.where(k[None, :] >= k[:, None], 0.0, -30000.0).astype(np.float32)
    base = np.stack([t % 256, 256 * (t // 256), np.ones(S), np.ones(S)]).astype(np.float32)
    c["c_krow"] = np.stack([base * (2.0 ** (-(hd + 1))) for hd in range(8)]).astype(np.float32)
    c["c_qrow"] = np.stack([np.ones(S), np.ones(S), -(t % 256), -256 * (t // 256)]).astype(np.float32)
    c["c_onehot"] = (np.arange(8)[:, None] == (t // 256)[None, :]).astype(np.float32)
    past = np.zeros((128, 4, 16), np.float32)
    for q4 in range(4):
        qb = q4 + 4
        for hh in range(2):
            past[:, q4, hh * 8 + qb:hh * 8 + 8] = -1e30
    c["c_past"] = past
    return c


def host_params(inp):
    pvec = np.zeros((DEPTH, 128, PV_N), np.float32)
    wabd = np.zeros((DEPTH, 2, 128, 4, 128), np.float32)
    for l in range(DEPTH):
        pvec[l, :, PV_MIXG:PV_MIXG + 8] = _chunked(inp["mix_norm_gain"][l])
        pvec[l, :, PV_FFNG:PV_FFNG + 8] = _chunked(inp["ffn_norm_gain"][l])
        pvec[l, :, PV_MEMG:PV_MEMG + 8] = _chunked(inp["mem_norm_gain"][l])
        pvec[l, :, PV_FING:PV_FING + 8] = _chunked(inp["final_norm_gain"])
        for k in range(4):
            pvec[l, :, PV_LCW + k * 4:PV_LCW + k * 4 + 4] = _chunked(inp["lru_conv_w"][l, k])
        pvec[l, :, PV_LCB:PV_LCB + 4] = _chunked(inp["lru_conv_b"][l])
        pvec[l, :, PV_LBA:PV_LBA + 4] = _chunked(inp["lru_b_a"][l].reshape(-1))
        pvec[l, :, PV_LBX:PV_LBX + 4] = _chunked(inp["lru_b_x"][l].reshape(-1))
        pvec[l, :, PV_LAM:PV_LAM + 4] = _chunked(inp["lru_lambda"][l])
        for k in range(3):
            pvec[l, :, PV_FCW + k * 24:PV_FCW + k * 24 + 24] = _chunked(inp["ffn_conv_w"][l, k])
        pvec[l, :, PV_FCB:PV_FCB + 24] = _chunked(inp["ffn_conv_b"][l])
        for g, nm in enumerate(("lru_w_a", "lru_w_x")):
            w = inp[nm][l]
            for hd in range(8):
                c, o = hd // 2, (hd % 2) * 64
                wabd[l, g, o:o + 64, c, o:o + 64] = w[hd]
    return pvec, wabd


_CACHE = {}


def kernel(**inputs):
    inp = {k: np.asarray(v) for k, v in inputs.items()}
    if "nc" not in _CACHE:
        _CACHE["nc"] = build_program()[0]
    nc = _CACHE["nc"]
    pvec, wabd = host_params(inp)
    consts = host_consts()
    shared = dict(consts)
    shared.update(pvec=pvec, wabd=wabd)
    for k in ("w_in", "w_mem_kv", "w_branch", "w_out", "w_ffn_gate", "w_ffn_up", "w_ffn_down"):
        shared[k] = np.ascontiguousarray(inp[k], dtype=np.float32)
    in_maps = []
    for c in range(NCORES):
        m = dict(shared)
        m["x"] = np.ascontiguousarray(inp["x"][c * SEQ_PER_CORE:(c + 1) * SEQ_PER_CORE], dtype=np.float32)
        m["mem"] = np.ascontiguousarray(inp["mem"][c * SEQ_PER_CORE:(c + 1) * SEQ_PER_CORE], dtype=np.float32)
        in_maps.append(m)
    res = run_bass_kernel_spmd(nc, in_maps, core_ids=list(range(NCORES)))
    out = np.concatenate([np.asarray(r["out"]) for r in res.results], axis=0)
    return out.astype(np.float32)
```

```python
import math
from contextlib import ExitStack
import numpy as np
import concourse.bass as bass
import concourse.mybir as mybir
from concourse.bass_utils import run_bass_kernel_spmd

F32 = mybir.dt.float32
BF16 = mybir.dt.bfloat16
U8 = mybir.dt.uint8
AF = mybir.ActivationFunctionType
ALU = mybir.AluOpType
AX = mybir.AxisListType

NCORES = 8
SEQ_PER_CORE = 2
S = 2048
D = 1024
DEPTH = 2
NT = 4
TT = 512
GELU = AF.Gelu_apprx_tanh

ENGS = ["pe", "act", "dve", "pool", "sp"]
NDMA = 24
GRAN = 256


class Dep:
    __slots__ = ("w", "rs")

    def __init__(self):
        self.w = {}
        self.rs = {}


class _Rec:
    def __getattr__(self, name):
        def f(*a, **k):
            self.call = (name, a, k)
            return self
        return f


class Prog:
    def __init__(self, nc, stack):
        self.nc = nc
        self.streams = {e: [] for e in ENGS}
        self.cnt = {e: 0 for e in ENGS}
        self.seen = {e: {} for e in ENGS}
        self.sem = {}
        for e in ENGS:
            self.sem[e] = stack.enter_context(nc.semaphore("s_" + e))
        self.dsem = []
        self.dtot = []
        for i in range(NDMA):
            self.dsem.append(stack.enter_context(nc.semaphore("d_%d" % i)))
            self.dtot.append(0)
            self.sem[("dma", i)] = self.dsem[i]
        self.dnext = 0
        self.ninst = 0

    def _need(self, eng, reads, writes):
        need = {}
        for d in reads:
            for k, v in d.w.items():
                if need.get(k, 0) < v:
                    need[k] = v
        for d in writes:
            for k, v in d.w.items():
                if need.get(k, 0) < v:
                    need[k] = v
            for k, v in d.rs.items():
                if need.get(k, 0) < v:
                    need[k] = v
        out = []
        seen = self.seen[eng]
        raw_self = 0
        if eng in ("act", "dve", "pool"):
            for d in reads:
                v = d.w.get(eng, 0)
                if v > raw_self:
                    raw_self = v
        for k, v in need.items():
            if k == eng:
                if raw_self == 0:
                    continue
                v = raw_self
            if seen.get(k, 0) >= v:
                continue
            seen[k] = v
            out.append((self.sem[k], v))
        return out

    @staticmethod
    def _mark(key, val, reads, writes):
        for d in reads:
            if d.rs.get(key, 0) < val:
                d.rs[key] = val
        for d in writes:
            if d.w.get(key, 0) < val:
                d.w[key] = val

    def op(self, eng, fn, reads=(), writes=(), inc=True):
        waits = self._need(eng, reads, writes)
        if inc:
            self.cnt[eng] += 1
            val = self.cnt[eng]
        else:
            val = self.cnt[eng] + 1
        self._mark(eng, val, reads, writes)
        sem = self.sem[eng]
        self.ninst += 1
        rec = _Rec()
        fn(rec)
        cname, cargs, ckw = rec.call

        def thunk(h):
            for s, v in waits[:-1]:
                h.wait_ge(s, v)
            inst = getattr(h, cname)(*cargs, **ckw)
            if waits:
                inst._wait_ge(*waits[-1])
            if inc:
                inst.then_inc(sem, 1)
        self.streams[eng].append(thunk)

    def dma(self, eng, out, in_, reads=(), writes=(), **kw):
        k = self.dnext
        self.dnext = (self.dnext + 1) % NDMA
        key = ("dma", k)
        waits = self._need(eng, reads, writes)
        prev = self.dtot[k]
        if prev > 0 and self.seen[eng].get(key, 0) < prev:
            self.seen[eng][key] = prev
            waits.append((self.dsem[k], prev))
        self.dtot[k] += 16
        val = self.dtot[k]
        self._mark(key, val, reads, writes)
        sem = self.dsem[k]
        self.ninst += 1

        def thunk(h):
            for s, v in waits:
                h.wait_ge(s, v)
            h.dma_start(out=out, in_=in_, **kw).then_inc(sem, 16)
        self.streams[eng].append(thunk)

    def finish(self):
        waits = [(self.dsem[k], self.dtot[k]) for k in range(NDMA) if self.dtot[k] > 0]
        others = [(self.sem[e], self.cnt[e]) for e in ENGS if e != "sp" and self.cnt[e] > 0]

        def fthunk(h):
            for s, v in waits + others:
                h.wait_ge(s, v)
        self.streams["sp"].append(fthunk)
        nc = self.nc
        st = self.streams
        with nc.Block() as block:
            @block.tensor
            def _(h):
                for t in st["pe"]:
                    t(h)

            @block.scalar
            def _(h):
                for t in st["act"]:
                    t(h)

            @block.vector
            def _(h):
                for t in st["dve"]:
                    t(h)

            @block.gpsimd
            def _(h):
                for t in st["pool"]:
                    t(h)

            @block.sync
            def _(h):
                for t in st["sp"]:
                    t(h)


class SBT:
    def __init__(self, arena, gran, off, shape, dt):
        self.esz = 4 if dt == F32 else 2
        self.off = off
        self.shape = shape
        self.dt = dt
        n = 1
        for s_ in shape:
            n *= s_
        self.nbytes = n * self.esz
        v = arena[:, off:off + self.nbytes].bitcast(dt)
        if len(shape) == 2:
            v = v.rearrange("p (a b) -> p a b", a=shape[0])
        elif len(shape) == 3:
            v = v.rearrange("p (a b c) -> p a b c", a=shape[0], b=shape[1])
        self.v = v
        self.gran = gran

    def dall(self):
        return self.gran[self.off // GRAN:(self.off + self.nbytes + GRAN - 1) // GRAN]

    def d(self, lo, hi):
        a = self.off + lo * self.esz
        b = self.off + hi * self.esz
        return self.gran[a // GRAN:(b + GRAN - 1) // GRAN]

    def d2(self, i0, lo, hi):
        n1 = self.shape[-1] if len(self.shape) == 2 else self.shape[1] * self.shape[2]
        return self.d(i0 * n1 + lo, i0 * n1 + hi)

    def d2s(self, i0s, lo, hi):
        out = []
        for i0 in i0s:
            out += self.d2(i0, lo, hi)
        return out

    def d3(self, i0, i1, lo, hi):
        n2 = self.shape[2]
        base = (i0 * self.shape[1] + i1) * n2
        return self.d(base + lo, base + hi)


class Arena:
    def __init__(self, nc, nbytes):
        self.t = nc.alloc_sbuf_tensor("arena", [128, nbytes], U8)
        self.nbytes = nbytes
        self.gran = [Dep() for _ in range((nbytes + GRAN - 1) // GRAN + 1)]
        self.top = 0

    def alloc(self, shape, dt):
        off = (self.top + GRAN - 1) // GRAN * GRAN
        n = 4 if dt == F32 else 2
        for s_ in shape:
            n *= s_
        assert off + n <= self.nbytes, ("SBUF arena overflow", off, n, self.nbytes)
        b = SBT(self.t, self.gran, off, list(shape), dt)
        self.top = off + b.nbytes
        self.peak = max(getattr(self, "peak", 0), self.top)
        return b

    def mark(self):
        return self.top

    def release(self, m):
        self.top = m


PV_MIXG, PV_FFNG, PV_MEMG, PV_FING = 0, 8, 16, 24
PV_LCW, PV_LCB, PV_LBA, PV_LBX, PV_LAM = 32, 48, 52, 56, 60
PV_FCW, PV_FCB = 64, 136
PV_N = 160


def _chunked(v):
    return np.ascontiguousarray(v.reshape(-1, 128).T)


def build_program(n_layers=DEPTH, n_seq=SEQ_PER_CORE, debug=None):
    nc = bass.Bass("TRN2", target_bir_lowering=False)
    dram = {}

    def din(name, shape):
        dram[name] = nc.dram_tensor(name, list(shape), F32, kind="ExternalInput").ap()
        return dram[name]

    x_d = din("x", [SEQ_PER_CORE, S, D])
    mem_d = din("mem", [SEQ_PER_CORE, 256, D])
    w_in_d = din("w_in", [DEPTH, D, 6144])
    w_mkv_d = din("w_mem_kv", [DEPTH, D, 1024])
    w_br_d = din("w_branch", [DEPTH, 3, 512, D])
    w_out_d = din("w_out", [DEPTH, D, D])
    w_fg_d = din("w_ffn_gate", [DEPTH, D, 3072])
    w_fu_d = din("w_ffn_up", [DEPTH, D, 3072])
    w_fd_d = din("w_ffn_down", [DEPTH, 3072, D])
    pvec_d = din("pvec", [DEPTH, 128, PV_N])
    wabd_d = din("wabd", [DEPTH, 2, 128, 4, 128])
    cident_d = din("c_ident", [128, 128])
    ctri_d = din("c_tri", [128, 128])
    ckrow_d = din("c_krow", [8, 4, S])
    cqrow_d = din("c_qrow", [4, S])
    coneh_d = din("c_onehot", [8, S])
    cpast_d = din("c_past", [128, 4, 16])
    out_d = nc.dram_tensor("out", [SEQ_PER_CORE, S, D], F32, kind="ExternalOutput").ap()
    dbg_d = {}
    if debug:
        for name, shape in debug.items():
            dbg_d[name] = nc.dram_tensor("dbg_" + name, list(shape), F32, kind="ExternalOutput").ap()

    stack = ExitStack()
    P = Prog(nc, stack)
    total = nc.sbuf_top - nc.sbuf_base - 64
    AR = Arena(nc, total // GRAN * GRAN - GRAN)

    psb = [nc.alloc_psum_tensor("ps%d" % i, [128, 512], F32) for i in range(8)]
    psd = [Dep() for _ in range(8)]

    class Ring:
        def __init__(self, banks):
            self.banks = banks
            self.i = 0

        def next(self):
            b = self.banks[self.i % len(self.banks)]
            self.i += 1
            return psb[b], [psd[b]]

    xT = AR.alloc([8, S], F32)
    hT = AR.alloc([8, S], BF16)
    pv = AR.alloc([DEPTH, PV_N], F32)
    identf = AR.alloc([128], F32)
    identb = AR.alloc([128], BF16)
    onesf = AR.alloc([128], F32)
    onesb = AR.alloc([128], BF16)
    trib = AR.alloc([128], BF16)
    pastm = AR.alloc([4, 16], F32)
    cst = AR.alloc([8], F32)
    lruc = AR.alloc([DEPTH, 8], F32)
    wabd = AR.alloc([2, 4 * 128], BF16)
    gtail = AR.alloc([24, 2], F32)
    RING_SLOTS = 8
    SLOT = 4096
    ring_off = (AR.top + GRAN - 1) // GRAN * GRAN
    AR.top = ring_off + RING_SLOTS * SLOT
    ring_state = {"i": 0}

    def ring_alloc(shape, dt=BF16):
        n = 1
        for s_ in shape:
            n *= s_
        nb = n * 2
        ns = (nb + SLOT - 1) // SLOT
        i = ring_state["i"]
        if i + ns > RING_SLOTS:
            i = 0
        ring_state["i"] = i + ns
        return SBT(AR.t, AR.gran, ring_off + i * SLOT, list(shape), dt)

    phase_base = AR.mark()

    def dbg(name, sbt_ap, deps, dram_ap=None):
        if debug and name in dbg_d:
            P.dma("sp", dbg_d[name] if dram_ap is None else dram_ap, sbt_ap, reads=deps)

    def mm_group(ps_ap, ps_deps, pairs, rdeps):
        n = len(pairs)
        for i, (l_ap, r_ap) in enumerate(pairs):
            P.op("pe", (lambda h, l_ap=l_ap, r_ap=r_ap, i=i: h.matmul(ps_ap, l_ap, r_ap, start=(i == 0), stop=(i == n - 1))),
                 reads=rdeps if i == 0 else (), writes=ps_deps, inc=(i == n - 1))

    def wload(dst, src_ap):
        P.dma("pool", dst.v, src_ap, writes=dst.dall())

    P.dma("sp", pv.v, pvec_d.rearrange("l p n -> p l n"), writes=pv.dall())
    P.dma("sp", identf.v, cident_d, writes=identf.dall())
    P.dma("pool", identb.v, cident_d, writes=identb.dall())
    P.dma("pool", trib.v, ctri_d, writes=trib.dall())
    P.dma("sp", pastm.v, cpast_d, writes=pastm.dall())
    P.op("dve", lambda h: h.memset(onesf.v, 1.0), writes=onesf.dall())
    P.op("dve", lambda h: h.memset(onesb.v, 1.0), writes=onesb.dall())
    P.op("dve", lambda h: h.memset(cst.v[:, 0:1], 1e-6), writes=cst.dall())
    P.op("dve", lambda h: h.memset(cst.v[:, 1:2], 1.0), writes=cst.dall())
    P.op("dve", lambda h: h.memset(gtail.v, 0.0), writes=gtail.dall())

    for l in range(n_layers):
        m0 = AR.mark()
        t_ = AR.alloc([4], F32); e_ = AR.alloc([4], F32); z_ = AR.alloc([4], F32)
        z2 = AR.alloc([4], F32); pl = AR.alloc([4], F32); ab = AR.alloc([4], F32)
        lam = pv.v[:, l, PV_LAM:PV_LAM + 4]
        dd = t_.dall() + e_.dall() + z_.dall() + z2.dall() + pl.dall() + ab.dall() + pv.dall() + lruc.dall()
        P.op("dve", lambda h, lam=lam: h.tensor_scalar(t_.v, lam, -1.0, None, op0=ALU.mult), dd, dd)
        P.op("dve", lambda h, lam=lam: h.tensor_tensor(ab.v, t_.v, lam, op=ALU.max), dd, dd)
        P.op("act", lambda h: h.activation(out=e_.v, in_=ab.v, func=AF.Exp, scale=-1.0), dd, dd)
        P.op("dve", lambda h: h.tensor_scalar(z_.v, e_.v, 2.0, None, op0=ALU.add), dd, dd)
        P.op("dve", lambda h: h.reciprocal(z_.v, z_.v), dd, dd)
        P.op("dve", lambda h: h.tensor_tensor(z_.v, z_.v, e_.v, op=ALU.mult), dd, dd)
        P.op("dve", lambda h: h.tensor_tensor(z2.v, z_.v, z_.v, op=ALU.mult), dd, dd)
        P.op("dve", lambda h: h.tensor_scalar(pl.v, z2.v, 1.0 / 13, 1.0 / 11, op0=ALU.mult, op1=ALU.add), dd, dd)
        for cf in (1.0 / 9, 1.0 / 7, 1.0 / 5, 1.0 / 3, 1.0):
            P.op("dve", lambda h: h.tensor_tensor(pl.v, pl.v, z2.v, op=ALU.mult), dd, dd)
            P.op("dve", lambda h, cf=cf: h.tensor_scalar(pl.v, pl.v, cf, None, op0=ALU.add), dd, dd)
        P.op("dve", lambda h: h.tensor_tensor(pl.v, pl.v, z_.v, op=ALU.mult), dd, dd)
        P.op("dve", lambda h: h.tensor_scalar(pl.v, pl.v, 2.0, None, op0=ALU.mult), dd, dd)
        P.op("dve", lambda h: h.tensor_scalar(t_.v, t_.v, 0.0, None, op0=ALU.max), dd, dd)
        P.op("dve", lambda h: h.tensor_tensor(pl.v, pl.v, t_.v, op=ALU.add), dd, dd)
        P.op("dve", lambda h, l=l: h.tensor_scalar(lruc.v[:, l, 0:4], pl.v, -8.0, None, op0=ALU.mult), dd, dd)
        P.op("dve", lambda h, l=l: h.tensor_scalar(lruc.v[:, l, 4:8], pl.v, -16.0, None, op0=ALU.mult), dd, dd)
        AR.release(m0)

    P.marks = []

    def mark(name):
        P.marks.append((name, len(P.streams['pe']), len(P.streams['act']), len(P.streams['dve'])))

    R_MM = Ring([0, 1, 2, 3])
    R_AUX = Ring([4, 5])
    R_ACC = Ring([6, 7])

    def rmsnorm_to_hT(gain_col, l, nrs=2):
        m0 = AR.mark()
        sq = [AR.alloc([TT], F32) for _ in range(2)]
        rs = [AR.alloc([TT], F32) for _ in range(nrs)] * (2 // nrs)
        for j in range(NT):
            t0 = j * TT
            ps, pd = R_MM.next()
            for c in range(8):
                s_ = sq[c % 2]
                P.op("act", lambda h, s_=s_, c=c: h.activation(out=s_.v, in_=xT.v[:, c, t0:t0 + TT], func=AF.Square),
                     reads=xT.d2(c, t0, t0 + TT), writes=s_.dall())
                P.op("pe", lambda h, s_=s_, c=c, ps=ps: h.matmul(ps[:, :], onesf.v, s_.v, start=(c == 0), stop=(c == 7)),
                     reads=s_.dall() + onesf.dall(), writes=pd)
            r_ = rs[j % 2]
            P.op("act", lambda h, r_=r_, ps=ps: h.activation(out=r_.v, in_=ps[:, :], func=AF.Sqrt, scale=1.0 / D, bias=cst.v[:, 0:1]),
                 reads=pd + cst.dall(), writes=r_.dall())
            P.op("dve", lambda h, r_=r_: h.reciprocal(r_.v, r_.v), reads=r_.dall(), writes=r_.dall())
            for c in range(8):
                P.op("dve", lambda h, r_=r_, c=c: h.scalar_tensor_tensor(
                    out=hT.v[:, c, t0:t0 + TT], in0=xT.v[:, c, t0:t0 + TT], scalar=pv.v[:, l, gain_col + c:gain_col + c + 1],
                    in1=r_.v, op0=ALU.mult, op1=ALU.mult),
                    reads=xT.d2(c, t0, t0 + TT) + r_.dall() + pv.dall(), writes=hT.d2(c, t0, t0 + TT))
        AR.release(m0)

    def hT_rhs(k, t0, n):
        return hT.v[:, k, t0:t0 + n]

    def hT_deps(t0, n):
        return hT.d2s(range(8), t0, t0 + n)

    def load_x(b):
        m0 = AR.mark()
        stg = [AR.alloc([D], F32) for _ in range(4)]
        for tt in range(S // 128):
            s_ = stg[tt % 4]
            P.dma("sp", s_.v, x_d[b, tt * 128:(tt + 1) * 128, :], writes=s_.dall())
            for g in range(2):
                ps, pd = R_MM.next()
                for cc in range(4):
                    c = g * 4 + cc
                    P.op("pe", lambda h, s_=s_, c=c, cc=cc, ps=ps: h.transpose(ps[:, cc * 128:(cc + 1) * 128], s_.v[:, c * 128:(c + 1) * 128], identf.v),
                         reads=s_.dall() + identf.dall(), writes=pd, inc=(cc == 3))
                eng = "act" if g == 0 else "dve"
                outap = xT.v[:, g * 4:(g + 1) * 4, tt * 128:(tt + 1) * 128]
                inap = ps[:, :].rearrange("p (a b) -> p a b", a=4)
                wd = xT.d2s(range(g * 4, g * 4 + 4), tt * 128, (tt + 1) * 128)
                if eng == "act":
                    P.op("act", lambda h, outap=outap, inap=inap: h.copy(out=outap, in_=inap), reads=pd, writes=wd)
                else:
                    P.op("dve", lambda h, outap=outap, inap=inap: h.tensor_copy(out=outap, in_=inap), reads=pd, writes=wd)
        AR.release(m0)

    def final_store(b):
        m0 = AR.mark()
        sq = [AR.alloc([TT], F32) for _ in range(2)]
        rs = [AR.alloc([TT], F32) for _ in range(2)]
        nrm = [AR.alloc([8, TT], F32) for _ in range(2)]
        stg = [AR.alloc([D], F32) for _ in range(4)]
        si = 0
        for j in range(NT):
            t0 = j * TT
            ps, pd = R_MM.next()
            for c in range(8):
                s_ = sq[c % 2]
                P.op("act", lambda h, s_=s_, c=c: h.activation(out=s_.v, in_=xT.v[:, c, t0:t0 + TT], func=AF.Square),
                     reads=xT.d2(c, t0, t0 + TT), writes=s_.dall())
                P.op("pe", lambda h, s_=s_, c=c, ps=ps: h.matmul(ps[:, :], onesf.v, s_.v, start=(c == 0), stop=(c == 7)),
                     reads=s_.dall() + onesf.dall(), writes=pd)
            r_ = rs[j % 2]
            P.op("act", lambda h, r_=r_, ps=ps: h.activation(out=r_.v, in_=ps[:, :], func=AF.Sqrt, scale=1.0 / D, bias=cst.v[:, 0:1]),
                 reads=pd + cst.dall(), writes=r_.dall())
            P.op("dve", lambda h, r_=r_: h.reciprocal(r_.v, r_.v), reads=r_.dall(), writes=r_.dall())
            n_ = nrm[j % 2]
            for c in range(8):
                P.op("dve", lambda h, r_=r_, c=c, n_=n_: h.scalar_tensor_tensor(
                    out=n_.v[:, c, :], in0=xT.v[:, c, t0:t0 + TT], scalar=pv.v[:, 0, PV_FING + c:PV_FING + c + 1],
                    in1=r_.v, op0=ALU.mult, op1=ALU.mult),
                    reads=xT.d2(c, t0, t0 + TT) + r_.dall() + pv.dall(), writes=n_.d2(c, 0, TT))
            for sub in range(4):
                s_ = stg[si % 4]
                si += 1
                for g in range(2):
                    ps2, pd2 = R_MM.next()
                    for cc in range(4):
                        c = g * 4 + cc
                        P.op("pe", lambda h, n_=n_, c=c, cc=cc, ps2=ps2, sub=sub: h.transpose(
                            ps2[:, cc * 128:(cc + 1) * 128], n_.v[:, c, sub * 128:(sub + 1) * 128], identf.v),
                            reads=n_.d2(c, 0, TT) + identf.dall(), writes=pd2, inc=(cc == 3))
                    if g == 0:
                        P.op("act", lambda h, s_=s_, ps2=ps2: h.copy(out=s_.v[:, 0:512], in_=ps2[:, :]), reads=pd2, writes=s_.dall())
                    else:
                        P.op("dve", lambda h, s_=s_, ps2=ps2: h.tensor_copy(out=s_.v[:, 512:1024], in_=ps2[:, :]), reads=pd2, writes=s_.dall())
                P.dma("sp", out_d[b, t0 + sub * 128:t0 + (sub + 1) * 128, :], s_.v, reads=s_.dall())
        AR.release(m0)

    def lru_phase(l, yA):
        m0 = AR.mark()
        xat = [AR.alloc([4, 3 + TT], F32) for _ in range(2)]
        P.op("dve", lambda h: h.memset(xat[0].v[:, :, 0:3], 0.0), writes=xat[0].dall())
        P.dma("pool", wabd.v, wabd_d[l].rearrange("g p c o -> p g (c o)"), writes=wabd.dall())
        wga = ring_alloc([8, 512])
        wload(wga, w_in_d[l, :, 512:1024].rearrange("(k p) n -> p k n", p=128))
        wxa = ring_alloc([8, 512])
        wload(wxa, w_in_d[l, :, 0:512].rearrange("(k p) n -> p k n", p=128))
        for j in range(NT):
            t0 = j * TT
            for c in range(4):
                ps, pd = R_MM.next()
                mm_group(ps[:, :], pd, [(wga.v[:, k, c * 128:(c + 1) * 128], hT_rhs(k, t0, TT)) for k in range(8)],
                         wga.dall() + hT_deps(t0, TT))
                P.op("act", lambda h, ps=ps, c=c, t0=t0: h.activation(out=yA.v[:, c, t0:t0 + TT], in_=ps[:, :], func=GELU),
                     reads=pd, writes=yA.d2(c, t0, t0 + TT))
        NS = 2
        tmp = {}
        for nm in ("xc", "r", "i", "a", "m"):
            tmp[nm] = [AR.alloc([TT], F32) for _ in range(NS)]
        tmp["xb"] = [AR.alloc([TT], BF16) for _ in range(NS)]
        carry = AR.alloc([4], F32)
        r_gate = Ring([4, 5, 6, 7])
        iters = [(j, c) for j in range(NT) for c in range(4)]
        ctx = {}

        def head(idx):
            j, c = iters[idx]
            t0 = j * TT
            xa = xat[j % 2]
            xap = xat[(j - 1) % 2]
            q = idx % NS
            xc, xb = tmp["xc"][q], tmp["xb"][q]
            if j > 0:
                P.op("act", lambda h: h.copy(out=xa.v[:, c, 0:3], in_=xap.v[:, c, TT:TT + 3]),
                     reads=xap.d2(c, TT, TT + 3), writes=xa.d2(c, 0, 3))
            ps, pd = R_MM.next()
            mm_group(ps[:, :], pd, [(wxa.v[:, k, c * 128:(c + 1) * 128], hT_rhs(k, t0, TT)) for k in range(8)],
                     wxa.dall() + hT_deps(t0, TT))
            P.op("act", lambda h: h.copy(out=xa.v[:, c, 3:3 + TT], in_=ps[:, :]), reads=pd, writes=xa.d2(c, 3, 3 + TT))
            cw = lambda k: pv.v[:, l, PV_LCW + k * 4 + c:PV_LCW + k * 4 + c + 1]
            cb = pv.v[:, l, PV_LCB + c:PV_LCB + c + 1]
            xin = lambda k: xa.v[:, c, k:k + TT]
            xdeps = xa.d2(c, 0, TT + 3) + pv.dall()
            P.op("dve", lambda h: h.tensor_scalar(xc.v, xin(3), cw(3), cb, op0=ALU.mult, op1=ALU.add), reads=xdeps, writes=xc.dall())
            for k in range(3):
                P.op("dve", lambda h, k=k: h.scalar_tensor_tensor(out=xc.v, in0=xin(k), scalar=cw(k), in1=xc.v, op0=ALU.mult, op1=ALU.add),
                     reads=xdeps + xc.dall(), writes=xc.dall())
            P.op("dve", lambda h: h.tensor_copy(out=xb.v, in_=xc.v), reads=xc.dall(), writes=xb.dall())
            psa, pda = r_gate.next()
            P.op("pe", lambda h: h.matmul(psa[:, :], wabd.v[:, 0, c * 128:(c + 1) * 128], xb.v, start=True, stop=True),
                 reads=xb.dall() + wabd.dall(), writes=pda)
            psx, pdx = r_gate.next()
            P.op("pe", lambda h: h.matmul(psx[:, :], wabd.v[:, 1, c * 128:(c + 1) * 128], xb.v, start=True, stop=True),
                 reads=xb.dall() + wabd.dall(), writes=pdx)
            ctx[idx] = (psa, pda, psx, pdx)

        def tail(idx):
            j, c = iters[idx]
            t0 = j * TT
            q = idx % NS
            xc, r_, i_, a_, m_ = (tmp[n][q] for n in ("xc", "r", "i", "a", "m"))
            psa, pda, psx, pdx = ctx.pop(idx)
            P.op("act", lambda h: h.activation(out=r_.v, in_=psa[:, :], func=AF.Sigmoid, bias=pv.v[:, l, PV_LBA + c:PV_LBA + c + 1]),
                 reads=pda + pv.dall(), writes=r_.dall())
            P.op("act", lambda h: h.activation(out=i_.v, in_=psx[:, :], func=AF.Sigmoid, bias=pv.v[:, l, PV_LBX + c:PV_LBX + c + 1]),
                 reads=pdx + pv.dall(), writes=i_.dall())
            P.op("act", lambda h: h.activation(out=a_.v, in_=r_.v, func=AF.Exp, scale=lruc.v[:, l, c:c + 1]),
                 reads=r_.dall() + lruc.dall(), writes=a_.dall())
            P.op("act", lambda h: h.activation(out=m_.v, in_=r_.v, func=AF.Exp, scale=lruc.v[:, l, 4 + c:5 + c]),
                 reads=r_.dall() + lruc.dall(), writes=m_.dall())
            P.op("act", lambda h: h.activation(out=m_.v, in_=m_.v, func=AF.Ln, scale=-1.0, bias=cst.v[:, 1:2]),
                 reads=m_.dall() + cst.dall(), writes=m_.dall())
            P.op("act", lambda h: h.activation(out=m_.v, in_=m_.v, func=AF.Exp, scale=0.5), reads=m_.dall(), writes=m_.dall())
            P.op("dve", lambda h: h.tensor_tensor(i_.v, i_.v, xc.v, op=ALU.mult), reads=i_.dall() + xc.dall(), writes=i_.dall())
            P.op("dve", lambda h: h.tensor_tensor(i_.v, i_.v, m_.v, op=ALU.mult), reads=i_.dall() + m_.dall(), writes=i_.dall())
            hcur = r_
            if j == 0:
                P.op("dve", lambda h: h.tensor_tensor_scan(hcur.v, a_.v, i_.v, 0.0, ALU.mult, ALU.add),
                     reads=a_.dall() + i_.dall(), writes=hcur.dall())
            else:
                P.op("dve", lambda h: h.tensor_tensor_scan(hcur.v, a_.v, i_.v, carry.v[:, c:c + 1], ALU.mult, ALU.add),
                     reads=a_.dall() + i_.dall() + carry.dall(), writes=hcur.dall())
            P.op("dve", lambda h: h.tensor_copy(out=carry.v[:, c:c + 1], in_=hcur.v[:, TT - 1:TT]), reads=hcur.dall(), writes=carry.dall())
            P.op("dve", lambda h: h.tensor_tensor(yA.v[:, c, t0:t0 + TT], yA.v[:, c, t0:t0 + TT], hcur.v, op=ALU.mult),
                 reads=hcur.dall() + yA.d2(c, t0, t0 + TT), writes=yA.d2(c, t0, t0 + TT))

        for idx in range(len(iters)):
            if idx == 0:
                head(0)
            if idx + 1 < len(iters):
                head(idx + 1)
            tail(idx)
        AR.release(m0)

    def xattn_phase(l, b, yC):
        m0 = AR.mark()
        memK = AR.alloc([4, 256], BF16)
        memV = AR.alloc([2, 512], BF16)
        memh = AR.alloc([8, 256], BF16)
        m1 = AR.mark()
        mstg = [AR.alloc([D], F32)] * 2
        memT = AR.alloc([8, 256], F32)
        sq = [AR.alloc([256], F32) for _ in range(2)]
        rs = AR.alloc([256], F32)
        wk = ring_alloc([8, 512])
        wload(wk, w_mkv_d[l, :, 0:512].rearrange("(k p) n -> p k n", p=128))
        wv = ring_alloc([8, 512])
        wload(wv, w_mkv_d[l, :, 512:1024].rearrange("(k p) n -> p k n", p=128))
        wq = ring_alloc([8, 512])
        wload(wq, w_in_d[l, :, 2560:3072].rearrange("(k p) n -> p k n", p=128))
        for mt in range(2):
            s_ = mstg[mt]
            P.dma("sp", s_.v, mem_d[b, mt * 128:(mt + 1) * 128, :], writes=s_.dall())
            for g in range(2):
                ps, pd = R_MM.next()
                for cc in range(4):
                    c = g * 4 + cc
                    P.op("pe", lambda h, s_=s_, c=c, cc=cc, ps=ps: h.transpose(ps[:, cc * 128:(cc + 1) * 128], s_.v[:, c * 128:(c + 1) * 128], identf.v),
                         reads=s_.dall() + identf.dall(), writes=pd, inc=(cc == 3))
                outap = memT.v[:, g * 4:(g + 1) * 4, mt * 128:(mt + 1) * 128]
                inap = ps[:, :].rearrange("p (a b) -> p a b", a=4)
                P.op("act", lambda h, outap=outap, inap=inap: h.copy(out=outap, in_=inap), reads=pd, writes=memT.dall())
        ps, pd = R_MM.next()
        for c in range(8):
            s_ = sq[c % 2]
            P.op("act", lambda h, s_=s_, c=c: h.activation(out=s_.v, in_=memT.v[:, c, :], func=AF.Square),
                 reads=memT.dall(), writes=s_.dall())
            P.op("pe", lambda h, s_=s_, c=c, ps=ps: h.matmul(ps[:, 0:256], onesf.v, s_.v, start=(c == 0), stop=(c == 7)),
                 reads=s_.dall() + onesf.dall(), writes=pd)
        P.op("act", lambda h, ps=ps: h.activation(out=rs.v, in_=ps[:, 0:256], func=AF.Sqrt, scale=1.0 / D, bias=cst.v[:, 0:1]),
             reads=pd + cst.dall(), writes=rs.dall())
        P.op("dve", lambda h: h.reciprocal(rs.v, rs.v), reads=rs.dall(), writes=rs.dall())
        for c in range(8):
            P.op("dve", lambda h, c=c: h.scalar_tensor_tensor(
                out=memh.v[:, c, :], in0=memT.v[:, c, :], scalar=pv.v[:, l, PV_MEMG + c:PV_MEMG + c + 1],
                in1=rs.v, op0=ALU.mult, op1=ALU.mult),
                reads=memT.dall() + rs.dall() + pv.dall(), writes=memh.dall())
        for hd in range(4):
            ps, pd = R_MM.next()
            mm_group(ps[:, 0:256], pd, [(wk.v[:, k, hd * 128:(hd + 1) * 128], memh.v[:, k, :]) for k in range(8)],
                     wk.dall() + memh.dall())
            P.op("act", lambda h, ps=ps, hd=hd: h.copy(out=memK.v[:, hd, :], in_=ps[:, 0:256]), reads=pd, writes=memK.dall())
        for mt in range(2):
            ps, pd = R_MM.next()
            mm_group(ps[:, :], pd, [(memh.v[:, k, mt * 128:(mt + 1) * 128], wv.v[:, k, :]) for k in range(8)],
                     wv.dall() + memh.dall())
            P.op("act", lambda h, ps=ps, mt=mt: h.copy(out=memV.v[:, mt, :], in_=ps[:, :]), reads=pd, writes=memV.dall())
        AR.release(m1)
        qx = [AR.alloc([TT], BF16) for _ in range(2)]
        pt = [AR.alloc([2, TT], BF16) for _ in range(2)]
        rd = [AR.alloc([TT], F32) for _ in range(2)]
        sc = 128 ** -0.5
        r_q = Ring([0, 1])
        r_s = Ring([2, 3, 4, 5])
        iters = [(j, hd) for j in range(NT) for hd in range(4)]
        ctx = {}

        def head(idx):
            j, hd = iters[idx]
            t0 = j * TT
            q_ = qx[idx % 2]
            ps, pd = r_q.next()
            mm_group(ps[:, :], pd, [(wq.v[:, k, hd * 128:(hd + 1) * 128], hT_rhs(k, t0, TT)) for k in range(8)],
                     wq.dall() + hT_deps(t0, TT))
            P.op("act", lambda h: h.copy(out=q_.v, in_=ps[:, :]), reads=pd, writes=q_.dall())
            sb = []
            for mt in range(2):
                sps, spd = r_s.next()
                P.op("pe", lambda h, sps=sps, mt=mt: h.matmul(sps[:, :], memK.v[:, hd, mt * 128:(mt + 1) * 128], q_.v, start=True, stop=True),
                     reads=q_.dall() + memK.dall(), writes=spd)
                sb.append((sps, spd))
            ctx[idx] = sb

        def tail(idx):
            j, hd = iters[idx]
            t0 = j * TT
            p_ = pt[idx % 2]; r_ = rd[idx % 2]
            sb = ctx.pop(idx)
            for mt in range(2):
                sps, spd = sb[mt]
                P.op("act", lambda h, sps=sps, mt=mt: h.activation(out=p_.v[:, mt, :], in_=sps[:, :], func=AF.Exp, scale=sc),
                     reads=spd, writes=p_.dall())
            ops, opd = psb[6], [psd[6]]
            mm_group(ops[:, :], opd, [(memV.v[:, mt, hd * 128:(hd + 1) * 128], p_.v[:, mt, :]) for mt in range(2)],
                     memV.dall() + p_.dall())
            dps, dpd = psb[7], [psd[7]]
            mm_group(dps[:, :], dpd, [(onesb.v, p_.v[:, mt, :]) for mt in range(2)], onesb.dall() + p_.dall())
            P.op("act", lambda h: h.activation(out=r_.v, in_=dps[:, :], func=AF.Ln), reads=dpd, writes=r_.dall())
            P.op("act", lambda h: h.activation(out=r_.v, in_=r_.v, func=AF.Exp, scale=-1.0), reads=r_.dall(), writes=r_.dall())
            P.op("dve", lambda h: h.tensor_tensor(yC.v[:, hd, t0:t0 + TT], ops[:, :], r_.v, op=ALU.mult),
                 reads=opd + r_.dall(), writes=yC.d2(hd, t0, t0 + TT))

        for idx in range(len(iters)):
            if idx == 0:
                head(0)
            if idx + 1 < len(iters):
                head(idx + 1)
            tail(idx)
        AR.release(m0)

    def moba_phase(l, yB):
        m0 = AR.mark()
        NSET = 2
        KA = [[AR.alloc([S], BF16) for _ in range(2)] for _ in range(NSET)]
        QA = [[AR.alloc([S], BF16) for _ in range(2)] for _ in range(NSET)]
        VT = AR.alloc([16, 2, 128], BF16)
        NPT = 3
        PT = [AR.alloc([2, 256], BF16) for _ in range(NPT)]
        kmT = [[AR.alloc([8], BF16) for _ in range(2)] for _ in range(NSET)]
        ksum = AR.alloc([8], F32)
        gm = AR.alloc([16], F32)
        top8 = AR.alloc([16], F32)
        stage = [[AR.alloc([2, 128], BF16) for _ in range(2)] for _ in range(4)]
        dsh = [AR.alloc([256], F32) for _ in range(2)]
        r_proj = Ring([0, 1])
        r_sc = Ring([3, 4, 5])
        r_acc = Ring([6, 7])
        gate_bank, gate_dep = psb[2], [psd[2]]
        for s_ in range(NSET):
            for t_ in KA[s_] + QA[s_]:
                P.op("dve", lambda h, t_=t_: h.memset(t_.v, 0.0), writes=t_.dall())
            for hh in range(2):
                P.dma("pool", QA[s_][hh].v[64:68, :], cqrow_d, writes=QA[s_][hh].dall())
                P.dma("pool", KA[s_][hh].v[96:104, :], coneh_d, writes=KA[s_][hh].dall())
        P.op("dve", lambda h: h.memset(VT.v, 1.0), writes=VT.dall())
        for q4 in range(4):
            for sub in range(2):
                t_ = stage[q4][sub]
                P.op("dve", lambda h, t_=t_: h.memset(t_.v, 0.0), writes=t_.dall())
        state = {"pti": 0, "acc": 0, "wv": {}}
        yield

        def project(pair):
            s_ = pair % NSET
            ka, qa, km = KA[s_], QA[s_], kmT[s_]
            wq = ring_alloc([8, 128]); wload(wq, w_in_d[l, :, 1024 + pair * 128:1024 + (pair + 1) * 128].rearrange("(k p) n -> p k n", p=128))
            wk = ring_alloc([8, 128]); wload(wk, w_in_d[l, :, 1536 + pair * 128:1536 + (pair + 1) * 128].rearrange("(k p) n -> p k n", p=128))
            for hh in range(2):
                P.dma("pool", ka[hh].v[64:68, :], ckrow_d[pair * 2 + hh], writes=ka[hh].dall())
            for j in range(NT):
                t0 = j * TT
                ps, pd = r_proj.next()
                mm_group(ps[:, :], pd, [(wk.v[:, k, :], hT_rhs(k, t0, TT)) for k in range(8)], wk.dall() + hT_deps(t0, TT))
                for hh in range(2):
                    P.op("act", lambda h, ps=ps, hh=hh: h.copy(out=ka[hh].v[0:64, t0:t0 + TT], in_=ps[hh * 64:(hh + 1) * 64, :]),
                         reads=pd, writes=ka[hh].d(t0, t0 + TT))
                ps, pd = r_proj.next()
                mm_group(ps[:, :], pd, [(wq.v[:, k, :], hT_rhs(k, t0, TT)) for k in range(8)], wq.dall() + hT_deps(t0, TT))
                for hh in range(2):
                    P.op("act", lambda h, ps=ps, hh=hh: h.mul(out=qa[hh].v[0:64, t0:t0 + TT], in_=ps[hh * 64:(hh + 1) * 64, :], mul=0.125),
                         reads=pd, writes=qa[hh].d(t0, t0 + TT))
            for hh in range(2):
                P.op("dve", lambda h, hh=hh: h.tensor_reduce(out=ksum.v[0:64, :], in_=ka[hh].v[0:64, :].rearrange("p (a b) -> p a b", a=8),
                                                              axis=AX.X, op=ALU.add),
                     reads=ka[hh].dall(), writes=ksum.dall())
                P.op("act", lambda h, hh=hh: h.mul(out=km[hh].v[0:64, :], in_=ksum.v[0:64, :], mul=1.0 / 256), reads=ksum.dall(), writes=km[hh].dall())
            for qb in range(4, 8):
                for sub in range(2):
                    qs = qb * 256 + sub * 128
                    gcol = ((qb - 4) * 2 + sub) * 16
                    for hh in range(2):
                        P.op("pe", lambda h, hh=hh, qs=qs, gcol=gcol: h.matmul(gate_bank[:, gcol + hh * 8:gcol + (hh + 1) * 8], qa[hh].v[0:64, qs:qs + 128],
                                                                             km[hh].v[0:64, :], start=True, stop=True),
                             reads=qa[hh].d(qs, qs + 128) + km[hh].dall(), writes=gate_dep)
                    P.op("dve", lambda h, qb=qb, gcol=gcol: h.tensor_tensor(gm.v, gate_bank[:, gcol:gcol + 16], pastm.v[:, qb - 4, :], op=ALU.add),
                         reads=gate_dep + pastm.dall(), writes=gm.dall())
                    st_ = stage[qb - 4][sub]
                    for hh in range(2):
                        P.op("dve", lambda h, hh=hh: h.max(out=top8.v[:, hh * 8:(hh + 1) * 8], in_=gm.v[:, hh * 8:(hh + 1) * 8]),
                             reads=gm.dall(), writes=top8.dall())
                        P.op("dve", lambda h, hh=hh, st_=st_, qb=qb: h.tensor_scalar(
                            st_.v[:, hh, 96:96 + qb], gm.v[:, hh * 8:hh * 8 + qb], top8.v[:, hh * 8 + 2:hh * 8 + 3], -30000.0,
                            op0=ALU.is_lt, op1=ALU.mult),
                            reads=gm.dall() + top8.dall(), writes=st_.dall())

        def project_v(pair):
            vt = VT
            wv = ring_alloc([8, 128]); wload(wv, w_in_d[l, :, 2048 + pair * 128:2048 + (pair + 1) * 128].rearrange("(k p) n -> p k n", p=128))
            for j in range(NT):
                ps, pd = r_proj.next()
                for sub in range(4):
                    tk = j * 4 + sub
                    mm_group(ps[:, sub * 128:(sub + 1) * 128], pd, [(hT_rhs(k, tk * 128, 128), wv.v[:, k, :]) for k in range(8)],
                             wv.dall() + hT_deps(tk * 128, 128))
                pv4 = ps[:, :].rearrange("p (a b) -> p a b", a=4)
                P.op("dve", lambda h, pv4=pv4, j=j: h.tensor_copy(out=vt.v[:, j * 4:(j + 1) * 4, 0, 0:64], in_=pv4[:, :, 0:64]),
                     reads=pd, writes=vt.d2s(range(j * 4, j * 4 + 4), 0, 256))
                P.op("dve", lambda h, pv4=pv4, j=j: h.tensor_copy(out=vt.v[:, j * 4:(j + 1) * 4, 1, 64:128], in_=pv4[:, :, 64:128]),
                     reads=pd, writes=vt.d2s(range(j * 4, j * 4 + 4), 0, 256))

        def mask_rows(pair):
            s_ = pair % NSET
            qa = QA[s_]
            for qb in range(4, 8):
                for sub in range(2):
                    qs = qb * 256 + sub * 128
                    st_ = stage[qb - 4][sub]
                    tps, tpd = r_sc.next()
                    tpb = tps[:, :].bitcast(BF16)
                    for hh in range(2):
                        P.op("pe", lambda h, tpb=tpb, st_=st_, hh=hh: h.transpose(tpb[:, hh * 128:(hh + 1) * 128], st_.v[:, hh, :], identb.v),
                             reads=st_.dall() + identb.dall(), writes=tpd, inc=(hh == 1))
                    for hh in range(2):
                        P.op("act", lambda h, tpb=tpb, hh=hh, qs=qs: h.copy(out=qa[hh].v[96:104, qs:qs + 128], in_=tpb[96:104, hh * 128:(hh + 1) * 128]),
                             reads=tpd, writes=qa[hh].d(qs, qs + 128))

        def attention(pair, qbs):
            s_ = pair % NSET
            ka, qa, vt = KA[s_], QA[s_], VT
            units = []
            for qb in qbs:
                q0 = qb * 256
                for hh in range(2):
                    ai = state["acc"]; state["acc"] += 1
                    ops, opd = r_acc.next()
                    ds_ = dsh[ai % 2]
                    nun = qb + 1
                    for ui in range(nun):
                        units.append(dict(qb=qb, q0=q0, hh=hh, ui=ui, nun=nun, ops=ops, opd=opd, ds=ds_))
            for u in units:
                u["p"] = PT[state["pti"] % NPT]; state["pti"] += 1
                u["sps"], u["spd"] = None, None

            def emit_qk(u):
                sps, spd = r_sc.next()
                u["sps"], u["spd"] = sps, spd
                hh, q0, qb = u["hh"], u["q0"], u["qb"]
                if u["ui"] == 0:
                    k0 = 2 * qb
                    P.op("pe", lambda h: h.matmul(sps[:, 0:256], ka[hh].v[:, k0 * 128:(k0 + 1) * 128], qa[hh].v[:, q0:q0 + 256], start=True, stop=False),
                         reads=ka[hh].d(k0 * 128, (k0 + 1) * 128) + qa[hh].d(q0, q0 + 256) + trib.dall() + identb.dall(), writes=spd, inc=False)
                    P.op("pe", lambda h: h.matmul(sps[:, 0:128], identb.v, trib.v, start=False, stop=True), writes=spd, inc=False)
                    P.op("pe", lambda h: h.matmul(sps[:, 384:512], ka[hh].v[:, (k0 + 1) * 128:(k0 + 2) * 128], qa[hh].v[:, q0 + 128:q0 + 256], start=True, stop=False),
                         reads=ka[hh].d((k0 + 1) * 128, (k0 + 2) * 128) + qa[hh].d(q0, q0 + 256), writes=spd, inc=False)
                    P.op("pe", lambda h: h.matmul(sps[:, 384:512], identb.v, trib.v, start=False, stop=True), writes=spd)
                else:
                    kp = u["ui"] - 1
                    for i2 in range(2):
                        kt = 2 * kp + i2
                        P.op("pe", lambda h, kt=kt, i2=i2: h.matmul(sps[:, i2 * 256:(i2 + 1) * 256], ka[hh].v[:, kt * 128:(kt + 1) * 128],
                                                                  qa[hh].v[:, q0:q0 + 256], start=True, stop=True),
                             reads=ka[hh].d(kt * 128, (kt + 1) * 128) + qa[hh].d(q0, q0 + 256), writes=spd, inc=(i2 == 1))

            def emit_rest(u):
                sps, spd, p_ = u["sps"], u["spd"], u["p"]
                hh, q0, qb, ops, opd = u["hh"], u["q0"], u["qb"], u["ops"], u["opd"]
                last = (u["ui"] == u["nun"] - 1)
                if u["ui"] == 0:
                    k0 = 2 * qb
                    P.op("act", lambda h: h.activation(out=p_.v[:, 0, :], in_=sps[:, 0:256], func=AF.Exp), reads=spd, writes=p_.dall())
                    P.op("act", lambda h: h.activation(out=p_.v[:, 1, 128:256], in_=sps[:, 384:512], func=AF.Exp), reads=spd, writes=p_.dall())
                    P.op("pe", lambda h: h.matmul(ops[:, 0:256], vt.v[:, k0, hh, :], p_.v[:, 0, :], start=True, stop=False),
                         reads=vt.d2(k0, 0, 256) + p_.dall(), writes=opd, inc=False)
                    P.op("pe", lambda h: h.matmul(ops[:, 128:256], vt.v[:, k0 + 1, hh, :], p_.v[:, 1, 128:256], start=False, stop=last),
                         reads=vt.d2(k0 + 1, 0, 256) + p_.dall(), writes=opd, inc=True)
                else:
                    kp = u["ui"] - 1
                    P.op("act", lambda h: h.activation(out=p_.v.rearrange("p a b -> p (a b)"), in_=sps[:, :], func=AF.Exp),
                         reads=spd, writes=p_.dall())
                    for i2 in range(2):
                        kt = 2 * kp + i2
                        P.op("pe", lambda h, kt=kt, i2=i2: h.matmul(ops[:, 0:256], vt.v[:, kt, hh, :], p_.v[:, i2, :], start=False, stop=(last and i2 == 1)),
                             reads=vt.d2(kt, 0, 256) + p_.dall(), writes=opd, inc=(i2 == 1))
                if last:
                    ds_ = u["ds"]
                    olo, dlo = (0, 64) if hh == 0 else (64, 0)
                    P.op("act", lambda h: h.copy(out=ds_.v[olo:olo + 64, :], in_=ops[dlo:dlo + 64, 0:256]), reads=opd, writes=ds_.dall())
                    P.op("dve", lambda h: h.reciprocal(ds_.v[olo:olo + 64, :], ds_.v[olo:olo + 64, :]), reads=ds_.dall(), writes=ds_.dall())
                    P.op("dve", lambda h: h.tensor_tensor(yB.v[olo:olo + 64, pair, q0:q0 + 256], ops[olo:olo + 64, 0:256], ds_.v[olo:olo + 64, :], op=ALU.mult),
                         reads=opd + ds_.dall(), writes=yB.d2(pair, q0, q0 + 256))

            DEPTH_QK = 2
            for i in range(min(DEPTH_QK, len(units))):
                emit_qk(units[i])
            for i, u in enumerate(units):
                if i + DEPTH_QK < len(units):
                    emit_qk(units[i + DEPTH_QK])
                emit_rest(u)

        project(0)
        for pair in range(4):
            project_v(pair)
            attention(pair, range(0, 4))
            mask_rows(pair)
            if pair + 1 < 4:
                project(pair + 1)
            attention(pair, range(4, 8))
        AR.release(m0)

    def merge_phase(l, yA, yB, yC):
        m0 = AR.mark()
        mg = AR.alloc([8, 1024], BF16)
        NGT = 2 if (AR.nbytes - AR.top) >= 2 * 3 * 2048 + 1024 else 1
        gt = [[AR.alloc([TT], F32) for _ in range(3)] for _ in range(NGT)]
        gi = 0
        ys = [yA, yB, yC]
        gview = w_in_d[l, :, 3072:6144].rearrange("(k p) (n d) -> p k n d", p=128, n=3)
        bview = w_br_d[l].rearrange("n (k p) d -> p k n d", p=128)
        for hf in range(2):
            for dc in range(8):
                G = ring_alloc([8, 3, 128])
                B = ring_alloc([4, 3, 128])
                for n in range(3):
                    P.dma("pool", G.v[:, :, n, :], gview[:, :, n, dc * 128:(dc + 1) * 128], writes=G.dall())
                    P.dma("pool", B.v[:, :, n, :], bview[:, :, n, dc * 128:(dc + 1) * 128], writes=B.dall())
                for jj in range(2):
                    t0 = hf * 1024 + jj * TT
                    gts = gt[gi % NGT]; gi += 1
                    for n in range(3):
                        ps, pd = R_MM.next()
                        mm_group(ps[:, :], pd, [(G.v[:, k, n, :], hT_rhs(k, t0, TT)) for k in range(8)],
                                 G.dall() + hT_deps(t0, TT))
                        P.op("act", lambda h, ps=ps, n=n, gts=gts: h.activation(out=gts[n].v, in_=ps[:, :], func=AF.Sigmoid), reads=pd, writes=gts[n].dall())
                        ps2, pd2 = R_MM.next()
                        mm_group(ps2[:, :], pd2, [(B.v[:, k, n, :], ys[n].v[:, k, t0:t0 + TT]) for k in range(4)],
                                 B.dall() + ys[n].d2s(range(4), t0, t0 + TT))
                        P.op("dve", lambda h, ps2=ps2, n=n, gts=gts: h.tensor_tensor(gts[n].v, ps2[:, :], gts[n].v, op=ALU.mult),
                             reads=pd2 + gts[n].dall(), writes=gts[n].dall())
                    P.op("dve", lambda h, gts=gts: h.tensor_tensor(gts[0].v, gts[0].v, gts[1].v, op=ALU.add),
                         reads=gts[0].dall() + gts[1].dall(), writes=gts[0].dall())
                    P.op("dve", lambda h, dc=dc, jj=jj, gts=gts: h.tensor_tensor(mg.v[:, dc, jj * TT:(jj + 1) * TT], gts[0].v, gts[2].v, op=ALU.add),
                         reads=gts[0].dall() + gts[2].dall(), writes=mg.d2(dc, jj * TT, (jj + 1) * TT))
            for pp in range(2):
                Wo = ring_alloc([8, 512])
                wload(Wo, w_out_d[l, :, pp * 512:(pp + 1) * 512].rearrange("(k p) n -> p k n", p=128))
                for jj in range(2):
                    t0 = hf * 1024 + jj * TT
                    for dci in range(4):
                        dc = pp * 4 + dci
                        ps, pd = R_MM.next()
                        mm_group(ps[:, :], pd, [(Wo.v[:, k, dci * 128:(dci + 1) * 128], mg.v[:, k, jj * TT:(jj + 1) * TT]) for k in range(8)],
                                 Wo.dall() + mg.d2s(range(8), jj * TT, (jj + 1) * TT))
                        P.op("dve", lambda h, ps=ps, dc=dc, t0=t0: h.tensor_tensor(xT.v[:, dc, t0:t0 + TT], ps[:, :], xT.v[:, dc, t0:t0 + TT], op=ALU.add),
                             reads=pd + xT.d2(dc, t0, t0 + TT), writes=xT.d2(dc, t0, t0 + TT))
        AR.release(m0)

    def ffn_phase(l):
        m0 = AR.mark()
        act = AR.alloc([24, 1024], BF16)
        gbuf = [AR.alloc([2 + 1024], F32) for _ in range(2)]
        acc = [AR.alloc([TT], F32) for _ in range(2)]
        gel = [AR.alloc([TT], F32) for _ in range(2)]
        it = 0
        R_F = Ring([0, 1, 2, 3, 4, 5, 6, 7])
        for hf in range(2):
            for ffg in range(12):
                Wg = ring_alloc([8, 256]); wload(Wg, w_fg_d[l, :, ffg * 256:(ffg + 1) * 256].rearrange("(k p) n -> p k n", p=128))
                Wu = ring_alloc([8, 256]); wload(Wu, w_fu_d[l, :, ffg * 256:(ffg + 1) * 256].rearrange("(k p) n -> p k n", p=128))
                for fci in range(2):
                    fc = ffg * 2 + fci
                    gb = gbuf[fc % 2]
                    if hf == 0:
                        P.op("dve", lambda h, gb=gb: h.memset(gb.v[:, 0:2], 0.0), writes=gb.d(0, 2))
                    else:
                        P.op("dve", lambda h, gb=gb, fc=fc: h.tensor_copy(out=gb.v[:, 0:2], in_=gtail.v[:, fc, :]), reads=gtail.dall(), writes=gb.d(0, 2))
                    for jj in range(2):
                        t0 = hf * 1024 + jj * TT
                        g0 = 2 + jj * TT
                        ps, pd = R_F.next()
                        mm_group(ps[:, :], pd, [(Wg.v[:, k, fci * 128:(fci + 1) * 128], hT_rhs(k, t0, TT)) for k in range(8)],
                                 Wg.dall() + hT_deps(t0, TT))
                        ups, upd = R_F.next()
                        mm_group(ups[:, :], upd, [(Wu.v[:, k, fci * 128:(fci + 1) * 128], hT_rhs(k, t0, TT)) for k in range(8)],
                                 Wu.dall() + hT_deps(t0, TT))
                        P.op("act", lambda h, ps=ps, gb=gb, g0=g0: h.copy(out=gb.v[:, g0:g0 + TT], in_=ps[:, :]), reads=pd, writes=gb.d(g0, g0 + TT))
                        a_ = acc[it % 2]; ge = gel[it % 2]; it += 1
                        cw = lambda k, fc=fc: pv.v[:, l, PV_FCW + k * 24 + fc:PV_FCW + k * 24 + fc + 1]
                        cb = pv.v[:, l, PV_FCB + fc:PV_FCB + fc + 1]
                        gd = gb.d(g0 - 2, g0 + TT) + pv.dall()
                        P.op("dve", lambda h, a_=a_, gb=gb, g0=g0, cw=cw, cb=cb: h.tensor_scalar(a_.v, gb.v[:, g0:g0 + TT], cw(2), cb, op0=ALU.mult, op1=ALU.add),
                             reads=gd, writes=a_.dall())
                        P.op("dve", lambda h, a_=a_, gb=gb, g0=g0, cw=cw: h.scalar_tensor_tensor(out=a_.v, in0=gb.v[:, g0 - 1:g0 - 1 + TT], scalar=cw(1), in1=a_.v,
                                                                                               op0=ALU.mult, op1=ALU.add),
                             reads=gd + a_.dall(), writes=a_.dall())
                        P.op("dve", lambda h, a_=a_, gb=gb, g0=g0, cw=cw: h.scalar_tensor_tensor(out=a_.v, in0=gb.v[:, g0 - 2:g0 - 2 + TT], scalar=cw(0), in1=a_.v,
                                                                                               op0=ALU.mult, op1=ALU.add),
                             reads=gd + a_.dall(), writes=a_.dall())
                        P.op("act", lambda h, a_=a_, ge=ge: h.activation(out=ge.v, in_=a_.v, func=GELU), reads=a_.dall(), writes=ge.dall())
                        P.op("dve", lambda h, ups=ups, ge=ge, fc=fc, jj=jj: h.tensor_tensor(act.v[:, fc, jj * TT:(jj + 1) * TT], ups[:, :], ge.v, op=ALU.mult),
                             reads=upd + ge.dall(), writes=act.d2(fc, jj * TT, (jj + 1) * TT))
                    if hf == 0:
                        P.op("dve", lambda h, gb=gb, fc=fc: h.tensor_copy(out=gtail.v[:, fc, :], in_=gb.v[:, 1024:1026]), reads=gb.d(1024, 1026), writes=gtail.dall())
            for dcg in range(4):
                Wd = ring_alloc([24, 256]); wload(Wd, w_fd_d[l, :, dcg * 256:(dcg + 1) * 256].rearrange("(k p) n -> p k n", p=128))
                for jj in range(2):
                    t0 = hf * 1024 + jj * TT
                    for dci in range(2):
                        dc = dcg * 2 + dci
                        ps, pd = R_F.next()
                        mm_group(ps[:, :], pd, [(Wd.v[:, k, dci * 128:(dci + 1) * 128], act.v[:, k, jj * TT:(jj + 1) * TT]) for k in range(24)],
                                 Wd.dall() + act.d2s(range(24), jj * TT, (jj + 1) * TT))
                        P.op("dve", lambda h, ps=ps, dc=dc, t0=t0: h.tensor_tensor(xT.v[:, dc, t0:t0 + TT], ps[:, :], xT.v[:, dc, t0:t0 + TT], op=ALU.add),
                             reads=pd + xT.d2(dc, t0, t0 + TT), writes=xT.d2(dc, t0, t0 + TT))
        AR.release(m0)

    for b in range(n_seq):
        mark('load'); load_x(b)
        for l in range(n_layers):
            mY = AR.mark()
            yB = AR.alloc([4, S], BF16)
            mark('norm1')
            gen = moba_phase(l, yB)
            next(gen)
            rmsnorm_to_hT(PV_MIXG, l, nrs=1)
            mark('moba')
            for _ in gen:
                pass
            yA = AR.alloc([4, S], BF16)
            mark('lru'); lru_phase(l, yA)
            yC = AR.alloc([4, S], BF16)
            mark('xattn'); xattn_phase(l, b, yC)
            if debug and b == 0:
                for nm, y_ in (("yA%d" % l, yA), ("yB%d" % l, yB), ("yC%d" % l, yC)):
                    if nm in dbg_d:
                        P.dma("pool", dbg_d[nm].rearrange("(c p) t -> p c t", p=128), y_.v, reads=y_.dall())
            mark('merge'); merge_phase(l, yA, yB, yC)
            AR.release(mY)
            if debug and b == 0 and ("xmid%d" % l) in dbg_d:
                P.dma("sp", dbg_d["xmid%d" % l].rearrange("(c p) t -> p c t", p=128), xT.v, reads=xT.dall())
            mark('norm2'); rmsnorm_to_hT(PV_FFNG, l)
            mark('ffn'); ffn_phase(l)
            if debug and b == 0 and ("xout%d" % l) in dbg_d:
                P.dma("sp", dbg_d["xout%d" % l].rearrange("(c p) t -> p c t", p=128), xT.v, reads=xT.dall())
        mark('final'); final_store(b)
    mark('end')
    P.finish()
    stack.close()
    return nc, P


def host_consts():
    t = np.arange(S)
    c = {}
    c["c_ident"] = np.eye(128, dtype=np.float32)
    k = np.arange(128)
    c["c_tri"] = np.where(k[None, :] >= k[:, None], 0.0, -30000.0).astype(np.float32)
    base = np.stack([t % 256, 256 * (t // 256), np.ones(S), np.ones(S)]).astype(np.float32)
    c["c_krow"] = np.stack([base * (2.0 ** (-(hd + 1))) for hd in range(8)]).astype(np.float32)
    c["c_qrow"] = np.stack([np.ones(S), np.ones(S), -(t % 256), -256 * (t // 256)]).astype(np.float32)
    c["c_onehot"] = (np.arange(8)[:, None] == (t // 256)[None, :]).astype(np.float32)
    past = np.zeros((128, 4, 16), np.float32)
    for q4 in range(4):
        qb = q4 + 4
        for hh in range(2):
            past[:, q4, hh * 8 + qb:hh * 8 + 8] = -1e30
    c["c_past"] = past
    return c


def host_params(inp):
    pvec = np.zeros((DEPTH, 128, PV_N), np.float32)
    wabd = np.zeros((DEPTH, 2, 128, 4, 128), np.float32)
    for l in range(DEPTH):
        pvec[l, :, PV_MIXG:PV_MIXG + 8] = _chunked(inp["mix_norm_gain"][l])
        pvec[l, :, PV_FFNG:PV_FFNG + 8] = _chunked(inp["ffn_norm_gain"][l])
        pvec[l, :, PV_MEMG:PV_MEMG + 8] = _chunked(inp["mem_norm_gain"][l])
        pvec[l, :, PV_FING:PV_FING + 8] = _chunked(inp["final_norm_gain"])
        for k in range(4):
            pvec[l, :, PV_LCW + k * 4:PV_LCW + k * 4 + 4] = _chunked(inp["lru_conv_w"][l, k])
        pvec[l, :, PV_LCB:PV_LCB + 4] = _chunked(inp["lru_conv_b"][l])
        pvec[l, :, PV_LBA:PV_LBA + 4] = _chunked(inp["lru_b_a"][l].reshape(-1))
        pvec[l, :, PV_LBX:PV_LBX + 4] = _chunked(inp["lru_b_x"][l].reshape(-1))
        pvec[l, :, PV_LAM:PV_LAM + 4] = _chunked(inp["lru_lambda"][l])
        for k in range(3):
            pvec[l, :, PV_FCW + k * 24:PV_FCW + k * 24 + 24] = _chunked(inp["ffn_conv_w"][l, k])
        pvec[l, :, PV_FCB:PV_FCB + 24] = _chunked(inp["ffn_conv_b"][l])
        for g, nm in enumerate(("lru_w_a", "lru_w_x")):
            w = inp[nm][l]
            for hd in range(8):
                c, o = hd // 2, (hd % 2) * 64
                wabd[l, g, o:o + 64, c, o:o + 64] = w[hd]
    return pvec, wabd


_CACHE = {}


def kernel(**inputs):
    inp = {k: np.asarray(v) for k, v in inputs.items()}
    if "nc" not in _CACHE:
        _CACHE["nc"] = build_program()[0]
    nc = _CACHE["nc"]
    pvec, wabd = host_params(inp)
    consts = host_consts()
    shared = dict(consts)
    shared.update(pvec=pvec, wabd=wabd)
    for k in ("w_in", "w_mem_kv", "w_branch", "w_out", "w_ffn_gate", "w_ffn_up", "w_ffn_down"):
        shared[k] = np.ascontiguousarray(inp[k], dtype=np.float32)
    in_maps = []
    for c in range(NCORES):
        m = dict(shared)
        m["x"] = np.ascontiguousarray(inp["x"][c * SEQ_PER_CORE:(c + 1) * SEQ_PER_CORE], dtype=np.float32)
        m["mem"] = np.ascontiguousarray(inp["mem"][c * SEQ_PER_CORE:(c + 1) * SEQ_PER_CORE], dtype=np.float32)
        in_maps.append(m)
    res = run_bass_kernel_spmd(nc, in_maps, core_ids=list(range(NCORES)))
    out = np.concatenate([np.asarray(r["out"]) for r in res.results], axis=0)
    return out.astype(np.float32)
```
